# Optimizing a Trainium2 kernel written in Bass

```python
import jax, jax.numpy as jnp
from jax import lax
import numpy as np


D_MODEL = 4096
BATCH = 2
SEQ = 8192
DEPTH = 1

HEAD_DIM = 64
N_Q_HEADS = 32
N_KV_HEADS = 4
Q_PER_KV = N_Q_HEADS // N_KV_HEADS
ATTN_WIDTH = N_Q_HEADS * HEAD_DIM
KV_WIDTH = N_KV_HEADS * HEAD_DIM
WINDOW = 128
ATTN_BLOCK = 128

POOL_WINDOWS = (2, 4, 8, 16)
N_POOL_GROUPS = 4
POOL_WIDTH = D_MODEL // 2
POOL_GROUP_DIM = POOL_WIDTH // N_POOL_GROUPS

IN_WIDTH = ATTN_WIDTH + 2 * KV_WIDTH + POOL_WIDTH + 2 * D_MODEL
SPLITS = (ATTN_WIDTH, ATTN_WIDTH + KV_WIDTH, ATTN_WIDTH + 2 * KV_WIDTH,
          ATTN_WIDTH + 2 * KV_WIDTH + POOL_WIDTH, ATTN_WIDTH + 2 * KV_WIDTH + POOL_WIDTH + D_MODEL)

N_GROUPS = 8
EXPERTS_PER_GROUP = 8
N_EXPERTS = N_GROUPS * EXPERTS_PER_GROUP
TOP_K = 2
EXPERT_FF = 512
MOE_BLOCK = 128

EPS = 1e-6
NEG_INF = -1e30

kernel_name = 'hybrid_swa_pool_hier_moe'


def rms_norm(x, w):
    x32 = x.astype(jnp.float32)
    y = x32 * lax.rsqrt(jnp.mean(x32 * x32, axis=-1, keepdims=True) + EPS)
    return (y * w.astype(jnp.float32)).astype(x.dtype)


def alibi_slopes(n):
    return 2.0 ** (-8.0 * jnp.arange(1, n + 1, dtype=jnp.float32) / n)


def sliding_window_attention(q, k, v, sinks):
    B, S = q.shape[0], q.shape[1]
    nb = S // ATTN_BLOCK
    qb = q.reshape(B, nb, ATTN_BLOCK, N_KV_HEADS, Q_PER_KV, HEAD_DIM)

    def banded(t):
        pad = jnp.zeros_like(t[:, :ATTN_BLOCK])
        prev = jnp.concatenate([pad, t[:, :-ATTN_BLOCK]], axis=1)
        prev = prev.reshape(B, nb, ATTN_BLOCK, N_KV_HEADS, HEAD_DIM)
        cur = t.reshape(B, nb, ATTN_BLOCK, N_KV_HEADS, HEAD_DIM)
        return jnp.concatenate([prev, cur], axis=2)

    kb, vb = banded(k), banded(v)
    scores = jnp.einsum('bnqkgd,bnjkd->bnkgqj', qb, kb).astype(jnp.float32) * (HEAD_DIM ** -0.5)
    i = jnp.arange(ATTN_BLOCK)[:, None]
    j = jnp.arange(2 * ATTN_BLOCK)[None, :]
    dist = i + ATTN_BLOCK - j
    key_pos = jnp.arange(nb)[:, None, None] * ATTN_BLOCK - ATTN_BLOCK + j[None]
    valid = (dist >= 0) & (dist < WINDOW) & (key_pos >= 0)
    slopes = alibi_slopes(N_Q_HEADS).reshape(N_KV_HEADS, Q_PER_KV)
    scores = scores - slopes[:, :, None, None] * dist.astype(jnp.float32)
    scores = jnp.where(valid[None, :, None, None], scores, NEG_INF)
    sink = sinks.astype(jnp.float32).reshape(N_KV_HEADS, Q_PER_KV)[:, :, None, None]
    m = jnp.maximum(scores.max(axis=-1, keepdims=True), sink)
    p = jnp.exp(scores - m)
    p = p / (p.sum(axis=-1, keepdims=True) + jnp.exp(sink - m))
    out = jnp.einsum('bnkgqj,bnjkd->bnqkgd', p.astype(v.dtype), vb)
    return out.reshape(B, S, ATTN_WIDTH)


def multiscale_pool(p, w_pool, pool_scale):
    B, S = p.shape[0], p.shape[1]
    pg = p.reshape(B, S, N_POOL_GROUPS, POOL_GROUP_DIM).astype(jnp.float32)
    cs = jnp.cumsum(pg, axis=1)
    t = jnp.arange(1, S + 1, dtype=jnp.float32)
    diffs = []
    for g, w in enumerate(POOL_WINDOWS):
        c_g = cs[:, :, g]
        lagged = jnp.pad(c_g, ((0, 0), (w, 0), (0, 0)))[:, :S]
        mean = (c_g - lagged) / jnp.minimum(t, float(w))[None, :, None]
        diffs.append(mean - pg[:, :, g])
    d = jnp.stack(diffs, axis=2).astype(p.dtype)
    y = jnp.einsum('bsgc,gcd->bsgd', d, w_pool).reshape(B, S, POOL_WIDTH)
    return y * pool_scale


def hierarchical_moe(h, w_router_group, b_router_group, w_router_expert, b_router_expert,
                     w_gate, w_up, w_down):
    B, S, D = h.shape
    T = B * S
    hf = h.reshape(T, D)
    g_logits = (hf @ w_router_group + b_router_group).astype(jnp.float32)
    g_prob = jax.nn.softmax(g_logits, axis=-1)
    g_sel = jnp.argmax(g_logits, axis=-1)
    p_group = jnp.take_along_axis(g_prob, g_sel[:, None], axis=-1)[:, 0]
    e_logits = (hf @ w_router_expert + b_router_expert).astype(jnp.float32)
    e_logits = e_logits.reshape(T, N_GROUPS, EXPERTS_PER_GROUP)
    e_in = jnp.take_along_axis(e_logits, g_sel[:, None, None], axis=1)[:, 0]
    top_v, top_i = lax.top_k(e_in, TOP_K)
    weights = p_group[:, None] * jax.nn.softmax(top_v, axis=-1)
    expert_id = g_sel[:, None] * EXPERTS_PER_GROUP + top_i

    A = T * TOP_K
    cap = A + N_EXPERTS * MOE_BLOCK
    nblk = cap // MOE_BLOCK
    flat_e = expert_id.reshape(A).astype(jnp.int32)
    flat_t = jnp.repeat(jnp.arange(T, dtype=jnp.int32), TOP_K)
    flat_w = weights.reshape(A)
    order = jnp.argsort(flat_e)
    se = flat_e[order]
    counts = jax.ops.segment_sum(jnp.ones_like(flat_e), flat_e, num_segments=N_EXPERTS)
    padded = ((counts + MOE_BLOCK - 1) // MOE_BLOCK) * MOE_BLOCK
    start = jnp.cumsum(counts) - counts
    pend = jnp.cumsum(padded)
    pstart = pend - padded
    dest = pstart[se] + (jnp.arange(A, dtype=jnp.int32) - start[se])
    buf_tok = jnp.full((cap,), T, dtype=jnp.int32).at[dest].set(flat_t[order])
    buf_w = jnp.zeros((cap,), jnp.float32).at[dest].set(flat_w[order])
    blk_start = jnp.arange(nblk, dtype=jnp.int32) * MOE_BLOCK
    blk_e = jnp.minimum(jnp.searchsorted(pend, blk_start, side='right'), N_EXPERTS - 1)

    h_pad = jnp.concatenate([hf, jnp.zeros((1, D), hf.dtype)], axis=0)
    xs = h_pad[buf_tok].reshape(nblk, MOE_BLOCK, D)

    def expert_block(args):
        xb, e = args
        a = xb @ w_gate[e]
        u = xb @ w_up[e]
        return (jax.nn.silu(a) * u) @ w_down[e]

    ys = lax.map(expert_block, (xs, blk_e)).reshape(cap, D)
    ys = ys * buf_w[:, None].astype(ys.dtype)
    out = jnp.zeros((T + 1, D), ys.dtype).at[buf_tok].add(ys)[:T]
    return out.reshape(B, S, D)


def hybrid_layer(x, c, w_ada, b_ada, norm1_w, w_in, q_norm_w, k_norm_w, sinks, w_pool, pool_scale,
                 w_attn_up, w_pool_up, w_out, norm2_w, w_router_group, b_router_group,
                 w_router_expert, b_router_expert, w_gate, w_up, w_down):
    B, S, D = x.shape
    ada = jax.nn.silu(c) @ w_ada + b_ada
    shift1, scale1, gate1, shift2, scale2, gate2 = [a[:, None, :] for a in jnp.split(ada, 6, axis=-1)]

    h = rms_norm(x, norm1_w) * (1.0 + scale1) + shift1
    proj = h @ w_in
    q, k, v, p, ga, gb = jnp.split(proj, SPLITS, axis=-1)
    q = rms_norm(q.reshape(B, S, N_Q_HEADS, HEAD_DIM), q_norm_w)
    k = rms_norm(k.reshape(B, S, N_KV_HEADS, HEAD_DIM), k_norm_w)
    v = v.reshape(B, S, N_KV_HEADS, HEAD_DIM)
    y_attn = sliding_window_attention(q, k, v, sinks) @ w_attn_up
    y_pool = multiscale_pool(p, w_pool, pool_scale) @ w_pool_up
    mixed = jax.nn.sigmoid(ga) * y_attn + jax.nn.sigmoid(gb) * y_pool
    x = x + gate1 * (mixed @ w_out)

    h2 = rms_norm(x, norm2_w) * (1.0 + scale2) + shift2
    y_ffn = hierarchical_moe(h2, w_router_group, b_router_group, w_router_expert, b_router_expert,
                             w_gate, w_up, w_down)
    return x + gate2 * y_ffn


def setup_inputs(seed: int = 0) -> dict:
    key = jax.random.key(seed)
    ks = jax.random.split(key, 22)
    L = DEPTH

    def nrm(k, shape, scale):
        return jax.random.normal(k, shape, jnp.float32) * scale

    return {
        'x': nrm(ks[0], (BATCH, SEQ, D_MODEL), 1.0),
        'c': nrm(ks[1], (BATCH, D_MODEL), 1.0),
        'w_ada': nrm(ks[2], (L, D_MODEL, 6 * D_MODEL), 0.2 * D_MODEL ** -0.5),
        'b_ada': nrm(ks[3], (L, 6 * D_MODEL), 0.02),
        'norm1_w': 1.0 + nrm(ks[4], (L, D_MODEL), 0.02),
        'w_in': nrm(ks[5], (L, D_MODEL, IN_WIDTH), D_MODEL ** -0.5),
        'q_norm_w': 1.0 + nrm(ks[6], (L, HEAD_DIM), 0.02),
        'k_norm_w': 1.0 + nrm(ks[7], (L, HEAD_DIM), 0.02),
        'sinks': nrm(ks[8], (L, N_Q_HEADS), 0.5),
        'w_pool': nrm(ks[9], (L, N_POOL_GROUPS, POOL_GROUP_DIM, POOL_GROUP_DIM), POOL_GROUP_DIM ** -0.5),
        'pool_scale': 1.0 + nrm(ks[10], (L, POOL_WIDTH), 0.02),
        'w_attn_up': nrm(ks[11], (L, ATTN_WIDTH, D_MODEL), ATTN_WIDTH ** -0.5),
        'w_pool_up': nrm(ks[12], (L, POOL_WIDTH, D_MODEL), POOL_WIDTH ** -0.5),
        'w_out': nrm(ks[13], (L, D_MODEL, D_MODEL), D_MODEL ** -0.5),
        'norm2_w': 1.0 + nrm(ks[14], (L, D_MODEL), 0.02),
        'w_router_group': nrm(ks[15], (L, D_MODEL, N_GROUPS), D_MODEL ** -0.5),
        'b_router_group': nrm(ks[16], (L, N_GROUPS), 0.01),
        'w_router_expert': nrm(ks[17], (L, D_MODEL, N_EXPERTS), D_MODEL ** -0.5),
        'b_router_expert': nrm(ks[18], (L, N_EXPERTS), 0.01),
        'w_gate': nrm(ks[19], (L, N_EXPERTS, D_MODEL, EXPERT_FF), D_MODEL ** -0.5),
        'w_up': nrm(ks[20], (L, N_EXPERTS, D_MODEL, EXPERT_FF), D_MODEL ** -0.5),
        'w_down': nrm(ks[21], (L, N_EXPERTS, EXPERT_FF, D_MODEL), EXPERT_FF ** -0.5),
    }


def reference(x, c, w_ada, b_ada, norm1_w, w_in, q_norm_w, k_norm_w, sinks, w_pool, pool_scale,
              w_attn_up, w_pool_up, w_out, norm2_w, w_router_group, b_router_group,
              w_router_expert, b_router_expert, w_gate, w_up, w_down):
    for l in range(DEPTH):
        x = hybrid_layer(x, c, w_ada[l], b_ada[l], norm1_w[l], w_in[l], q_norm_w[l], k_norm_w[l],
                         sinks[l], w_pool[l], pool_scale[l], w_attn_up[l], w_pool_up[l], w_out[l],
                         norm2_w[l], w_router_group[l], b_router_group[l], w_router_expert[l],
                         b_router_expert[l], w_gate[l], w_up[l], w_down[l])
    return x
```

```python
import numpy as np
from contextlib import ExitStack
import concourse.bass as bass
import concourse.mybir as mybir
from concourse.bass_utils import run_bass_kernel_spmd

F32 = mybir.dt.float32
BF16 = mybir.dt.bfloat16
I32 = mybir.dt.int32
ALU = mybir.AluOpType
AF = mybir.ActivationFunctionType
AX = mybir.AxisListType

D = 4096
KC = 32
NL = 2048
NT = 16
HALO = 128
NTOK = NL + HALO
TT = 17
CAP = 128
NE = 64
NSLOT = NE * CAP
ZROW = NSLOT
EPS = 1e-6
NEG = -30000.0
INW = 12800


class Sched:
    def __init__(self, nc, stack):
        self.nc = nc
        self.stack = stack
        self.esem = {}
        self.cnt = {e: 0 for e in ["pe", "act", "dve", "pool", "sp"]}
        for e in ["pe", "act", "dve", "pool"]:
            self.esem[e] = stack.enter_context(nc.semaphore("c_" + e))
        self.dpool = []
        self.dkey = {}
        self.dfree = []
        self.res = {}
        self.waited = {e: {} for e in ["pe", "act", "dve", "pool", "sp"]}
        self.pending = {e: [] for e in ["pe", "act", "dve", "pool", "sp"]}
        self.sems = {}
        self.trace = {}
        self.mute = False
        self.only = None

    def set_phase(self, k):
        self.mute = (self.only is not None) and (k not in self.only)

    def _dma_sem(self, key):
        if key not in self.dkey:
            if self.dfree:
                idx = self.dfree.pop()
            else:
                idx = len(self.dpool)
                sem = self.stack.enter_context(self.nc.semaphore(f"d{idx}"))
                self.dpool.append([sem, 0])
                self.sems[f"d#{idx}"] = sem
            self.dkey[key] = idx
        return self.dkey[key]

    def _deps(self, eng, reads, writes):
        toks = {}

        def add(t):
            if t is None:
                return
            n, v = t
            if toks.get(n, 0) < v:
                toks[n] = v

        for k in reads:
            r = self.res.get(k)
            if r:
                add(r[0])
        for k in writes:
            r = self.res.get(k)
            if r:
                add(r[0])
                for t in r[1]:
                    add(t)
        out = []
        for n, v in toks.items():
            if eng == "pe" and n == "c_pe":
                continue
            if self.waited[eng].get(n, 0) >= v:
                continue
            self.waited[eng][n] = v
            out.append((n, v))
        return out

    def _record(self, tok, reads, writes):
        for k in reads:
            r = self.res.setdefault(k, [None, []])
            r[1].append(tok)
            if len(r[1]) > 32:
                mm = {}
                for n, v in r[1]:
                    mm[n] = max(mm.get(n, 0), v)
                r[1] = list(mm.items())
        for k in writes:
            self.res[k] = [tok, []]

    def op(self, eng, fn, reads=(), writes=(), inc=True):
        if self.mute:
            return
        reads = list(reads)
        writes = list(writes)
        deps = self._deps(eng, reads, writes)
        if inc:
            self.cnt[eng] += 1
            tok = ("c_" + eng, self.cnt[eng])
            pend = self.pending[eng]
            self.pending[eng] = []
            for (pr, pw) in pend:
                self._record(tok, pr, pw)
            self._record(tok, reads, writes)
        else:
            self.pending[eng].append((reads, writes))
        self._emit(eng, deps, fn, inc)

    def dma(self, eng, key, fn, reads=(), writes=()):
        if self.mute:
            return
        reads = list(reads)
        writes = list(writes)
        deps = self._deps(eng, reads, writes)
        idx = self._dma_sem(key)
        self.dpool[idx][1] += 16
        tok = (f"d#{idx}", self.dpool[idx][1])
        self._record(tok, reads, writes)
        self._emit(eng, deps, fn, ("dma", idx))

    def barrier(self):
        for e in ["pe", "act", "dve", "pool"]:
            assert not self.pending[e], f"pending accesses without inc on {e}"
        toks = [("c_" + e, self.cnt[e]) for e in ["pe", "act", "dve", "pool"] if self.cnt[e] > 0]
        toks += [(f"d#{i}", v[1]) for i, v in enumerate(self.dpool) if v[1] > 0]
        for e in ["pe", "act", "dve", "pool", "sp"]:
            deps = []
            for n, v in toks:
                if self.waited[e].get(n, 0) >= v:
                    continue
                self.waited[e][n] = v
                deps.append((n, v))
            if deps:
                self._emit(e, deps, None, False)
        self.dkey = {}
        self.dfree = list(range(len(self.dpool)))

    def _engobj(self, e):
        nc = self.nc
        return {"pe": nc.tensor, "act": nc.scalar, "dve": nc.vector, "pool": nc.gpsimd, "sp": nc.sync}[e]

    def _semh(self, n):
        if n.startswith("c_"):
            return self.esem[n[2:]]
        return self.sems[n]

    def _emit(self, engname, deps, fn, inc):
        eng = self._engobj(engname)
        tr_ = self.trace.setdefault(engname, [])
        for n, v in deps:
            eng.wait_ge(self._semh(n), v)
            tr_.append(("w", n, v))
        if fn is None:
            return
        ins = fn(eng)
        if inc is True:
            ins.then_inc(self.esem[engname], 1)
            tr_.append(("i", "c_" + engname, 1))
        elif isinstance(inc, tuple):
            ins.then_inc(self.dpool[inc[1]][0], 16)
            tr_.append(("i", f"d#{inc[1]}", 16))
        else:
            tr_.append(("n",))

    def simulate(self):
        val = {}
        pc = {e: 0 for e in self.trace}
        progress = True
        while progress:
            progress = False
            for e, t in self.trace.items():
                while pc[e] < len(t):
                    op = t[pc[e]]
                    if op[0] == "w":
                        if val.get(op[1], 0) < op[2]:
                            break
                    elif op[0] == "i":
                        val[op[1]] = val.get(op[1], 0) + op[2]
                    pc[e] += 1
                    progress = True
        stuck = {e: (pc[e], len(t), t[pc[e]]) for e, t in self.trace.items() if pc[e] < len(t)}
        return stuck, val


def mm(out, lhsT, rhs, st, sp):
    return lambda e: e.matmul(out, lhsT=lhsT, rhs=rhs, start=st, stop=sp)


def tr(out, in_, ident):
    return lambda e: e.transpose(out=out, in_=in_, identity=ident)


def dmaf(out, in_):
    return lambda e: e.dma_start(out=out, in_=in_)


def build_nc(debug=(), last_phase=99, only=None):
    nc = bass.Bass("TRN2", target_bir_lowering=False)

    def din(name, shape, dt=F32):
        return nc.dram_tensor(name, list(shape), dt, kind="ExternalInput").ap()

    def dscr(name, shape, dt):
        if name in debug:
            return nc.dram_tensor(name, list(shape), dt, kind="ExternalOutput").ap()
        return nc.dram_tensor(name, list(shape), dt).ap()

    xh = din("xh", [NTOK, D])
    cpp = din("cpp", [128, 32])
    flag_d = din("flag", [128, 2])
    invc_d = din("invc", [128, 64])
    w_ada = din("w_ada", [D, 6 * D])
    b_ada = din("b_ada", [6 * D])
    bada_pp = din("bada_pp", [128, 64])
    n1w_pp = din("n1w_pp", [128, 32])
    n2w = din("n2w", [D])
    w_in = din("w_in", [D, INW])
    qkw_rep = din("qkw_rep", [128, 512])
    sinks_d = din("sinks", [32])
    abias_d = din("abias", [128, 32 * 256])
    w_pool = din("w_pool", [4, 512, 512])
    pscale_pp = din("pscale_pp", [128, 16])
    w_aup = din("w_aup", [2048, D])
    w_bup = din("w_bup", [2048, D])
    w_out = din("w_out", [D, D])
    wr_d = din("wr", [D, 72])
    rb_d = din("rb", [72])
    w_gate = din("w_gate", [NE, D, 512])
    w_up = din("w_up", [NE, D, 512])
    w_down = din("w_down", [NE, 512, D])
    consts_d = din("consts", [128, 128 * 3 + 64])
    out_d = nc.dram_tensor("out", [NL, D], F32, kind="ExternalOutput").ap()

    adab = dscr("adab", [4, 128, D], F32)
    qkT_s = dscr("qkT_s", [2048 + 256, NTOK], BF16)
    v_s = dscr("v_s", [NTOK, 256], BF16)
    dT_s = dscr("dT_s", [2048, NL], BF16)
    sgT_s = dscr("sgT_s", [2 * D, NL], BF16)
    mixT_s = dscr("mixT_s", [D, NL], BF16)
    x1_s = dscr("x1_s", [NL, D], F32)
    xs_buf = dscr("xs_buf", [NSLOT + 1, D], BF16)
    y_s = dscr("y_s", [NSLOT + 1, D], F32)
    dbg_s = dscr("dbg_s", [128, NT * 8], F32)

    _uid = [0]

    def _un(n):
        _uid[0] += 1
        return f"{n}_u{_uid[0]}"

    with ExitStack() as top:
        S = Sched(nc, top)
        S.only = only
        sbt = lambda n, s, d: top.enter_context(nc.sbuf_tensor(_un(n), s, d))
        cst = sbt("cst", [128, 128 * 3 + 64], F32)
        identf = cst[:, 0:128]
        ident = sbt("ident", [128, 128], BF16)
        tri = sbt("tri", [128, 128], BF16)
        ones = sbt("ones", [128, 128], BF16)
        ebase = cst[:, 384:448]
        flag = sbt("flagt", [128, 2], F32)
        g1 = sbt("g1", [128, 32], F32)
        sh1 = sbt("sh1", [128, 32], F32)
        epst = sbt("epst", [128, 1], F32)
        cw = sbt("cw", [128, NT, 2], F32)
        di = sbt("di", [128, NT, 2], I32)

        bcreg = nc.gpsimd.alloc_register("bcreg")
        nc.gpsimd.reg_mov(bcreg, NSLOT)
        S.dma("sp", "c0", dmaf(cst[:], consts_d), writes=["cst"])
        S.dma("sp", "c0", dmaf(flag[:], flag_d), writes=["flag"])
        S.op("dve", lambda e: e.tensor_copy(out=ident[:], in_=cst[:, 0:128]), reads=["cst"], writes=["ident"])
        S.op("dve", lambda e: e.tensor_copy(out=tri[:], in_=cst[:, 128:256]), reads=["cst"], writes=["tri"])
        S.op("dve", lambda e: e.tensor_copy(out=ones[:], in_=cst[:, 256:384]), reads=["cst"], writes=["ones"])
        S.op("dve", lambda e: e.memset(epst[:], EPS), writes=["epst"])

        S.set_phase(0)
        with ExitStack() as ph:
            sb = lambda n, s, d: ph.enter_context(nc.sbuf_tensor(_un(n), s, d))
            pp = lambda n, s, d: ph.enter_context(nc.psum_tensor(_un(n), s, d))
            ct = sb("ct", [128, 32], F32)
            cs = sb("cs", [128, 32], BF16)
            cmat = sb("cmat", [128, 32, 128], BF16)
            wA = [sb(f"wA{i}", [128, 32, 512], BF16) for i in range(2)]
            bb = sb("bb", [128, 4 * D], F32)
            n2b = sb("n2b", [128, D], F32)
            bpp = sb("bpp", [128, 64], F32)
            n1pp = sb("n1pp", [128, 32], F32)
            adaT = sb("adaT", [128, 64], F32)
            stg = [sb(f"stg{i}", [128, 512], F32) for i in range(2)]
            pA = [pp(f"pA{i}", [128, 512], F32) for i in range(2)]
            pTa = pp("pTa", [128, 64], F32)

            S.dma("sp", "c0", dmaf(ct[:], cpp), writes=["ct"])
            S.dma("sp", "c0", dmaf(bpp[:], bada_pp), writes=["bpp"])
            S.dma("sp", "c0", dmaf(n1pp[:], n1w_pp), writes=["n1pp"])
            S.dma("sp", "c1", dmaf(bb[:], b_ada[2 * D:6 * D].partition_broadcast(128)), writes=["bb"])
            S.dma("sp", "c1", dmaf(n2b[:], n2w.partition_broadcast(128)), writes=["n2b"])
            S.op("act", lambda e: e.activation(out=cs[:], in_=ct[:], func=AF.Silu), reads=["ct"], writes=["cs"])
            S.op("dve", lambda e: e.tensor_copy(out=cmat[:], in_=cs[:].unsqueeze(2).to_broadcast([128, 32, 128])),
                 reads=["cs"], writes=["cmat"])

            def loadA(blk):
                i = blk % 2
                S.dma("pool", f"wA{i}", dmaf(wA[i][:], w_ada[:, blk * 512:(blk + 1) * 512].rearrange("(k p) n -> p k n", p=128)),
                      writes=[f"wA{i}"])

            loadA(0)
            for blk in range(48):
                if blk + 1 < 48:
                    loadA(blk + 1)
                i = blk % 2
                if blk < 16:
                    for fc in range(4):
                        col = blk * 4 + fc
                        for kc in range(KC):
                            S.op("pe", mm(pTa[:, col:col + 1], wA[i][:, kc, fc * 128:(fc + 1) * 128], cs[:, kc:kc + 1], kc == 0, kc == KC - 1),
                                 reads=[f"wA{i}", "cs"], writes=["pTa"], inc=(kc == KC - 1 and fc == 3))
                    if blk == 15:
                        S.op("dve", lambda e: e.tensor_tensor(out=adaT[:], in0=pTa[:], in1=bpp[:], op=ALU.add),
                             reads=["pTa", "bpp"], writes=["adaT"])
                        S.op("dve", lambda e: e.scalar_tensor_tensor(out=g1[:], in0=adaT[:, 32:64], scalar=1.0, in1=n1pp[:], op0=ALU.add, op1=ALU.mult),
                             reads=["adaT", "n1pp"], writes=["g1"])
                        S.op("dve", lambda e: e.tensor_copy(out=sh1[:], in_=adaT[:, 0:32]), reads=["adaT"], writes=["sh1"])
                else:
                    for kc in range(KC):
                        S.op("pe", mm(pA[i][:], cmat[:, kc, :], wA[i][:, kc, :], kc == 0, kc == KC - 1),
                             reads=[f"wA{i}", "cmat"], writes=[f"pA{i}"], inc=(kc == KC - 1))
                    cols = (blk - 16) * 512
                    which = (blk - 16) // 8
                    c4 = cols - which * D
                    S.op("dve", lambda e, i=i, cols=cols: e.tensor_tensor(out=stg[i][:], in0=pA[i][:], in1=bb[:, cols:cols + 512], op=ALU.add),
                         reads=[f"pA{i}", "bb"], writes=[f"stg{i}"])
                    if which == 2:
                        S.op("dve", lambda e, i=i, c4=c4: e.scalar_tensor_tensor(out=stg[i][:], in0=stg[i][:], scalar=1.0, in1=n2b[:, c4:c4 + 512], op0=ALU.add, op1=ALU.mult),
                             reads=[f"stg{i}", "n2b"], writes=[f"stg{i}"])
                    S.dma("sp", f"stg{i}", dmaf(adab[which][:, c4:c4 + 512], stg[i][:]), reads=[f"stg{i}"], writes=["adab"])
            S.barrier()
        if last_phase <= 0:
            S.barrier()
            return nc

        S.set_phase(1)
        with ExitStack() as ph12:
            hT = ph12.enter_context(nc.sbuf_tensor(_un("hT"), [128, KC, NTOK], BF16))
            with ExitStack() as ph:
                sb = lambda n, s, d: ph.enter_context(nc.sbuf_tensor(_un(n), s, d))
                pp = lambda n, s, d: ph.enter_context(nc.psum_tensor(_un(n), s, d))
                xt = [sb(f"xt{i}", [128, D], F32) for i in range(2)]
                xb = sb("xb", [128, D], BF16)
                st = sb("st1", [128, TT, 4], F32)
                pT = [pp(f"pT{i}", [128, 8, 128], BF16) for i in range(2)]
                S.dma("sp", "xt0", dmaf(xt[0][:], xh[0:128, :]), writes=["xt0"])
                for t in range(TT):
                    i = t % 2
                    if t + 1 < TT:
                        S.dma("sp", f"xt{1 - i}", dmaf(xt[1 - i][:], xh[(t + 1) * 128:(t + 2) * 128, :]), writes=[f"xt{1 - i}"])
                    S.op("act", lambda e, i=i, t=t: e.activation(out=xb[:], in_=xt[i][:], func=AF.Square, accum_out=st[:, t, 0:1]),
                         reads=[f"xt{i}"], writes=["xb", f"st{t}"])
                    S.op("act", lambda e, t=t: e.activation(out=st[:, t, 1:2], in_=st[:, t, 0:1], func=AF.Sqrt, scale=1.0 / D, bias=epst[:, 0:1]),
                         reads=[f"st{t}", "epst"], writes=[f"st{t}"])
                    S.op("dve", lambda e, t=t: e.reciprocal(out=st[:, t, 2:3], in_=st[:, t, 1:2]), reads=[f"st{t}"], writes=[f"st{t}"])
                    S.op("act", lambda e, i=i, t=t: e.activation(out=xb[:], in_=xt[i][:], func=AF.Copy, scale=st[:, t, 2:3]),
                         reads=[f"xt{i}", f"st{t}"], writes=["xb"])
                    for g in range(4):
                        pi = (t * 4 + g) % 2
                        for j in range(8):
                            kc = g * 8 + j
                            S.op("pe", tr(pT[pi][:, j, :], xb[:, kc * 128:(kc + 1) * 128], ident[:]),
                                 reads=["xb", "ident"], writes=[f"pT{pi}"], inc=(j == 7))
                        for j in range(8):
                            kc = g * 8 + j
                            eng = "dve" if j % 2 == 0 else "act"
                            if eng == "dve":
                                S.op("dve", lambda e, pi=pi, j=j, kc=kc, t=t: e.tensor_scalar(out=hT[:, kc, t * 128:(t + 1) * 128], in0=pT[pi][:, j, :], scalar1=g1[:, kc:kc + 1], scalar2=sh1[:, kc:kc + 1], op0=ALU.mult, op1=ALU.add),
                                     reads=[f"pT{pi}", "g1", "sh1"], writes=[f"hT{t}"])
                            else:
                                S.op("act", lambda e, pi=pi, j=j, kc=kc, t=t: e.activation(out=hT[:, kc, t * 128:(t + 1) * 128], in_=pT[pi][:, j, :], func=AF.Identity, scale=g1[:, kc:kc + 1], bias=sh1[:, kc:kc + 1]),
                                     reads=[f"pT{pi}", "g1", "sh1"], writes=[f"hT{t}"])
                S.barrier()

            S.set_phase(2)
            with ExitStack() as ph:
                sb = lambda n, s, d: ph.enter_context(nc.sbuf_tensor(_un(n), s, d))
                pp = lambda n, s, d: ph.enter_context(nc.psum_tensor(_un(n), s, d))
                wB = [sb(f"wB{i}", [128, KC, 256], BF16) for i in range(2)]
                qkw = sb("qkw", [128, 512], F32)
                invc = sb("invct", [128, 64], F32)
                st2 = sb("st2", [128, 8], F32)
                U1 = sb("U1", [128, 2 * NTOK], BF16)
                stT = U1[:].rearrange("p (a b) -> p a b", a=2)
                vst = U1[:].rearrange("p (a b) -> p a b", a=TT)
                pb2 = U1[:].bitcast(F32)
                U2 = sb("U2", [128, NTOK], F32)
                pb1 = U2[:]
                sq = U2[:, 0:256]
                qn = U2[:, 256:512]
                qb = [U2[:, 512:640].bitcast(BF16), U2[:, 640:768].bitcast(BF16)]
                pt = sb("pt", [128, NTOK], F32)
                dst0 = sb("dst0", [128, NL], BF16)
                dst = [dst0, dst0]
                bank = [pp(f"bk{i}", [128, 512], F32) for i in range(7)]
                pTq = pp("pTq", [128, 2, 128], BF16)
                S.dma("sp", "c0", dmaf(qkw[:], qkw_rep), writes=["qkw"])
                S.dma("sp", "c0", dmaf(invc[:], invc_d), writes=["invc"])
                allhT = [f"hT{t}" for t in range(TT)]

                def loadB(b):
                    i = b % 2
                    S.dma("pool", f"wB{i}", dmaf(wB[i][:], w_in[:, b * 256:(b + 1) * 256].rearrange("(k p) n -> p k n", p=128)),
                          writes=[f"wB{i}"])

                NBLK = INW // 256
                loadB(0)
                ecnt = 0
                for b in range(NBLK):
                    if b + 1 < NBLK:
                        loadB(b + 1)
                    i = b % 2
                    wk = f"wB{i}"
                    if b < 10:
                        for t in range(TT):
                            bi = t % 2
                            pq = bank[bi]
                            for kc in range(KC):
                                S.op("pe", mm(pq[:, 0:256], hT[:, kc, t * 128:(t + 1) * 128], wB[i][:, kc, :], kc == 0, kc == KC - 1),
                                     reads=[wk, f"hT{t}"], writes=[f"bk{bi}"], inc=(kc == KC - 1))
                            if b == 9:
                                S.op("act", lambda e, pq=pq, t=t: e.copy(out=vst[:, t, :], in_=pq[:, 0:256]), reads=[f"bk{bi}"], writes=["U1"])
                                continue
                            wsl = qkw[:, 0:256] if b < 8 else qkw[:, 256:512]
                            S.op("act", lambda e, pq=pq: e.activation(out=sq[:], in_=pq[:, 0:256], func=AF.Square), reads=[f"bk{bi}"], writes=["U2"])
                            S.op("dve", lambda e: e.tensor_reduce(out=st2[:, 0:4], in_=sq[:].rearrange("p (a b) -> p a b", a=4), axis=AX.X, op=ALU.add),
                                 reads=["U2"], writes=["st2"])
                            S.op("act", lambda e: e.activation(out=st2[:, 4:8], in_=st2[:, 0:4], func=AF.Sqrt, scale=1.0 / 64, bias=epst[:, 0:1]),
                                 reads=["st2", "epst"], writes=["st2"])
                            S.op("dve", lambda e: e.reciprocal(out=st2[:, 0:4], in_=st2[:, 4:8]), reads=["st2"], writes=["st2"])
                            S.op("dve", lambda e, pq=pq: e.tensor_tensor(out=qn[:].rearrange("p (a b) -> p a b", a=4), in0=pq[:, 0:256].rearrange("p (a b) -> p a b", a=4),
                                                                    in1=st2[:, 0:4].unsqueeze(2).to_broadcast([128, 4, 64]), op=ALU.mult),
                                 reads=[f"bk{bi}", "st2"], writes=["U2"])
                            qi = ecnt % 2
                            ecnt += 1
                            qscale = 0.125 if b < 8 else 1.0
                            S.op("dve", lambda e, qi=qi, wsl=wsl, qscale=qscale: e.scalar_tensor_tensor(out=qb[qi][:], in0=qn[:], scalar=qscale, in1=wsl, op0=ALU.mult, op1=ALU.mult),
                                 reads=["U2", "qkw"], writes=["U2"])
                            for j in range(2):
                                S.op("pe", tr(pTq[:, j, :], qb[qi][:, j * 128:(j + 1) * 128], ident[:]), reads=["U2", "ident"], writes=["pTq"], inc=(j == 1))
                            S.op("act", lambda e, t=t: e.copy(out=stT[:, :, t * 128:(t + 1) * 128], in_=pTq[:]), reads=["pTq"], writes=["U1"])
                        if b == 9:
                            S.dma("sp", "vst", dmaf(v_s.rearrange("(t p) c -> p t c", p=128), vst[:]), reads=["U1"], writes=["v_s"])
                        else:
                            for j in range(2):
                                S.dma("sp", "stT", dmaf(qkT_s[b * 256 + j * 128: b * 256 + (j + 1) * 128, :], stT[:, j, :]), reads=["U1"], writes=["qkT_s"])
                    else:
                        for m in range(2):
                            is_p = b < 18
                            col0 = b * 256 + m * 128 - 2560
                            for tb in range(4):
                                for kc in range(KC):
                                    S.op("pe", mm(bank[tb][:], wB[i][:, kc, m * 128:(m + 1) * 128], hT[:, kc, 128 + tb * 512:128 + (tb + 1) * 512], kc == 0, kc == KC - 1),
                                         reads=[wk] + allhT[1 + tb * 4:1 + (tb + 1) * 4], writes=[f"bk{tb}"], inc=(kc == KC - 1))
                            di_ = ecnt % 2
                            ecnt += 1
                            if is_p:
                                for kc in range(KC):
                                    S.op("pe", mm(bank[4][:, 0:128], wB[i][:, kc, m * 128:(m + 1) * 128], hT[:, kc, 0:128], kc == 0, kc == KC - 1),
                                         reads=[wk, "hT0"], writes=["bk4"], inc=(kc == KC - 1))
                                g = col0 // 512
                                w = (2, 4, 8, 16)[g]
                                S.op("act", lambda e: e.activation(out=pt[:, 0:128], in_=bank[4][:, 0:128], func=AF.Copy, scale=flag[:, 0:1]),
                                     reads=["bk4", "flag"], writes=["pt"])
                                for tb in range(4):
                                    S.op("act", lambda e, tb=tb: e.copy(out=pt[:, 128 + tb * 512:128 + (tb + 1) * 512], in_=bank[tb][:]),
                                         reads=[f"bk{tb}"], writes=["pt"])
                                src = pt
                                srck = "pt"
                                sh = 1
                                bufs = [(pb1, "U2"), (pb2, "U1")]
                                bi2 = 0
                                while sh < w:
                                    dstb, dk = bufs[bi2 % 2]
                                    lo = 2 * sh - 1
                                    S.op("dve", lambda e, dstb=dstb, src=src, sh=sh, lo=lo: e.tensor_tensor(out=dstb[:, lo:NTOK], in0=src[:, lo:NTOK], in1=src[:, lo - sh:NTOK - sh], op=ALU.add),
                                         reads=[srck], writes=[dk])
                                    src, srck = dstb, dk
                                    sh *= 2
                                    bi2 += 1
                                S.op("dve", lambda e, src=src, w=w, di_=di_: e.scalar_tensor_tensor(out=dst[di_][:], in0=src[:, 128:NTOK], scalar=1.0 / w, in1=pt[:, 128:NTOK], op0=ALU.mult, op1=ALU.subtract),
                                     reads=[srck, "pt"], writes=["dst0"])
                                fixb, fixk = (pb1, "U2") if src is not pb1 else (pb2, "U1")
                                S.op("dve", lambda e, src=src, g=g, fixb=fixb: e.tensor_tensor(out=fixb[:, 0:16], in0=src[:, 128:144], in1=invc[:, g * 16:(g + 1) * 16], op=ALU.mult),
                                     reads=[srck, "invc"], writes=[fixk])
                                S.op("dve", lambda e, fixb=fixb, di_=di_: e.tensor_tensor(out=dst[di_][:, 0:16], in0=fixb[:, 0:16], in1=pt[:, 128:144], op=ALU.subtract),
                                     reads=[fixk, "pt"], writes=["dst0"])
                                S.dma("sp", "dst0", dmaf(dT_s[col0:col0 + 128, :], dst[di_][:]), reads=["dst0"], writes=["dT_s"])
                            else:
                                for tb in range(4):
                                    S.op("act", lambda e, tb=tb, di_=di_: e.activation(out=dst[di_][:, tb * 512:(tb + 1) * 512], in_=bank[tb][:], func=AF.Sigmoid),
                                         reads=[f"bk{tb}"], writes=["dst0"])
                                r0 = col0 - 2048
                                S.dma("sp", "dst0", dmaf(sgT_s[r0:r0 + 128, :], dst[di_][:]), reads=["dst0"], writes=["sgT_s"])
                S.barrier()
        if last_phase <= 2:
            S.barrier()
            return nc

        S.set_phase(3)
        with ExitStack() as ph34:
            AT = ph34.enter_context(nc.sbuf_tensor(_un("AT"), [128, 16, NL], BF16))
            with ExitStack() as ph:
                sb = lambda n, s, d: ph.enter_context(nc.sbuf_tensor(_un(n), s, d))
                pp = lambda n, s, d: ph.enter_context(nc.psum_tensor(_un(n), s, d))
                abias = sb("abias", [128, 32, 256], F32)
                esink = sb("esink", [128, 32], F32)
                vall = sb("vall", [128, TT, 256], BF16)
                qTg = [sb(f"qTg{i}", [128, 4, NTOK], BF16) for i in range(2)]
                kTa = [sb(f"kTa{i}", [128, NTOK], BF16) for i in range(2)]
                kTb = [sb(f"kTb{i}", [128, NTOK], BF16) for i in range(2)]
                ssb = [sb(f"ssb{i}", [128, 512], F32) for i in range(2)]
                Pb = [sb(f"Pb{i}", [128, 512], BF16) for i in range(2)]
                PT = [sb(f"PT{i}", [128, 4, 128], BF16) for i in range(2)]
                rsb = sb("rsb", [128, 2, 8], F32)
                Atok = [sb(f"Atok{i}", [128, 512], BF16) for i in range(2)]
                pS = [pp(f"pS{i}", [128, 512], F32) for i in range(2)]
                pPT = [pp(f"pPT{i}", [128, 4, 128], BF16) for i in range(2)]
                pO = [pp(f"pO{i}", [128, 128], F32) for i in range(2)]
                pAT = pp("pAT", [128, 4, 128], BF16)
                S.dma("sp", "c0", dmaf(abias[:].rearrange("p a b -> p (a b)"), abias_d), writes=["abias"])
                S.dma("sp", "c0", dmaf(esink[:], sinks_d.partition_broadcast(128)), writes=["esink"])
                S.dma("sp", "c0", dmaf(vall[:], v_s.rearrange("(t p) c -> p t c", p=128)), reads=["v_s"], writes=["vall"])
                S.op("act", lambda e: e.activation(out=esink[:], in_=esink[:], func=AF.Exp), reads=["esink"], writes=["esink"])

                def loadqk(kvh):
                    i = kvh % 2
                    S.dma("sp", f"qk{i}", dmaf(qTg[i][:], qkT_s[kvh * 512:(kvh + 1) * 512, :].rearrange("(c p) t -> p c t", p=128)),
                          reads=["qkT_s"], writes=[f"qTg{i}"])
                    S.dma("sp", f"qk{i}", dmaf(kTa[i][0:64, :], qkT_s[2048 + kvh * 64:2048 + (kvh + 1) * 64, :]), reads=["qkT_s"], writes=[f"kTg{i}"])
                    S.dma("sp", f"qk{i}", dmaf(kTb[i][64:128, :], qkT_s[2048 + kvh * 64:2048 + (kvh + 1) * 64, :]), reads=["qkT_s"], writes=[f"kTg{i}"])

                for i in range(2):
                    S.op("pool", lambda e, i=i: e.memset(kTa[i][64:128, :], 0.0), writes=[f"kTg{i}"])
                    S.op("pool", lambda e, i=i: e.memset(kTb[i][0:64, :], 0.0), writes=[f"kTg{i}"])
                loadqk(0)
                u = 0
                for kvh in range(4):
                    if kvh + 1 < 4:
                        loadqk(kvh + 1)
                    qi = kvh % 2
                    for n in range(NT):
                        ai = (kvh * NT + n) % 2
                        qc0 = 128 + n * 128
                        kc0 = n * 128
                        for hp in range(4):
                            h0 = kvh * 8 + 2 * hp
                            ui = u % 2
                            u += 1
                            S.op("pe", mm(pS[ui][:, 0:256], qTg[qi][:, hp, qc0:qc0 + 128], kTa[qi][:, kc0:kc0 + 256], True, True),
                                 reads=[f"qTg{qi}", f"kTg{qi}"], writes=[f"pS{ui}"], inc=False)
                            S.op("pe", mm(pS[ui][:, 256:512], qTg[qi][:, hp, qc0:qc0 + 128], kTb[qi][:, kc0:kc0 + 256], True, True),
                                 reads=[f"qTg{qi}", f"kTg{qi}"], writes=[f"pS{ui}"], inc=True)
                            S.op("dve", lambda e, ui=ui, h0=h0: e.tensor_tensor(out=ssb[ui][:].rearrange("p (a b) -> p a b", a=2), in0=pS[ui][:].rearrange("p (a b) -> p a b", a=2), in1=abias[:, h0:h0 + 2, :], op=ALU.add),
                                 reads=[f"pS{ui}", "abias"], writes=[f"ssb{ui}"])
                            if n == 0:
                                S.op("dve", lambda e, ui=ui: e.tensor_scalar(out=ssb[ui][:].rearrange("p (a b) -> p a b", a=2)[:, :, 0:128], in0=ssb[ui][:].rearrange("p (a b) -> p a b", a=2)[:, :, 0:128], scalar1=flag[:, 1:2], scalar2=None, op0=ALU.add),
                                     reads=[f"ssb{ui}", "flag"], writes=[f"ssb{ui}"])
                            for a_ in range(2):
                                S.op("act", lambda e, ui=ui, a_=a_: e.activation(out=Pb[ui][:, a_ * 256:(a_ + 1) * 256], in_=ssb[ui][:, a_ * 256:(a_ + 1) * 256], func=AF.Exp, accum_out=rsb[:, ui, a_:a_ + 1]),
                                     reads=[f"ssb{ui}"], writes=[f"Pb{ui}", f"rsb{ui}"])
                            S.op("dve", lambda e, ui=ui, h0=h0: e.tensor_tensor(out=rsb[:, ui, 2:4], in0=rsb[:, ui, 0:2], in1=esink[:, h0:h0 + 2], op=ALU.add),
                                 reads=[f"rsb{ui}", "esink"], writes=[f"rsb{ui}"])
                            S.op("dve", lambda e, ui=ui: e.reciprocal(out=rsb[:, ui, 4:6], in_=rsb[:, ui, 2:4]), reads=[f"rsb{ui}"], writes=[f"rsb{ui}"])
                            for j in range(4):
                                S.op("pe", tr(pPT[ui][:, j, :], Pb[ui][:, j * 128:(j + 1) * 128], ident[:]), reads=[f"Pb{ui}", "ident"], writes=[f"pPT{ui}"], inc=(j == 3))
                            if u % 2 == 0:
                                S.op("act", lambda e, ui=ui: e.copy(out=PT[ui][:], in_=pPT[ui][:]), reads=[f"pPT{ui}"], writes=[f"PT{ui}"])
                            else:
                                S.op("dve", lambda e, ui=ui: e.tensor_copy(out=PT[ui][:], in_=pPT[ui][:]), reads=[f"pPT{ui}"], writes=[f"PT{ui}"])
                            vs = slice(kvh * 64, (kvh + 1) * 64)
                            S.op("pe", mm(pO[ui][:, 0:64], PT[ui][:, 0, :], vall[:, n, vs], True, False), reads=[f"PT{ui}", "vall"], writes=[f"pO{ui}"], inc=False)
                            S.op("pe", mm(pO[ui][:, 0:64], PT[ui][:, 1, :], vall[:, n + 1, vs], False, True), reads=[f"PT{ui}", "vall"], writes=[f"pO{ui}"], inc=False)
                            S.op("pe", mm(pO[ui][:, 64:128], PT[ui][:, 2, :], vall[:, n, vs], True, False), reads=[f"PT{ui}", "vall"], writes=[f"pO{ui}"], inc=False)
                            S.op("pe", mm(pO[ui][:, 64:128], PT[ui][:, 3, :], vall[:, n + 1, vs], False, True), reads=[f"PT{ui}", "vall"], writes=[f"pO{ui}"], inc=True)
                            for a_ in range(2):
                                S.op("dve", lambda e, ui=ui, a_=a_, hp=hp, ai=ai: e.tensor_scalar(out=Atok[ai][:, hp * 128 + a_ * 64:hp * 128 + (a_ + 1) * 64], in0=pO[ui][:, a_ * 64:(a_ + 1) * 64], scalar1=rsb[:, ui, 4 + a_:5 + a_], scalar2=None, op0=ALU.mult),
                                     reads=[f"pO{ui}", f"rsb{ui}"], writes=[f"Atok{ai}"])
                        for j in range(4):
                            S.op("pe", tr(pAT[:, j, :], Atok[ai][:, j * 128:(j + 1) * 128], ident[:]), reads=[f"Atok{ai}", "ident"], writes=["pAT"], inc=(j == 3))
                        S.op("act", lambda e, kvh=kvh, n=n: e.copy(out=AT[:, kvh * 4:(kvh + 1) * 4, n * 128:(n + 1) * 128], in_=pAT[:]), reads=["pAT"], writes=[f"AT{n}"])
                S.barrier()

            S.set_phase(35)
            BT = ph34.enter_context(nc.sbuf_tensor(_un("BT"), [128, 16, NL], BF16))
            with ExitStack() as ph:
                sb = lambda n, s, d: ph.enter_context(nc.sbuf_tensor(_un(n), s, d))
                pp = lambda n, s, d: ph.enter_context(nc.psum_tensor(_un(n), s, d))
                wp = [sb(f"wp{i}", [128, 4, 512], BF16) for i in range(2)]
                dTg = [sb(f"dTg{i}", [128, 4, NL], BF16) for i in range(2)]
                psc = sb("psc", [128, 16], F32)
                bank = [pp(f"bq{i}", [128, 512], F32) for i in range(4)]
                S.dma("sp", "c0", dmaf(psc[:], pscale_pp), writes=["psc"])
                for g in range(4):
                    i = g % 2
                    S.dma("pool", f"wp{i}", dmaf(wp[i][:], w_pool[g].rearrange("(c p) n -> p c n", p=128)), writes=[f"wp{i}"])
                    S.dma("sp", f"dTg{i}", dmaf(dTg[i][:], dT_s[g * 512:(g + 1) * 512, :].rearrange("(c p) t -> p c t", p=128)), reads=["dT_s"], writes=[f"dTg{i}"])
                    for m in range(4):
                        for tb in range(4):
                            bi = (m * 4 + tb) % 4
                            for cc in range(4):
                                S.op("pe", mm(bank[bi][:], wp[i][:, cc, m * 128:(m + 1) * 128], dTg[i][:, cc, tb * 512:(tb + 1) * 512], cc == 0, cc == 3),
                                     reads=[f"wp{i}", f"dTg{i}"], writes=[f"bq{bi}"], inc=(cc == 3))
                            S.op("act", lambda e, bi=bi, g=g, m=m, tb=tb: e.activation(out=BT[:, g * 4 + m, tb * 512:(tb + 1) * 512], in_=bank[bi][:], func=AF.Copy, scale=psc[:, g * 4 + m:g * 4 + m + 1]),
                                 reads=[f"bq{bi}", "psc"], writes=["BT"])
                S.barrier()

            S.set_phase(4)
            with ExitStack() as ph:
                sb = lambda n, s, d: ph.enter_context(nc.sbuf_tensor(_un(n), s, d))
                pp = lambda n, s, d: ph.enter_context(nc.psum_tensor(_un(n), s, d))
                wUa = [sb(f"wUa{i}", [128, 16, 256], BF16) for i in range(2)]
                wUb = [sb(f"wUb{i}", [128, 16, 256], BF16) for i in range(2)]
                sga = [sb(f"sga{i}", [128, NL], BF16) for i in range(2)]
                sgb = [sb(f"sgb{i}", [128, NL], BF16) for i in range(2)]
                t1 = [sb(f"t1{i}", [128, 512], F32) for i in range(2)]
                t2 = [sb(f"t2{i}", [128, 512], F32) for i in range(2)]
                mx = [sb(f"mx{i}", [128, NL], BF16) for i in range(2)]
                pa = [pp(f"pa{i}", [128, 512], F32) for i in range(2)]
                pb = [pp(f"pb{i}", [128, 512], F32) for i in range(2)]
                allAT = [f"AT{n}" for n in range(NT)]

                def loadU(cb):
                    i = cb % 2
                    S.dma("pool", f"wUa{i}", dmaf(wUa[i][:], w_aup[:, cb * 256:(cb + 1) * 256].rearrange("(k p) n -> p k n", p=128)), writes=[f"wUa{i}"])
                    S.dma("pool", f"wUb{i}", dmaf(wUb[i][:], w_bup[:, cb * 256:(cb + 1) * 256].rearrange("(k p) n -> p k n", p=128)), writes=[f"wUb{i}"])

                loadU(0)
                u = 0
                for cb in range(16):
                    if cb + 1 < 16:
                        loadU(cb + 1)
                    i = cb % 2
                    for m2 in range(2):
                        M = cb * 2 + m2
                        mi = M % 2
                        S.dma("sp", f"sga{mi}", dmaf(sga[mi][:], sgT_s[M * 128:(M + 1) * 128, :]), reads=["sgT_s"], writes=[f"sga{mi}"])
                        S.dma("sp", f"sgb{mi}", dmaf(sgb[mi][:], sgT_s[D + M * 128:D + (M + 1) * 128, :]), reads=["sgT_s"], writes=[f"sgb{mi}"])
                        for tb in range(4):
                            ui = u % 2
                            u += 1
                            ts_ = slice(tb * 512, (tb + 1) * 512)
                            for kc in range(16):
                                S.op("pe", mm(pa[ui][:], wUa[i][:, kc, m2 * 128:(m2 + 1) * 128], AT[:, kc, ts_], kc == 0, kc == 15),
                                     reads=[f"wUa{i}"] + allAT[tb * 4:(tb + 1) * 4], writes=[f"pa{ui}"], inc=(kc == 15))
                            for kc in range(16):
                                S.op("pe", mm(pb[ui][:], wUb[i][:, kc, m2 * 128:(m2 + 1) * 128], BT[:, kc, ts_], kc == 0, kc == 15),
                                     reads=[f"wUb{i}", "BT"], writes=[f"pb{ui}"], inc=(kc == 15))
                            S.op("dve", lambda e, ui=ui, mi=mi, ts_=ts_: e.tensor_tensor(out=t1[ui][:], in0=pa[ui][:], in1=sga[mi][:, ts_], op=ALU.mult),
                                 reads=[f"pa{ui}", f"sga{mi}"], writes=[f"t1{ui}"])
                            S.op("dve", lambda e, ui=ui, mi=mi, ts_=ts_: e.tensor_tensor(out=t2[ui][:], in0=pb[ui][:], in1=sgb[mi][:, ts_], op=ALU.mult),
                                 reads=[f"pb{ui}", f"sgb{mi}"], writes=[f"t2{ui}"])
                            S.op("dve", lambda e, ui=ui, mi=mi, ts_=ts_: e.tensor_tensor(out=mx[mi][:, ts_], in0=t1[ui][:], in1=t2[ui][:], op=ALU.add),
                                 reads=[f"t1{ui}", f"t2{ui}"], writes=[f"mx{mi}"])
                        S.dma("sp", f"mx{mi}", dmaf(mixT_s[M * 128:(M + 1) * 128, :], mx[mi][:]), reads=[f"mx{mi}"], writes=["mixT_s"])
                S.barrier()
        if last_phase <= 4:
            S.barrier()
            return nc

        S.set_phase(5)
        with ExitStack() as ph:
            sb = lambda n, s, d: ph.enter_context(nc.sbuf_tensor(_un(n), s, d))
            pp = lambda n, s, d: ph.enter_context(nc.psum_tensor(_un(n), s, d))
            mixT = sb("mixT", [128, KC, NL], BF16)
            gate1b = sb("gate1b", [128, D], F32)
            wO = [sb(f"wO{i}", [128, KC, 256], BF16) for i in range(2)]
            xin = [sb(f"xin{i}", [128, 256], F32) for i in range(4)]
            o5 = [sb(f"o5{i}", [128, 256], F32) for i in range(4)]
            po = [pp(f"po{i}", [128, 512], F32) for i in range(4)]
            for i in range(8):
                S.dma("sp", f"mixT{i}", dmaf(mixT[:, 4 * i:4 * i + 4, :], mixT_s[4 * i * 128:(4 * i + 4) * 128, :].rearrange("(c p) t -> p c t", p=128)),
                      reads=["mixT_s"], writes=[f"mixT{i}"])
            S.dma("sp", "c1", dmaf(gate1b[:], adab[0]), reads=["adab"], writes=["gate1b"])
            allmix = [f"mixT{i}" for i in range(8)]

            def loadO(cb):
                i = cb % 2
                S.dma("pool", f"wO{i}", dmaf(wO[i][:], w_out[:, cb * 256:(cb + 1) * 256].rearrange("(k p) n -> p k n", p=128)), writes=[f"wO{i}"])

            loadO(0)
            u = 0
            for cb in range(16):
                if cb + 1 < 16:
                    loadO(cb + 1)
                i = cb % 2
                cs_ = slice(cb * 256, (cb + 1) * 256)
                for t in range(NT):
                    ui = u % 4
                    u += 1
                    S.dma("sp", f"xin{ui}", dmaf(xin[ui][:], xh[128 + t * 128:128 + (t + 1) * 128, cs_]), writes=[f"xin{ui}"])
                    for kc in range(KC):
                        S.op("pe", mm(po[ui][:, 0:256], mixT[:, kc, t * 128:(t + 1) * 128], wO[i][:, kc, :], kc == 0, kc == KC - 1),
                             reads=[f"wO{i}"] + (allmix if (cb == 0 and t == 0) else []), writes=[f"po{ui}"], inc=(kc == KC - 1))
                    S.op("dve", lambda e, ui=ui, cs_=cs_: e.tensor_tensor(out=o5[ui][:], in0=po[ui][:, 0:256], in1=gate1b[:, cs_], op=ALU.mult),
                         reads=[f"po{ui}", "gate1b"], writes=[f"o5{ui}"])
                    S.op("dve", lambda e, ui=ui: e.tensor_tensor(out=o5[ui][:], in0=o5[ui][:], in1=xin[ui][:], op=ALU.add),
                         reads=[f"o5{ui}", f"xin{ui}"], writes=[f"o5{ui}"])
                    S.dma("sp", f"o5{ui}", dmaf(x1_s[t * 128:(t + 1) * 128, cs_], o5[ui][:]), reads=[f"o5{ui}"], writes=["x1_s"])
            S.barrier()
        if last_phase <= 5:
            S.barrier()
            return nc

        S.set_phase(6)
        with ExitStack() as ph:
            sb = lambda n, s, d: ph.enter_context(nc.sbuf_tensor(_un(n), s, d))
            pp = lambda n, s, d: ph.enter_context(nc.psum_tensor(_un(n), s, d))
            g2b = sb("g2b", [128, D], F32)
            sh2b = sb("sh2b", [128, D], F32)
            x1t = [sb(f"x1t{i}", [128, D], F32) for i in range(2)]
            h2 = sb("h2", [128, D], F32)
            h2b = [sb(f"h2b{i}", [128, D], BF16) for i in range(2)]
            h2T = sb("h2T", [128, KC, 128], F32)
            wr = sb("wrt", [128, KC, 72], F32)
            rbb = sb("rbb", [128, 72], F32)
            lg = sb("lg", [128, 72], F32)
            sc = sb("sc", [128, 256], F32)
            posb = sb("posb", [128, 64], F32)
            ovt = sb("ovt", [128, 64], F32)
            tmp64 = sb("tmp64", [128, 64], F32)
            eb = sb("eb", [128, 64], BF16)
            eacc = sb("eacc", [128, 64], F32)
            eaccb = sb("eaccb", [128, 64], BF16)
            st6 = sb("st6", [128, NT, 4], F32)
            ptr = [pp(f"ptr{i}", [128, 512], F32) for i in range(2)]
            pl = pp("pl", [128, 128], F32)
            ppos = pp("ppos", [128, 64], F32)
            S.dma("sp", "c0", dmaf(g2b[:], adab[2]), reads=["adab"], writes=["g2b"])
            S.dma("sp", "c0", dmaf(sh2b[:], adab[1]), reads=["adab"], writes=["sh2b"])
            S.dma("sp", "c1", dmaf(wr[:], wr_d.rearrange("(k p) n -> p k n", p=128)), writes=["wr"])
            S.dma("sp", "c1", dmaf(rbb[:], rb_d.partition_broadcast(128)), writes=["rbb"])
            zt = sb("zt", [128, D], BF16)
            S.op("pool", lambda e: e.memset(zt[:], 0.0), writes=["zt"])
            for r in range(NE):
                S.dma("sp", "zf", dmaf(xs_buf[r * CAP:(r + 1) * CAP, :], zt[:]), reads=["zt"], writes=["xs_buf"])
            S.dma("sp", "zf", dmaf(xs_buf[ZROW:ZROW + 1, :], zt[0:1, :]), reads=["zt"], writes=["xs_buf"])
            S.op("dve", lambda e: e.memset(eacc[:], 0.0), writes=["eacc"])
            S.op("dve", lambda e: e.memset(eaccb[:], 0.0), writes=["eaccb"])
            gmax, negg, gsum, pgrp = sc[:, 0:1], sc[:, 1:2], sc[:, 2:3], sc[:, 3:4]
            m1, m2, dlt, e2, w1, w2, d1f, d2f = [sc[:, k:k + 1] for k in range(4, 12)]
            ohg, ein, oh1, msk, oh2, eg = [sc[:, 16 + 8 * k:24 + 8 * k] for k in range(6)]
            t88 = sc[:, 64:128]
            E1 = sc[:, 128:192]
            E2 = sc[:, 192:256]
            v88 = lambda ap: ap.rearrange("p (g j) -> p g j", g=8)

            def dv(fn, extra_r=(), extra_w=()):
                S.op("dve", fn, reads=["sc"] + list(extra_r), writes=["sc"] + list(extra_w))

            S.dma("sp", "x1t0", dmaf(x1t[0][:], x1_s[0:128, :]), reads=["x1_s"], writes=["x1t0"])
            for t in range(NT):
                i = t % 2
                if t + 1 < NT:
                    S.dma("sp", f"x1t{1 - i}", dmaf(x1t[1 - i][:], x1_s[(t + 1) * 128:(t + 2) * 128, :]), reads=["x1_s"], writes=[f"x1t{1 - i}"])
                S.op("act", lambda e, i=i, t=t: e.activation(out=h2b[i][:], in_=x1t[i][:], func=AF.Square, accum_out=st6[:, t, 0:1]),
                     reads=[f"x1t{i}"], writes=[f"h2b{i}", "st6"])
                S.op("act", lambda e, t=t: e.activation(out=st6[:, t, 1:2], in_=st6[:, t, 0:1], func=AF.Sqrt, scale=1.0 / D, bias=epst[:, 0:1]),
                     reads=["st6", "epst"], writes=["st6"])
                S.op("dve", lambda e, t=t: e.reciprocal(out=st6[:, t, 2:3], in_=st6[:, t, 1:2]), reads=["st6"], writes=["st6"])
                S.op("dve", lambda e, i=i, t=t: e.scalar_tensor_tensor(out=h2[:], in0=x1t[i][:], scalar=st6[:, t, 2:3], in1=g2b[:], op0=ALU.mult, op1=ALU.mult),
                     reads=[f"x1t{i}", "st6", "g2b"], writes=["h2"])
                S.op("pool", lambda e: e.tensor_tensor(out=h2[:], in0=h2[:], in1=sh2b[:], op=ALU.add), reads=["h2", "sh2b"], writes=["h2"])
                S.op("act", lambda e, i=i: e.copy(out=h2b[i][:], in_=h2[:]), reads=["h2"], writes=[f"h2b{i}"])
                for g4 in range(8):
                    pi = g4 % 2
                    for j in range(4):
                        kc = g4 * 4 + j
                        S.op("pe", tr(ptr[pi][:, j * 128:(j + 1) * 128], h2[:, kc * 128:(kc + 1) * 128], identf), reads=["h2", "cst"], writes=[f"ptr{pi}"], inc=(j == 3))
                    if g4 % 2 == 0:
                        S.op("act", lambda e, pi=pi, g4=g4: e.copy(out=h2T[:, g4 * 4:(g4 + 1) * 4, :].rearrange("p a b -> p (a b)"), in_=ptr[pi][:]), reads=[f"ptr{pi}"], writes=["h2T"])
                    else:
                        S.op("dve", lambda e, pi=pi, g4=g4: e.tensor_copy(out=h2T[:, g4 * 4:(g4 + 1) * 4, :].rearrange("p a b -> p (a b)"), in_=ptr[pi][:]), reads=[f"ptr{pi}"], writes=["h2T"])
                for kc in range(KC):
                    S.op("pe", mm(pl[:, 0:72], h2T[:, kc, :], wr[:, kc, :], kc == 0, kc == KC - 1), reads=["h2T", "wr"], writes=["pl"], inc=(kc == KC - 1))
                S.op("dve", lambda e: e.tensor_tensor(out=lg[:], in0=pl[:, 0:72], in1=rbb[:], op=ALU.add), reads=["pl", "rbb"], writes=["lg"])
                dv(lambda e: e.reduce_max(out=gmax, in_=lg[:, 0:8], axis=AX.X), ["lg"])
                dv(lambda e: e.tensor_scalar(out=ohg, in0=lg[:, 0:8], scalar1=gmax, scalar2=None, op0=ALU.is_equal), ["lg"])
                dv(lambda e: e.tensor_scalar(out=negg, in0=gmax, scalar1=-1.0, scalar2=None, op0=ALU.mult))
                S.op("act", lambda e: e.activation(out=eg, in_=lg[:, 0:8], func=AF.Exp, bias=negg, accum_out=gsum), reads=["sc", "lg"], writes=["sc"])
                dv(lambda e: e.reciprocal(out=pgrp, in_=gsum))
                dv(lambda e: e.tensor_tensor(out=v88(t88), in0=v88(lg[:, 8:72]), in1=ohg.unsqueeze(2).to_broadcast([128, 8, 8]), op=ALU.mult), ["lg"])
                dv(lambda e: e.tensor_reduce(out=ein, in_=t88.rearrange("p (g j) -> p j g", g=8), axis=AX.X, op=ALU.add))
                dv(lambda e: e.reduce_max(out=m1, in_=ein, axis=AX.X))
                dv(lambda e: e.tensor_scalar(out=oh1, in0=ein, scalar1=m1, scalar2=None, op0=ALU.is_equal))
                dv(lambda e: e.scalar_tensor_tensor(out=msk, in0=oh1, scalar=-1e30, in1=ein, op0=ALU.mult, op1=ALU.add))
                dv(lambda e: e.reduce_max(out=m2, in_=msk, axis=AX.X))
                dv(lambda e: e.tensor_scalar(out=oh2, in0=msk, scalar1=m2, scalar2=None, op0=ALU.is_equal))
                dv(lambda e: e.tensor_tensor(out=dlt, in0=m2, in1=m1, op=ALU.subtract))
                S.op("act", lambda e: e.activation(out=e2, in_=dlt, func=AF.Exp), reads=["sc"], writes=["sc"])
                dv(lambda e: e.tensor_scalar(out=w1, in0=e2, scalar1=1.0, scalar2=None, op0=ALU.add))
                dv(lambda e: e.reciprocal(out=w1, in_=w1))
                dv(lambda e: e.tensor_tensor(out=w2, in0=e2, in1=w1, op=ALU.mult))
                dv(lambda e, t=t: e.tensor_scalar(out=cw[:, t, 0:1], in0=w1, scalar1=pgrp, scalar2=None, op0=ALU.mult), [], ["cw"])
                dv(lambda e, t=t: e.tensor_scalar(out=cw[:, t, 1:2], in0=w2, scalar1=pgrp, scalar2=None, op0=ALU.mult), [], ["cw"])
                dv(lambda e: e.tensor_tensor(out=v88(E1), in0=ohg.unsqueeze(2).to_broadcast([128, 8, 8]), in1=oh1.unsqueeze(1).to_broadcast([128, 8, 8]), op=ALU.mult))
                dv(lambda e: e.tensor_tensor(out=v88(E2), in0=ohg.unsqueeze(2).to_broadcast([128, 8, 8]), in1=oh2.unsqueeze(1).to_broadcast([128, 8, 8]), op=ALU.mult))
                dv(lambda e: e.tensor_tensor(out=eb[:], in0=E1, in1=E2, op=ALU.add), [], ["eb"])
                S.op("pe", mm(ppos[:], tri[:], eb[:], True, False), reads=["tri", "eb"], writes=["ppos"], inc=False)
                S.op("pe", mm(ppos[:], ones[:], eaccb[:], False, True), reads=["ones", "eaccb"], writes=["ppos"], inc=True)
                S.op("dve", lambda e: e.tensor_tensor(out=posb[:], in0=ppos[:], in1=ebase, op=ALU.add), reads=["ppos", "cst"], writes=["posb"])
                S.op("dve", lambda e: e.tensor_scalar(out=ovt[:], in0=ppos[:], scalar1=float(CAP), scalar2=None, op0=ALU.is_ge), reads=["ppos"], writes=["ovt"])
                S.op("dve", lambda e: e.tensor_scalar(out=tmp64[:], in0=posb[:], scalar1=-1.0, scalar2=float(ZROW), op0=ALU.mult, op1=ALU.add), reads=["posb"], writes=["tmp64"])
                S.op("dve", lambda e: e.tensor_tensor(out=tmp64[:], in0=tmp64[:], in1=ovt[:], op=ALU.mult), reads=["tmp64", "ovt"], writes=["tmp64"])
                S.op("dve", lambda e: e.tensor_tensor(out=posb[:], in0=posb[:], in1=tmp64[:], op=ALU.add), reads=["posb", "tmp64"], writes=["posb"])
                dv(lambda e: e.tensor_tensor(out=t88, in0=E1, in1=posb[:], op=ALU.mult), ["posb"])
                dv(lambda e: e.reduce_sum(out=d1f, in_=t88, axis=AX.X))
                dv(lambda e: e.tensor_tensor(out=t88, in0=E2, in1=posb[:], op=ALU.mult), ["posb"])
                dv(lambda e: e.reduce_sum(out=d2f, in_=t88, axis=AX.X))
                dv(lambda e, t=t: e.tensor_copy(out=di[:, t, 0:1], in_=d1f), [], ["di"])
                dv(lambda e, t=t: e.tensor_copy(out=di[:, t, 1:2], in_=d2f), [], ["di"])
                S.op("dve", lambda e: e.tensor_tensor(out=eacc[:], in0=eacc[:], in1=eb[:], op=ALU.add), reads=["eacc", "eb"], writes=["eacc"])
                S.op("dve", lambda e: e.tensor_copy(out=eaccb[:], in_=eacc[:]), reads=["eacc"], writes=["eaccb"])
                for k2 in range(2):
                    S.dma("pool", f"scat{k2}", lambda e, t=t, k2=k2, i=i: e.indirect_dma_start(
                        out=xs_buf, out_offset=bass.IndirectOffsetOnAxis(ap=di[:, t, k2:k2 + 1], axis=0),
                        in_=h2b[i][:], in_offset=None, bounds_check=bcreg, oob_is_err=False),
                        reads=[f"h2b{i}", "di"], writes=["xs_buf"])
            if "dbg_s" in debug:
                S.dma("sp", "c0", dmaf(dbg_s[:, 0:32], cw[:].rearrange("p a b -> p (a b)")), reads=["cw"], writes=["dbg"])
            S.barrier()
        if last_phase <= 6:
            S.barrier()
            return nc

        S.set_phase(7)
        with ExitStack() as ph:
            sb = lambda n, s, d: ph.enter_context(nc.sbuf_tensor(_un(n), s, d))
            pp = lambda n, s, d: ph.enter_context(nc.psum_tensor(_un(n), s, d))
            NR = 14
            ring = [sb(f"ring{i}", [128, D], BF16) for i in range(NR)]
            xs = [sb(f"xs{i}", [128, D], BF16) for i in range(2)]
            xsT = [sb(f"xsT{i}", [128, KC, 128], BF16) for i in range(2)]
            sg = sb("sg", [128, 512], F32)
            act = sb("actb", [128, 512], BF16)
            actT = sb("actT", [128, 4, 128], BF16)
            yst = [sb(f"yst{i}", [128, D], F32) for i in range(2)]
            zrow = sb("zrow", [1, D], F32)
            pxT = [pp(f"pxT{i}", [128, 8, 128], BF16) for i in range(2)]
            pg = pp("pg", [128, 512], F32)
            pu = pp("pu", [128, 512], F32)
            paT = pp("paT", [128, 4, 128], BF16)
            py = [pp(f"py{i}", [128, 512], F32) for i in range(2)]
            S.op("dve", lambda e: e.memset(zrow[:], 0.0), writes=["zrow"])
            S.dma("sp", "c0", dmaf(y_s[ZROW:ZROW + 1, :], zrow[:]), reads=["zrow"], writes=["y_s"])
            units = []
            for e_ in range(NE):
                for kg in range(4):
                    units.append(("g", e_, kg))
                    units.append(("u", e_, kg))
                for db in range(4):
                    units.append(("d", e_, db))
            nload = [0]

            def ensure_loaded(upto):
                while nload[0] < min(len(units), upto):
                    j = nload[0]
                    kind, e_, k_ = units[j]
                    s_ = j % NR
                    if kind == "g":
                        src = w_gate[e_][k_ * 1024:(k_ + 1) * 1024, :].rearrange("(c p) n -> p c n", p=128)
                        dstv = ring[s_][:].rearrange("p (c n) -> p c n", c=8)
                    elif kind == "u":
                        src = w_up[e_][k_ * 1024:(k_ + 1) * 1024, :].rearrange("(c p) n -> p c n", p=128)
                        dstv = ring[s_][:].rearrange("p (c n) -> p c n", c=8)
                    else:
                        src = w_down[e_][:, k_ * 1024:(k_ + 1) * 1024].rearrange("(c p) n -> p c n", p=128)
                        dstv = ring[s_][:].rearrange("p (c n) -> p c n", c=4)
                    S.dma("pool", f"ring{s_}", dmaf(dstv, src), writes=[f"ring{s_}"])
                    nload[0] += 1

            ui = 0
            cpy = 0
            for e_ in range(NE):
                i = e_ % 2
                S.dma("sp", f"xs{i}", dmaf(xs[i][:], xs_buf[e_ * CAP:(e_ + 1) * CAP, :]), reads=["xs_buf"], writes=[f"xs{i}"])
                for g8 in range(4):
                    pi = g8 % 2
                    for j in range(8):
                        kc = g8 * 8 + j
                        S.op("pe", tr(pxT[pi][:, j, :], xs[i][:, kc * 128:(kc + 1) * 128], ident[:]), reads=[f"xs{i}", "ident"], writes=[f"pxT{pi}"], inc=(j == 7))
                    if g8 % 2 == 0:
                        S.op("act", lambda e, pi=pi, g8=g8, i=i: e.copy(out=xsT[i][:, g8 * 8:(g8 + 1) * 8, :], in_=pxT[pi][:]), reads=[f"pxT{pi}"], writes=[f"xsT{i}"])
                    else:
                        S.op("dve", lambda e, pi=pi, g8=g8, i=i: e.tensor_copy(out=xsT[i][:, g8 * 8:(g8 + 1) * 8, :], in_=pxT[pi][:]), reads=[f"pxT{pi}"], writes=[f"xsT{i}"])
                for kg in range(4):
                    for kind, pacc, pk in (("g", pg, "pg"), ("u", pu, "pu")):
                        ensure_loaded(ui + NR)
                        s_ = ui % NR
                        assert units[ui] == (kind, e_, kg)
                        wv = ring[s_][:].rearrange("p (c n) -> p c n", c=8)
                        for j in range(8):
                            kc = kg * 8 + j
                            S.op("pe", mm(pacc[:], xsT[i][:, kc, :], wv[:, j, :], kc == 0, kc == KC - 1),
                                 reads=[f"ring{s_}", f"xsT{i}"], writes=[pk], inc=(j == 7))
                        ui += 1
                S.op("act", lambda e: e.activation(out=sg[:], in_=pg[:], func=AF.Silu), reads=["pg"], writes=["sg"])
                S.op("dve", lambda e: e.tensor_tensor(out=act[:], in0=sg[:], in1=pu[:], op=ALU.mult), reads=["sg", "pu"], writes=["act"])
                for j in range(4):
                    S.op("pe", tr(paT[:, j, :], act[:, j * 128:(j + 1) * 128], ident[:]), reads=["act", "ident"], writes=["paT"], inc=(j == 3))
                S.op("act", lambda e: e.copy(out=actT[:], in_=paT[:]), reads=["paT"], writes=["actT"])
                for db in range(4):
                    ensure_loaded(ui + NR)
                    s_ = ui % NR
                    assert units[ui] == ("d", e_, db)
                    wv = ring[s_][:].rearrange("p (c n) -> p c n", c=4)
                    for dj in range(2):
                        yi = cpy % 2
                        cpy += 1
                        for fc in range(4):
                            S.op("pe", mm(py[yi][:], actT[:, fc, :], wv[:, fc, dj * 512:(dj + 1) * 512], fc == 0, fc == 3),
                                 reads=[f"ring{s_}", "actT"], writes=[f"py{yi}"], inc=(fc == 3))
                        c0 = db * 1024 + dj * 512
                        if yi == 0:
                            S.op("act", lambda e, yi=yi, i=i, c0=c0: e.copy(out=yst[i][:, c0:c0 + 512], in_=py[yi][:]), reads=[f"py{yi}"], writes=[f"yst{i}"])
                        else:
                            S.op("dve", lambda e, yi=yi, i=i, c0=c0: e.tensor_copy(out=yst[i][:, c0:c0 + 512], in_=py[yi][:]), reads=[f"py{yi}"], writes=[f"yst{i}"])
                    ui += 1
                S.dma("sp", f"yst{i}", dmaf(y_s[e_ * CAP:(e_ + 1) * CAP, :], yst[i][:]), reads=[f"yst{i}"], writes=["y_s"])
            S.barrier()
        if last_phase <= 7:
            S.barrier()
            return nc

        S.set_phase(8)
        with ExitStack() as ph:
            sb = lambda n, s, d: ph.enter_context(nc.sbuf_tensor(_un(n), s, d))
            gate2b = sb("gate2b", [128, D], F32)
            Y1 = [sb(f"Y1{i}", [128, D], F32) for i in range(2)]
            Y2 = [sb(f"Y2{i}", [128, D], F32) for i in range(2)]
            x1t = [sb(f"x1u{i}", [128, D], F32) for i in range(2)]
            acc = sb("acc8", [128, D], F32)
            o8 = [sb(f"o8{i}", [128, D], F32) for i in range(2)]
            S.dma("sp", "c0", dmaf(gate2b[:], adab[3]), reads=["adab"], writes=["gate2b"])
            for t in range(NT):
                i = t % 2
                S.dma("pool", f"Y1{i}", lambda e, t=t, i=i: e.indirect_dma_start(
                    out=Y1[i][:], out_offset=None, in_=y_s, in_offset=bass.IndirectOffsetOnAxis(ap=di[:, t, 0:1], axis=0),
                    bounds_check=bcreg, oob_is_err=False), reads=["y_s", "di"], writes=[f"Y1{i}"])
                S.dma("pool", f"Y2{i}", lambda e, t=t, i=i: e.indirect_dma_start(
                    out=Y2[i][:], out_offset=None, in_=y_s, in_offset=bass.IndirectOffsetOnAxis(ap=di[:, t, 1:2], axis=0),
                    bounds_check=bcreg, oob_is_err=False), reads=["y_s", "di"], writes=[f"Y2{i}"])
                S.dma("sp", f"x1u{i}", dmaf(x1t[i][:], x1_s[t * 128:(t + 1) * 128, :]), reads=["x1_s"], writes=[f"x1u{i}"])
                S.op("act", lambda e, t=t, i=i: e.activation(out=acc[:], in_=Y1[i][:], func=AF.Copy, scale=cw[:, t, 0:1]), reads=[f"Y1{i}", "cw"], writes=["acc"])
                S.op("dve", lambda e, t=t, i=i: e.scalar_tensor_tensor(out=acc[:], in0=Y2[i][:], scalar=cw[:, t, 1:2], in1=acc[:], op0=ALU.mult, op1=ALU.add),
                     reads=[f"Y2{i}", "cw", "acc"], writes=["acc"])
                S.op("dve", lambda e: e.tensor_tensor(out=acc[:], in0=acc[:], in1=gate2b[:], op=ALU.mult), reads=["acc", "gate2b"], writes=["acc"])
                S.op("dve", lambda e, i=i: e.tensor_tensor(out=o8[i][:], in0=acc[:], in1=x1t[i][:], op=ALU.add), reads=["acc", f"x1u{i}"], writes=[f"o8{i}"])
                S.dma("sp", f"o8{i}", dmaf(out_d[t * 128:(t + 1) * 128, :], o8[i][:]), reads=[f"o8{i}"], writes=["out"])
            S.barrier()
    nc._sched = S
    return nc


def _prep(inputs):
    x = np.asarray(inputs["x"], np.float32)
    c = np.asarray(inputs["c"], np.float32)
    g = lambda k: np.asarray(inputs[k], np.float32)[0]
    shared = {}
    shared["w_ada"] = g("w_ada")
    b_ada = g("b_ada")
    shared["b_ada"] = b_ada
    shared["bada_pp"] = np.ascontiguousarray(b_ada[:8192].reshape(64, 128).T)
    shared["n1w_pp"] = np.ascontiguousarray(g("norm1_w").reshape(32, 128).T)
    shared["n2w"] = g("norm2_w")
    shared["w_in"] = g("w_in")
    qw = np.tile(g("q_norm_w"), 4)
    kw = np.tile(g("k_norm_w"), 4)
    shared["qkw_rep"] = np.ascontiguousarray(np.broadcast_to(np.concatenate([qw, kw])[None, :], (128, 512)))
    shared["sinks"] = g("sinks")
    slopes = (2.0 ** (-8.0 * np.arange(1, 33, dtype=np.float32) / 32)).astype(np.float32)
    qi = np.arange(128)[:, None]
    kj = np.arange(256)[None, :]
    dist = qi + 128 - kj
    valid = (dist >= 0) & (dist < 128)
    ab = np.where(valid[None], -slopes[:, None, None] * dist[None].astype(np.float32), np.float32(NEG)).astype(np.float32)
    shared["abias"] = np.ascontiguousarray(ab.transpose(1, 0, 2).reshape(128, 32 * 256))
    shared["w_pool"] = g("w_pool")
    shared["pscale_pp"] = np.ascontiguousarray(g("pool_scale").reshape(16, 128).T)
    shared["w_aup"] = g("w_attn_up")
    shared["w_bup"] = g("w_pool_up")
    shared["w_out"] = g("w_out")
    shared["wr"] = np.ascontiguousarray(np.concatenate([g("w_router_group"), g("w_router_expert")], axis=1))
    shared["rb"] = np.concatenate([g("b_router_group"), g("b_router_expert")])
    shared["w_gate"] = g("w_gate")
    shared["w_up"] = g("w_up")
    shared["w_down"] = g("w_down")
    cst = np.zeros((128, 448), np.float32)
    cst[:, 0:128] = np.eye(128, dtype=np.float32)
    cst[:, 128:256] = (np.arange(128)[:, None] < np.arange(128)[None, :]).astype(np.float32)
    cst[:, 256:384] = 1.0
    cst[:, 384:448] = (np.arange(64) * CAP)[None, :].astype(np.float32)
    shared["consts"] = cst
    in_maps = []
    for i in range(8):
        b = i // 4
        s0 = (i % 4) * NL
        m = dict(shared)
        if s0 == 0:
            halo = np.zeros((HALO, D), np.float32)
        else:
            halo = x[b, s0 - HALO:s0]
        m["xh"] = np.ascontiguousarray(np.concatenate([halo, x[b, s0:s0 + NL]], axis=0))
        m["cpp"] = np.ascontiguousarray(c[b].reshape(32, 128).T)
        fl = np.zeros((128, 2), np.float32)
        fl[:, 0] = 0.0 if s0 == 0 else 1.0
        fl[:, 1] = NEG if s0 == 0 else 0.0
        m["flag"] = fl
        invc = np.zeros((128, 64), np.float32)
        for gi, w in enumerate((2, 4, 8, 16)):
            tpos = s0 + np.arange(16) + 1
            invc[:, gi * 16:(gi + 1) * 16] = (1.0 / np.minimum(tpos, w)).astype(np.float32)[None, :]
        m["invc"] = invc
        in_maps.append(m)
    return in_maps


_NC_CACHE = {}


def kernel(**inputs):
    in_maps = _prep(inputs)
    if "nc" not in _NC_CACHE:
        _NC_CACHE["nc"] = build_nc()
    nc = _NC_CACHE["nc"]
    res = run_bass_kernel_spmd(nc, in_maps, core_ids=list(range(8)))
    out = np.empty((2, 4 * NL, D), np.float32)
    for i in range(8):
        b = i // 4
        s0 = (i % 4) * NL
        out[b, s0:s0 + NL] = res.results[i]["out"]
    return out
```

```python
import numpy as np
from contextlib import ExitStack
import concourse.bass as bass
import concourse.mybir as mybir
from concourse.bass_utils import run_bass_kernel_spmd

F32 = mybir.dt.float32
BF16 = mybir.dt.bfloat16
I32 = mybir.dt.int32
ALU = mybir.AluOpType
AF = mybir.ActivationFunctionType
AX = mybir.AxisListType

D = 4096
KC = 32
NL = 2048
NT = 16
HALO = 128
NTOK = NL + HALO
TT = 17
CAP = 128
NE = 64
NSLOT = NE * CAP
ZROW = NSLOT
EPS = 1e-6
NEG = -30000.0
INW = 12800


class Sched:
    def __init__(self, nc, stack):
        self.nc = nc
        self.stack = stack
        self.esem = {}
        self.cnt = {e: 0 for e in ["pe", "act", "dve", "pool", "sp"]}
        for e in ["pe", "act", "dve", "pool"]:
            self.esem[e] = stack.enter_context(nc.semaphore("c_" + e))
        self.dpool = []
        self.dkey = {}
        self.dfree = []
        self.res = {}
        self.waited = {e: {} for e in ["pe", "act", "dve", "pool", "sp"]}
        self.pending = {e: [] for e in ["pe", "act", "dve", "pool", "sp"]}
        self.sems = {}
        self.trace = {}
        self.mute = False
        self.only = None

    def set_phase(self, k):
        self.mute = (self.only is not None) and (k not in self.only)

    def _dma_sem(self, key):
        if key not in self.dkey:
            if self.dfree:
                idx = self.dfree.pop()
            else:
                idx = len(self.dpool)
                sem = self.stack.enter_context(self.nc.semaphore(f"d{idx}"))
                self.dpool.append([sem, 0])
                self.sems[f"d#{idx}"] = sem
            self.dkey[key] = idx
        return self.dkey[key]

    def _deps(self, eng, reads, writes):
        toks = {}

        def add(t):
            if t is None:
                return
            n, v = t
            if toks.get(n, 0) < v:
                toks[n] = v

        for k in reads:
            r = self.res.get(k)
            if r:
                add(r[0])
        for k in writes:
            r = self.res.get(k)
            if r:
                add(r[0])
                for t in r[1]:
                    add(t)
        out = []
        for n, v in toks.items():
            if eng == "pe" and n == "c_pe":
                continue
            if self.waited[eng].get(n, 0) >= v:
                continue
            self.waited[eng][n] = v
            out.append((n, v))
        return out

    def _record(self, tok, reads, writes):
        for k in reads:
            r = self.res.setdefault(k, [None, []])
            r[1].append(tok)
            if len(r[1]) > 32:
                mm = {}
                for n, v in r[1]:
                    mm[n] = max(mm.get(n, 0), v)
                r[1] = list(mm.items())
        for k in writes:
            self.res[k] = [tok, []]

    def op(self, eng, fn, reads=(), writes=(), inc=True):
        if self.mute:
            return
        reads = list(reads)
        writes = list(writes)
        deps = self._deps(eng, reads, writes)
        if inc:
            self.cnt[eng] += 1
            tok = ("c_" + eng, self.cnt[eng])
            pend = self.pending[eng]
            self.pending[eng] = []
            for (pr, pw) in pend:
                self._record(tok, pr, pw)
            self._record(tok, reads, writes)
        else:
            self.pending[eng].append((reads, writes))
        self._emit(eng, deps, fn, inc)

    def dma(self, eng, key, fn, reads=(), writes=()):
        if self.mute:
            return
        reads = list(reads)
        writes = list(writes)
        deps = self._deps(eng, reads, writes)
        idx = self._dma_sem(key)
        self.dpool[idx][1] += 16
        tok = (f"d#{idx}", self.dpool[idx][1])
        self._record(tok, reads, writes)
        self._emit(eng, deps, fn, ("dma", idx))

    def barrier(self):
        for e in ["pe", "act", "dve", "pool"]:
            assert not self.pending[e], f"pending accesses without inc on {e}"
        toks = [("c_" + e, self.cnt[e]) for e in ["pe", "act", "dve", "pool"] if self.cnt[e] > 0]
        toks += [(f"d#{i}", v[1]) for i, v in enumerate(self.dpool) if v[1] > 0]
        for e in ["pe", "act", "dve", "pool", "sp"]:
            deps = []
            for n, v in toks:
                if self.waited[e].get(n, 0) >= v:
                    continue
                self.waited[e][n] = v
                deps.append((n, v))
            if deps:
                self._emit(e, deps, None, False)
        self.dkey = {}
        self.dfree = list(range(len(self.dpool)))

    def _engobj(self, e):
        nc = self.nc
        return {"pe": nc.tensor, "act": nc.scalar, "dve": nc.vector, "pool": nc.gpsimd, "sp": nc.sync}[e]

    def _semh(self, n):
        if n.startswith("c_"):
            return self.esem[n[2:]]
        return self.sems[n]

    def _emit(self, engname, deps, fn, inc):
        eng = self._engobj(engname)
        tr_ = self.trace.setdefault(engname, [])
        for n, v in deps:
            eng.wait_ge(self._semh(n), v)
            tr_.append(("w", n, v))
        if fn is None:
            return
        ins = fn(eng)
        if inc is True:
            ins.then_inc(self.esem[engname], 1)
            tr_.append(("i", "c_" + engname, 1))
        elif isinstance(inc, tuple):
            ins.then_inc(self.dpool[inc[1]][0], 16)
            tr_.append(("i", f"d#{inc[1]}", 16))
        else:
            tr_.append(("n",))

    def simulate(self):
        val = {}
        pc = {e: 0 for e in self.trace}
        progress = True
        while progress:
            progress = False
            for e, t in self.trace.items():
                while pc[e] < len(t):
                    op = t[pc[e]]
                    if op[0] == "w":
                        if val.get(op[1], 0) < op[2]:
                            break
                    elif op[0] == "i":
                        val[op[1]] = val.get(op[1], 0) + op[2]
                    pc[e] += 1
                    progress = True
        stuck = {e: (pc[e], len(t), t[pc[e]]) for e, t in self.trace.items() if pc[e] < len(t)}
        return stuck, val


def mm(out, lhsT, rhs, st, sp):
    return lambda e: e.matmul(out, lhsT=lhsT, rhs=rhs, start=st, stop=sp)


def tr(out, in_, ident):
    return lambda e: e.transpose(out=out, in_=in_, identity=ident)


def dmaf(out, in_):
    return lambda e: e.dma_start(out=out, in_=in_)


def build_nc(debug=(), last_phase=99, only=None):
    nc = bass.Bass("TRN2", target_bir_lowering=False)

    def din(name, shape, dt=F32):
        return nc.dram_tensor(name, list(shape), dt, kind="ExternalInput").ap()

    def dscr(name, shape, dt):
        if name in debug:
            return nc.dram_tensor(name, list(shape), dt, kind="ExternalOutput").ap()
        return nc.dram_tensor(name, list(shape), dt).ap()

    xh = din("xh", [NTOK, D])
    cpp = din("cpp", [128, 32])
    flag_d = din("flag", [128, 2])
    invc_d = din("invc", [128, 64])
    w_ada = din("w_ada", [D, 6 * D])
    b_ada = din("b_ada", [6 * D])
    bada_pp = din("bada_pp", [128, 64])
    n1w_pp = din("n1w_pp", [128, 32])
    n2w = din("n2w", [D])
    w_in = din("w_in", [D, INW])
    qkw_rep = din("qkw_rep", [128, 512])
    sinks_d = din("sinks", [32])
    abias_d = din("abias", [128, 32 * 256])
    w_pool = din("w_pool", [4, 512, 512])
    pscale_pp = din("pscale_pp", [128, 16])
    w_aup = din("w_aup", [2048, D])
    w_bup = din("w_bup", [2048, D])
    w_out = din("w_out", [D, D])
    wr_d = din("wr", [D, 72])
    rb_d = din("rb", [72])
    w_gate = din("w_gate", [NE, D, 512])
    w_up = din("w_up", [NE, D, 512])
    w_down = din("w_down", [NE, 512, D])
    consts_d = din("consts", [128, 128 * 3 + 64])
    out_d = nc.dram_tensor("out", [NL, D], F32, kind="ExternalOutput").ap()

    adab = dscr("adab", [4, 128, D], F32)
    qkT_s = dscr("qkT_s", [2048 + 256, NTOK], BF16)
    v_s = dscr("v_s", [NTOK, 256], BF16)
    dT_s = dscr("dT_s", [2048, NL], BF16)
    sgT_s = dscr("sgT_s", [2 * D, NL], BF16)
    mixT_s = dscr("mixT_s", [D, NL], BF16)
    x1_s = dscr("x1_s", [NL, D], F32)
    xs_buf = dscr("xs_buf", [NSLOT + 1, D], BF16)
    y_s = dscr("y_s", [NSLOT + 1, D], F32)
    dbg_s = dscr("dbg_s", [128, NT * 8], F32)

    _uid = [0]

    def _un(n):
        _uid[0] += 1
        return f"{n}_u{_uid[0]}"

    with ExitStack() as top:
        S = Sched(nc, top)
        S.only = only
        sbt = lambda n, s, d: top.enter_context(nc.sbuf_tensor(_un(n), s, d))
        cst = sbt("cst", [128, 128 * 3 + 64], F32)
        identf = cst[:, 0:128]
        ident = sbt("ident", [128, 128], BF16)
        tri = sbt("tri", [128, 128], BF16)
        ones = sbt("ones", [128, 128], BF16)
        ebase = cst[:, 384:448]
        flag = sbt("flagt", [128, 2], F32)
        g1 = sbt("g1", [128, 32], F32)
        sh1 = sbt("sh1", [128, 32], F32)
        epst = sbt("epst", [128, 1], F32)
        cw = sbt("cw", [128, NT, 2], F32)
        di = sbt("di", [128, NT, 2], I32)

        bcreg = nc.gpsimd.alloc_register("bcreg")
        nc.gpsimd.reg_mov(bcreg, NSLOT)
        S.dma("sp", "c0", dmaf(cst[:], consts_d), writes=["cst"])
        S.dma("sp", "c0", dmaf(flag[:], flag_d), writes=["flag"])
        S.op("dve", lambda e: e.tensor_copy(out=ident[:], in_=cst[:, 0:128]), reads=["cst"], writes=["ident"])
        S.op("dve", lambda e: e.tensor_copy(out=tri[:], in_=cst[:, 128:256]), reads=["cst"], writes=["tri"])
        S.op("dve", lambda e: e.tensor_copy(out=ones[:], in_=cst[:, 256:384]), reads=["cst"], writes=["ones"])
        S.op("dve", lambda e: e.memset(epst[:], EPS), writes=["epst"])

        S.set_phase(0)
        with ExitStack() as ph:
            sb = lambda n, s, d: ph.enter_context(nc.sbuf_tensor(_un(n), s, d))
            pp = lambda n, s, d: ph.enter_context(nc.psum_tensor(_un(n), s, d))
            ct = sb("ct", [128, 32], F32)
            cs = sb("cs", [128, 32], BF16)
            wA = [sb(f"wA{i}", [128, 32, 512], BF16) for i in range(2)]
            bpp = sb("bpp", [128, 64], F32)
            n1pp = sb("n1pp", [128, 32], F32)
            adaT = sb("adaT", [128, 64], F32)
            pTa = pp("pTa", [128, 64], F32)

            S.dma("sp", "c0", dmaf(ct[:], cpp), writes=["ct"])
            S.dma("sp", "c0", dmaf(bpp[:], bada_pp), writes=["bpp"])
            S.dma("sp", "c0", dmaf(n1pp[:], n1w_pp), writes=["n1pp"])
            S.op("act", lambda e: e.activation(out=cs[:], in_=ct[:], func=AF.Silu), reads=["ct"], writes=["cs"])

            def loadA(blk):
                i = blk % 2
                S.dma("pool", f"wA{i}", dmaf(wA[i][:], w_ada[:, blk * 512:(blk + 1) * 512].rearrange("(k p) n -> p k n", p=128)),
                      writes=[f"wA{i}"])

            loadA(0)
            for blk in range(16):
                if blk + 1 < 16:
                    loadA(blk + 1)
                i = blk % 2
                for fc in range(4):
                    col = blk * 4 + fc
                    for kc in range(KC):
                        S.op("pe", mm(pTa[:, col:col + 1], wA[i][:, kc, fc * 128:(fc + 1) * 128], cs[:, kc:kc + 1], kc == 0, kc == KC - 1),
                             reads=[f"wA{i}", "cs"], writes=["pTa"], inc=(kc == KC - 1 and fc == 3))
            S.op("dve", lambda e: e.tensor_tensor(out=adaT[:], in0=pTa[:], in1=bpp[:], op=ALU.add),
                 reads=["pTa", "bpp"], writes=["adaT"])
            S.op("dve", lambda e: e.scalar_tensor_tensor(out=g1[:], in0=adaT[:, 32:64], scalar=1.0, in1=n1pp[:], op0=ALU.add, op1=ALU.mult),
                 reads=["adaT", "n1pp"], writes=["g1"])
            S.op("dve", lambda e: e.tensor_copy(out=sh1[:], in_=adaT[:, 0:32]), reads=["adaT"], writes=["sh1"])
            S.barrier()
        if last_phase <= 0:
            S.barrier()
            return nc

        S.set_phase(1)
        with ExitStack() as ph12:
            hT = ph12.enter_context(nc.sbuf_tensor(_un("hT"), [128, KC, NTOK], BF16))
            with ExitStack() as ph:
                sb = lambda n, s, d: ph.enter_context(nc.sbuf_tensor(_un(n), s, d))
                pp = lambda n, s, d: ph.enter_context(nc.psum_tensor(_un(n), s, d))
                xt = [sb(f"xt{i}", [128, D], F32) for i in range(2)]
                xb = sb("xb", [128, D], BF16)
                st = sb("st1", [128, TT, 4], F32)
                pT = [pp(f"pT{i}", [128, 8, 128], BF16) for i in range(2)]
                S.dma("sp", "xt0", dmaf(xt[0][:], xh[0:128, :]), writes=["xt0"])
                for t in range(TT):
                    i = t % 2
                    if t + 1 < TT:
                        S.dma("sp", f"xt{1 - i}", dmaf(xt[1 - i][:], xh[(t + 1) * 128:(t + 2) * 128, :]), writes=[f"xt{1 - i}"])
                    S.op("act", lambda e, i=i, t=t: e.activation(out=xb[:], in_=xt[i][:], func=AF.Square, accum_out=st[:, t, 0:1]),
                         reads=[f"xt{i}"], writes=["xb", f"st{t}"])
                    S.op("act", lambda e, t=t: e.activation(out=st[:, t, 1:2], in_=st[:, t, 0:1], func=AF.Sqrt, scale=1.0 / D, bias=epst[:, 0:1]),
                         reads=[f"st{t}", "epst"], writes=[f"st{t}"])
                    S.op("dve", lambda e, t=t: e.reciprocal(out=st[:, t, 2:3], in_=st[:, t, 1:2]), reads=[f"st{t}"], writes=[f"st{t}"])
                    S.op("act", lambda e, i=i, t=t: e.activation(out=xb[:], in_=xt[i][:], func=AF.Copy, scale=st[:, t, 2:3]),
                         reads=[f"xt{i}", f"st{t}"], writes=["xb"])
                    for g in range(4):
                        pi = (t * 4 + g) % 2
                        for j in range(8):
                            kc = g * 8 + j
                            S.op("pe", tr(pT[pi][:, j, :], xb[:, kc * 128:(kc + 1) * 128], ident[:]),
                                 reads=["xb", "ident"], writes=[f"pT{pi}"], inc=(j == 7))
                        for j in range(8):
                            kc = g * 8 + j
                            eng = "dve" if j % 2 == 0 else "act"
                            if eng == "dve":
                                S.op("dve", lambda e, pi=pi, j=j, kc=kc, t=t: e.tensor_scalar(out=hT[:, kc, t * 128:(t + 1) * 128], in0=pT[pi][:, j, :], scalar1=g1[:, kc:kc + 1], scalar2=sh1[:, kc:kc + 1], op0=ALU.mult, op1=ALU.add),
                                     reads=[f"pT{pi}", "g1", "sh1"], writes=[f"hT{t}"])
                            else:
                                S.op("act", lambda e, pi=pi, j=j, kc=kc, t=t: e.activation(out=hT[:, kc, t * 128:(t + 1) * 128], in_=pT[pi][:, j, :], func=AF.Identity, scale=g1[:, kc:kc + 1], bias=sh1[:, kc:kc + 1]),
                                     reads=[f"pT{pi}", "g1", "sh1"], writes=[f"hT{t}"])
                S.barrier()

            S.set_phase(2)
            with ExitStack() as ph:
                sb = lambda n, s, d: ph.enter_context(nc.sbuf_tensor(_un(n), s, d))
                pp = lambda n, s, d: ph.enter_context(nc.psum_tensor(_un(n), s, d))
                wB = [sb(f"wB{i}", [128, KC, 256], BF16) for i in range(2)]
                qkw = sb("qkw", [128, 512], F32)
                invc = sb("invct", [128, 64], F32)
                NS = 3
                st2 = sb("st2", [128, NS, 8], F32)
                U1 = sb("U1", [128, 2 * NTOK], BF16)
                stT = U1[:].rearrange("p (a b) -> p a b", a=2)
                vst = U1[:].rearrange("p (a b) -> p a b", a=TT)
                pb2 = U1[:].bitcast(F32)
                U2 = sb("U2", [128, NTOK], F32)
                pb1 = U2[:]
                U2K = [f"U2_{s_}" for s_ in range(NS)]
                sqs = [U2[:, s_ * 640:s_ * 640 + 256] for s_ in range(NS)]
                qns = [U2[:, s_ * 640 + 256:s_ * 640 + 512] for s_ in range(NS)]
                qbs = [U2[:, s_ * 640 + 512:s_ * 640 + 640].bitcast(BF16) for s_ in range(NS)]
                pt = sb("pt", [128, NTOK], F32)
                dst0 = sb("dst0", [128, NL], BF16)
                dst = [dst0, dst0]
                bank = [pp(f"bk{i}", [128, 512], F32) for i in range(7)]
                pTqs = pp("pTq", [128, NS, 2, 128], BF16)
                S.dma("sp", "c0", dmaf(qkw[:], qkw_rep), writes=["qkw"])
                S.dma("sp", "c0", dmaf(invc[:], invc_d), writes=["invc"])
                allhT = [f"hT{t}" for t in range(TT)]

                def loadB(b):
                    i = b % 2
                    S.dma("pool", f"wB{i}", dmaf(wB[i][:], w_in[:, b * 256:(b + 1) * 256].rearrange("(k p) n -> p k n", p=128)),
                          writes=[f"wB{i}"])

                NBLK = INW // 256
                loadB(0)
                ecnt = 0
                for b in range(NBLK):
                    if b + 1 < NBLK:
                        loadB(b + 1)
                    i = b % 2
                    wk = f"wB{i}"
                    if b < 10:
                        for t in range(TT):
                            bi = t % 3
                            pq = bank[bi]
                            for kc in range(KC):
                                S.op("pe", mm(pq[:, 0:256], hT[:, kc, t * 128:(t + 1) * 128], wB[i][:, kc, :], kc == 0, kc == KC - 1),
                                     reads=[wk, f"hT{t}"], writes=[f"bk{bi}"], inc=(kc == KC - 1))
                            if b == 9:
                                S.op("act", lambda e, pq=pq, t=t: e.copy(out=vst[:, t, :], in_=pq[:, 0:256]), reads=[f"bk{bi}"], writes=["U1"])
                                continue
                            wsl = qkw[:, 0:256] if b < 8 else qkw[:, 256:512]
                            si = ecnt % NS
                            ecnt += 1
                            sq, qn, qbt, uk, sk = sqs[si], qns[si], qbs[si], U2K[si], f"st2_{si}"
                            S.op("act", lambda e, pq=pq, sq=sq: e.activation(out=sq, in_=pq[:, 0:256], func=AF.Square), reads=[f"bk{bi}"], writes=[uk])
                            S.op("dve", lambda e, sq=sq, si=si: e.tensor_reduce(out=st2[:, si, 0:4], in_=sq.rearrange("p (a b) -> p a b", a=4), axis=AX.X, op=ALU.add),
                                 reads=[uk], writes=[sk])
                            S.op("act", lambda e, si=si: e.activation(out=st2[:, si, 4:8], in_=st2[:, si, 0:4], func=AF.Sqrt, scale=1.0 / 64, bias=epst[:, 0:1]),
                                 reads=[sk, "epst"], writes=[sk])
                            S.op("dve", lambda e, si=si: e.reciprocal(out=st2[:, si, 0:4], in_=st2[:, si, 4:8]), reads=[sk], writes=[sk])
                            S.op("dve", lambda e, pq=pq, qn=qn, si=si: e.tensor_tensor(out=qn.rearrange("p (a b) -> p a b", a=4), in0=pq[:, 0:256].rearrange("p (a b) -> p a b", a=4),
                                                                    in1=st2[:, si, 0:4].unsqueeze(2).to_broadcast([128, 4, 64]), op=ALU.mult),
                                 reads=[f"bk{bi}", sk], writes=[uk])
                            qscale = 0.125 if b < 8 else 1.0
                            S.op("dve", lambda e, qbt=qbt, qn=qn, wsl=wsl, qscale=qscale: e.scalar_tensor_tensor(out=qbt, in0=qn, scalar=qscale, in1=wsl, op0=ALU.mult, op1=ALU.mult),
                                 reads=[uk, "qkw"], writes=[uk])
                            for j in range(2):
                                S.op("pe", tr(pTqs[:, si, j, :], qbt[:, j * 128:(j + 1) * 128], ident[:]), reads=[uk, "ident"], writes=[f"pTq{si}"], inc=(j == 1))
                            S.op("act", lambda e, t=t, si=si: e.copy(out=stT[:, :, t * 128:(t + 1) * 128], in_=pTqs[:, si, :, :]), reads=[f"pTq{si}"], writes=["U1"])
                        if b == 9:
                            S.dma("sp", "vst", dmaf(v_s.rearrange("(t p) c -> p t c", p=128), vst[:]), reads=["U1"], writes=["v_s"])
                        else:
                            for j in range(2):
                                S.dma("sp", "stT", dmaf(qkT_s[b * 256 + j * 128: b * 256 + (j + 1) * 128, :], stT[:, j, :]), reads=["U1"], writes=["qkT_s"])
                    else:
                        for m in range(2):
                            is_p = b < 18
                            col0 = b * 256 + m * 128 - 2560
                            for tb in range(4):
                                for kc in range(KC):
                                    S.op("pe", mm(bank[tb][:], wB[i][:, kc, m * 128:(m + 1) * 128], hT[:, kc, 128 + tb * 512:128 + (tb + 1) * 512], kc == 0, kc == KC - 1),
                                         reads=[wk] + allhT[1 + tb * 4:1 + (tb + 1) * 4], writes=[f"bk{tb}"], inc=(kc == KC - 1))
                            di_ = ecnt % 2
                            ecnt += 1
                            if is_p:
                                for kc in range(KC):
                                    S.op("pe", mm(bank[4][:, 0:128], wB[i][:, kc, m * 128:(m + 1) * 128], hT[:, kc, 0:128], kc == 0, kc == KC - 1),
                                         reads=[wk, "hT0"], writes=["bk4"], inc=(kc == KC - 1))
                                g = col0 // 512
                                w = (2, 4, 8, 16)[g]
                                S.op("act", lambda e: e.activation(out=pt[:, 0:128], in_=bank[4][:, 0:128], func=AF.Copy, scale=flag[:, 0:1]),
                                     reads=["bk4", "flag"], writes=["pt"])
                                for tb in range(4):
                                    S.op("act", lambda e, tb=tb: e.copy(out=pt[:, 128 + tb * 512:128 + (tb + 1) * 512], in_=bank[tb][:]),
                                         reads=[f"bk{tb}"], writes=["pt"])
                                src = pt
                                srck = ["pt"]
                                sh = 1
                                bufs = [(pb1, U2K), (pb2, ["U1"])]
                                bi2 = 0
                                while sh < w:
                                    dstb, dk = bufs[bi2 % 2]
                                    lo = 2 * sh - 1
                                    S.op("dve", lambda e, dstb=dstb, src=src, sh=sh, lo=lo: e.tensor_tensor(out=dstb[:, lo:NTOK], in0=src[:, lo:NTOK], in1=src[:, lo - sh:NTOK - sh], op=ALU.add),
                                         reads=srck, writes=dk)
                                    src, srck = dstb, dk
                                    sh *= 2
                                    bi2 += 1
                                S.op("dve", lambda e, src=src, w=w, di_=di_: e.scalar_tensor_tensor(out=dst[di_][:], in0=src[:, 128:NTOK], scalar=1.0 / w, in1=pt[:, 128:NTOK], op0=ALU.mult, op1=ALU.subtract),
                                     reads=srck + ["pt"], writes=["dst0"])
                                fixb, fixk = (pb1, U2K) if src is not pb1 else (pb2, ["U1"])
                                S.op("dve", lambda e, src=src, g=g, fixb=fixb: e.tensor_tensor(out=fixb[:, 0:16], in0=src[:, 128:144], in1=invc[:, g * 16:(g + 1) * 16], op=ALU.mult),
                                     reads=srck + ["invc"], writes=fixk)
                                S.op("dve", lambda e, fixb=fixb, di_=di_: e.tensor_tensor(out=dst[di_][:, 0:16], in0=fixb[:, 0:16], in1=pt[:, 128:144], op=ALU.subtract),
                                     reads=fixk + ["pt"], writes=["dst0"])
                                S.dma("sp", "dst0", dmaf(dT_s[col0:col0 + 128, :], dst[di_][:]), reads=["dst0"], writes=["dT_s"])
                            else:
                                for tb in range(4):
                                    S.op("act", lambda e, tb=tb, di_=di_: e.activation(out=dst[di_][:, tb * 512:(tb + 1) * 512], in_=bank[tb][:], func=AF.Sigmoid),
                                         reads=[f"bk{tb}"], writes=["dst0"])
                                r0 = col0 - 2048
                                S.dma("sp", "dst0", dmaf(sgT_s[r0:r0 + 128, :], dst[di_][:]), reads=["dst0"], writes=["sgT_s"])
                S.barrier()
        if last_phase <= 2:
            S.barrier()
            return nc

        S.set_phase(3)
        with ExitStack() as ph34:
            AT = ph34.enter_context(nc.sbuf_tensor(_un("AT"), [128, 16, NL], BF16))
            with ExitStack() as ph:
                sb = lambda n, s, d: ph.enter_context(nc.sbuf_tensor(_un(n), s, d))
                pp = lambda n, s, d: ph.enter_context(nc.psum_tensor(_un(n), s, d))
                abias = sb("abias", [128, 32, 256], F32)
                esink = sb("esink", [128, 32], F32)
                vall = sb("vall", [128, TT, 256], BF16)
                _q0 = sb("qTg0", [128, 4, NTOK], BF16)
                qTg = [_q0, _q0]
                _ka = sb("kTa0", [128, NTOK], BF16)
                _kb = sb("kTb0", [128, NTOK], BF16)
                kTa = [_ka, _ka]
                kTb = [_kb, _kb]
                ct2 = sb("ct2", [128, 32], F32)
                cs2 = sb("cs2", [128, 32], BF16)
                cmat = sb("cmat", [128, 32, 128], BF16)
                wA2 = [sb(f"wA2{i}", [128, KC, 256], BF16) for i in range(2)]
                bb2 = [sb(f"bb2{i}", [128, 256], F32) for i in range(2)]
                n2s = [sb(f"n2s{i}", [128, 256], F32) for i in range(2)]
                stg2 = [sb(f"stg2{i}", [128, 256], F32) for i in range(2)]
                _pA2 = pp("pA2", [128, 2, 256], F32)
                pA2 = [_pA2[:, 0, :], _pA2[:, 1, :]]
                S.dma("sp", "c1", dmaf(ct2[:], cpp), writes=["ct2"])
                S.op("act", lambda e: e.activation(out=cs2[:], in_=ct2[:], func=AF.Silu), reads=["ct2"], writes=["cs2"])
                S.op("dve", lambda e: e.tensor_copy(out=cmat[:], in_=cs2[:].unsqueeze(2).to_broadcast([128, 32, 128])),
                     reads=["cs2"], writes=["cmat"])

                def ada_load(j):
                    i = j % 2
                    col0 = 2 * D + j * 256
                    S.dma("pool", f"wA2{i}", dmaf(wA2[i][:], w_ada[:, col0:col0 + 256].rearrange("(k p) n -> p k n", p=128)), writes=[f"wA2{i}"])
                    S.dma("sp", f"bb2{i}", dmaf(bb2[i][:], b_ada[col0:col0 + 256].partition_broadcast(128)), writes=[f"bb2{i}"])
                    if j // 16 == 2:
                        c4 = (j % 16) * 256
                        S.dma("sp", f"bb2{i}", dmaf(n2s[i][:], n2w[c4:c4 + 256].partition_broadcast(128)), writes=[f"n2s{i}"])

                def ada_compute(j):
                    i = j % 2
                    which = j // 16
                    c4 = (j % 16) * 256
                    for kc in range(KC):
                        S.op("pe", mm(pA2[i][:], cmat[:, kc, :], wA2[i][:, kc, :], kc == 0, kc == KC - 1),
                             reads=[f"wA2{i}", "cmat"], writes=[f"pA2{i}"], inc=(kc == KC - 1))
                    S.op("dve", lambda e, i=i: e.tensor_tensor(out=stg2[i][:], in0=pA2[i][:], in1=bb2[i][:], op=ALU.add),
                         reads=[f"pA2{i}", f"bb2{i}"], writes=[f"stg2{i}"])
                    if which == 2:
                        S.op("dve", lambda e, i=i: e.scalar_tensor_tensor(out=stg2[i][:], in0=stg2[i][:], scalar=1.0, in1=n2s[i][:], op0=ALU.add, op1=ALU.mult),
                             reads=[f"stg2{i}", f"n2s{i}"], writes=[f"stg2{i}"])
                    S.dma("sp", f"stg2{i}", dmaf(adab[which][:, c4:c4 + 256], stg2[i][:]), reads=[f"stg2{i}"], writes=["adab"])

                ssb = [sb(f"ssb{i}", [128, 512], F32) for i in range(2)]
                Pb = [sb(f"Pb{i}", [128, 512], BF16) for i in range(2)]
                PT = [sb(f"PT{i}", [128, 4, 128], BF16) for i in range(2)]
                rsb = sb("rsb", [128, 2, 8], F32)
                Atok = [sb(f"Atok{i}", [128, 512], BF16) for i in range(2)]
                pS = [pp(f"pS{i}", [128, 512], F32) for i in range(2)]
                _pPT = pp("pPT", [128, 2, 4, 128], BF16)
                pPT = [_pPT[:, 0], _pPT[:, 1]]
                _pO = pp("pO", [128, 2, 128], F32)
                pO = [_pO[:, 0, :], _pO[:, 1, :]]
                pAT = pp("pAT", [128, 4, 128], BF16)
                S.dma("sp", "c0", dmaf(abias[:].rearrange("p a b -> p (a b)"), abias_d), writes=["abias"])
                S.dma("sp", "c0", dmaf(esink[:], sinks_d.partition_broadcast(128)), writes=["esink"])
                S.dma("sp", "c0", dmaf(vall[:], v_s.rearrange("(t p) c -> p t c", p=128)), reads=["v_s"], writes=["vall"])
                S.op("act", lambda e: e.activation(out=esink[:], in_=esink[:], func=AF.Exp), reads=["esink"], writes=["esink"])

                def loadqk(kvh):
                    i = 0
                    S.dma("sp", f"qk{i}", dmaf(qTg[i][:], qkT_s[kvh * 512:(kvh + 1) * 512, :].rearrange("(c p) t -> p c t", p=128)),
                          reads=["qkT_s"], writes=[f"qTg{i}"])
                    S.dma("sp", f"qk{i}", dmaf(kTa[i][0:64, :], qkT_s[2048 + kvh * 64:2048 + (kvh + 1) * 64, :]), reads=["qkT_s"], writes=[f"kTg{i}"])
                    S.dma("sp", f"qk{i}", dmaf(kTb[i][64:128, :], qkT_s[2048 + kvh * 64:2048 + (kvh + 1) * 64, :]), reads=["qkT_s"], writes=[f"kTg{i}"])

                S.op("pool", lambda e: e.memset(kTa[0][64:128, :], 0.0), writes=["kTg0"])
                S.op("pool", lambda e: e.memset(kTb[0][0:64, :], 0.0), writes=["kTg0"])
                ada_load(0)
                u = 0
                for kvh in range(4):
                    loadqk(kvh)
                    qi = 0
                    for n in range(NT):
                        jb = kvh * NT + n
                        if jb + 1 < 64:
                            ada_load(jb + 1)
                        ada_compute(jb)
                        ai = (kvh * NT + n) % 2
                        qc0 = 128 + n * 128
                        kc0 = n * 128
                        for hp in range(4):
                            h0 = kvh * 8 + 2 * hp
                            ui = u % 2
                            u += 1
                            S.op("pe", mm(pS[ui][:, 0:256], qTg[qi][:, hp, qc0:qc0 + 128], kTa[qi][:, kc0:kc0 + 256], True, True),
                                 reads=[f"qTg{qi}", f"kTg{qi}"], writes=[f"pS{ui}"], inc=False)
                            S.op("pe", mm(pS[ui][:, 256:512], qTg[qi][:, hp, qc0:qc0 + 128], kTb[qi][:, kc0:kc0 + 256], True, True),
                                 reads=[f"qTg{qi}", f"kTg{qi}"], writes=[f"pS{ui}"], inc=True)
                            S.op("dve", lambda e, ui=ui, h0=h0: e.tensor_tensor(out=ssb[ui][:].rearrange("p (a b) -> p a b", a=2), in0=pS[ui][:].rearrange("p (a b) -> p a b", a=2), in1=abias[:, h0:h0 + 2, :], op=ALU.add),
                                 reads=[f"pS{ui}", "abias"], writes=[f"ssb{ui}"])
                            if n == 0:
                                S.op("dve", lambda e, ui=ui: e.tensor_scalar(out=ssb[ui][:].rearrange("p (a b) -> p a b", a=2)[:, :, 0:128], in0=ssb[ui][:].rearrange("p (a b) -> p a b", a=2)[:, :, 0:128], scalar1=flag[:, 1:2], scalar2=None, op0=ALU.add),
                                     reads=[f"ssb{ui}", "flag"], writes=[f"ssb{ui}"])
                            for a_ in range(2):
                                S.op("act", lambda e, ui=ui, a_=a_: e.activation(out=Pb[ui][:, a_ * 256:(a_ + 1) * 256], in_=ssb[ui][:, a_ * 256:(a_ + 1) * 256], func=AF.Exp, accum_out=rsb[:, ui, a_:a_ + 1]),
                                     reads=[f"ssb{ui}"], writes=[f"Pb{ui}", f"rsb{ui}"])
                            S.op("dve", lambda e, ui=ui, h0=h0: e.tensor_tensor(out=rsb[:, ui, 2:4], in0=rsb[:, ui, 0:2], in1=esink[:, h0:h0 + 2], op=ALU.add),
                                 reads=[f"rsb{ui}", "esink"], writes=[f"rsb{ui}"])
                            S.op("dve", lambda e, ui=ui: e.reciprocal(out=rsb[:, ui, 4:6], in_=rsb[:, ui, 2:4]), reads=[f"rsb{ui}"], writes=[f"rsb{ui}"])
                            for j in range(4):
                                S.op("pe", tr(pPT[ui][:, j, :], Pb[ui][:, j * 128:(j + 1) * 128], ident[:]), reads=[f"Pb{ui}", "ident"], writes=[f"pPT{ui}"], inc=(j == 3))
                            if u % 2 == 0:
                                S.op("act", lambda e, ui=ui: e.copy(out=PT[ui][:], in_=pPT[ui][:]), reads=[f"pPT{ui}"], writes=[f"PT{ui}"])
                            else:
                                S.op("dve", lambda e, ui=ui: e.tensor_copy(out=PT[ui][:], in_=pPT[ui][:]), reads=[f"pPT{ui}"], writes=[f"PT{ui}"])
                            vs = slice(kvh * 64, (kvh + 1) * 64)
                            S.op("pe", mm(pO[ui][:, 0:64], PT[ui][:, 0, :], vall[:, n, vs], True, False), reads=[f"PT{ui}", "vall"], writes=[f"pO{ui}"], inc=False)
                            S.op("pe", mm(pO[ui][:, 0:64], PT[ui][:, 1, :], vall[:, n + 1, vs], False, True), reads=[f"PT{ui}", "vall"], writes=[f"pO{ui}"], inc=False)
                            S.op("pe", mm(pO[ui][:, 64:128], PT[ui][:, 2, :], vall[:, n, vs], True, False), reads=[f"PT{ui}", "vall"], writes=[f"pO{ui}"], inc=False)
                            S.op("pe", mm(pO[ui][:, 64:128], PT[ui][:, 3, :], vall[:, n + 1, vs], False, True), reads=[f"PT{ui}", "vall"], writes=[f"pO{ui}"], inc=True)
                            for a_ in range(2):
                                S.op("dve", lambda e, ui=ui, a_=a_, hp=hp, ai=ai: e.tensor_scalar(out=Atok[ai][:, hp * 128 + a_ * 64:hp * 128 + (a_ + 1) * 64], in0=pO[ui][:, a_ * 64:(a_ + 1) * 64], scalar1=rsb[:, ui, 4 + a_:5 + a_], scalar2=None, op0=ALU.mult),
                                     reads=[f"pO{ui}", f"rsb{ui}"], writes=[f"Atok{ai}"])
                        for j in range(4):
                            S.op("pe", tr(pAT[:, j, :], Atok[ai][:, j * 128:(j + 1) * 128], ident[:]), reads=[f"Atok{ai}", "ident"], writes=["pAT"], inc=(j == 3))
                        S.op("act", lambda e, kvh=kvh, n=n: e.copy(out=AT[:, kvh * 4:(kvh + 1) * 4, n * 128:(n + 1) * 128], in_=pAT[:]), reads=["pAT"], writes=[f"AT{n}"])
                S.barrier()

            S.set_phase(35)
            BT = ph34.enter_context(nc.sbuf_tensor(_un("BT"), [128, 16, NL], BF16))
            with ExitStack() as ph:
                sb = lambda n, s, d: ph.enter_context(nc.sbuf_tensor(_un(n), s, d))
                pp = lambda n, s, d: ph.enter_context(nc.psum_tensor(_un(n), s, d))
                wp = [sb(f"wp{i}", [128, 4, 512], BF16) for i in range(2)]
                dTg = [sb(f"dTg{i}", [128, 4, NL], BF16) for i in range(2)]
                psc = sb("psc", [128, 16], F32)
                bank = [pp(f"bq{i}", [128, 512], F32) for i in range(4)]
                S.dma("sp", "c0", dmaf(psc[:], pscale_pp), writes=["psc"])
                for g in range(4):
                    i = g % 2
                    S.dma("pool", f"wp{i}", dmaf(wp[i][:], w_pool[g].rearrange("(c p) n -> p c n", p=128)), writes=[f"wp{i}"])
                    S.dma("sp", f"dTg{i}", dmaf(dTg[i][:], dT_s[g * 512:(g + 1) * 512, :].rearrange("(c p) t -> p c t", p=128)), reads=["dT_s"], writes=[f"dTg{i}"])
                    for m in range(4):
                        for tb in range(4):
                            bi = (m * 4 + tb) % 4
                            for cc in range(4):
                                S.op("pe", mm(bank[bi][:], wp[i][:, cc, m * 128:(m + 1) * 128], dTg[i][:, cc, tb * 512:(tb + 1) * 512], cc == 0, cc == 3),
                                     reads=[f"wp{i}", f"dTg{i}"], writes=[f"bq{bi}"], inc=(cc == 3))
                            S.op("act", lambda e, bi=bi, g=g, m=m, tb=tb: e.activation(out=BT[:, g * 4 + m, tb * 512:(tb + 1) * 512], in_=bank[bi][:], func=AF.Copy, scale=psc[:, g * 4 + m:g * 4 + m + 1]),
                                 reads=[f"bq{bi}", "psc"], writes=["BT"])
                S.barrier()

            S.set_phase(4)
            with ExitStack() as ph:
                sb = lambda n, s, d: ph.enter_context(nc.sbuf_tensor(_un(n), s, d))
                pp = lambda n, s, d: ph.enter_context(nc.psum_tensor(_un(n), s, d))
                wUa = [sb(f"wUa{i}", [128, 16, 256], BF16) for i in range(2)]
                wUb = [sb(f"wUb{i}", [128, 16, 256], BF16) for i in range(2)]
                sga = [sb(f"sga{i}", [128, NL], BF16) for i in range(2)]
                sgb = [sb(f"sgb{i}", [128, NL], BF16) for i in range(2)]
                t1 = [sb(f"t1{i}", [128, 512], F32) for i in range(2)]
                t2 = [sb(f"t2{i}", [128, 512], F32) for i in range(2)]
                mx = [sb(f"mx{i}", [128, NL], BF16) for i in range(2)]
                pa = [pp(f"pa{i}", [128, 512], F32) for i in range(2)]
                pb = [pp(f"pb{i}", [128, 512], F32) for i in range(2)]
                allAT = [f"AT{n}" for n in range(NT)]

                def loadU(cb):
                    i = cb % 2
                    S.dma("pool", f"wUa{i}", dmaf(wUa[i][:], w_aup[:, cb * 256:(cb + 1) * 256].rearrange("(k p) n -> p k n", p=128)), writes=[f"wUa{i}"])
                    S.dma("pool", f"wUb{i}", dmaf(wUb[i][:], w_bup[:, cb * 256:(cb + 1) * 256].rearrange("(k p) n -> p k n", p=128)), writes=[f"wUb{i}"])

                loadU(0)
                u = 0
                for cb in range(16):
                    if cb + 1 < 16:
                        loadU(cb + 1)
                    i = cb % 2
                    for m2 in range(2):
                        M = cb * 2 + m2
                        mi = M % 2
                        S.dma("sp", f"sga{mi}", dmaf(sga[mi][:], sgT_s[M * 128:(M + 1) * 128, :]), reads=["sgT_s"], writes=[f"sga{mi}"])
                        S.dma("sp", f"sgb{mi}", dmaf(sgb[mi][:], sgT_s[D + M * 128:D + (M + 1) * 128, :]), reads=["sgT_s"], writes=[f"sgb{mi}"])
                        for tb in range(4):
                            ui = u % 2
                            u += 1
                            ts_ = slice(tb * 512, (tb + 1) * 512)
                            for kc in range(16):
                                S.op("pe", mm(pa[ui][:], wUa[i][:, kc, m2 * 128:(m2 + 1) * 128], AT[:, kc, ts_], kc == 0, kc == 15),
                                     reads=[f"wUa{i}"] + allAT[tb * 4:(tb + 1) * 4], writes=[f"pa{ui}"], inc=(kc == 15))
                            for kc in range(16):
                                S.op("pe", mm(pb[ui][:], wUb[i][:, kc, m2 * 128:(m2 + 1) * 128], BT[:, kc, ts_], kc == 0, kc == 15),
                                     reads=[f"wUb{i}", "BT"], writes=[f"pb{ui}"], inc=(kc == 15))
                            S.op("dve", lambda e, ui=ui, mi=mi, ts_=ts_: e.tensor_tensor(out=t1[ui][:], in0=pa[ui][:], in1=sga[mi][:, ts_], op=ALU.mult),
                                 reads=[f"pa{ui}", f"sga{mi}"], writes=[f"t1{ui}"])
                            S.op("dve", lambda e, ui=ui, mi=mi, ts_=ts_: e.tensor_tensor(out=t2[ui][:], in0=pb[ui][:], in1=sgb[mi][:, ts_], op=ALU.mult),
                                 reads=[f"pb{ui}", f"sgb{mi}"], writes=[f"t2{ui}"])
                            S.op("dve", lambda e, ui=ui, mi=mi, ts_=ts_: e.tensor_tensor(out=mx[mi][:, ts_], in0=t1[ui][:], in1=t2[ui][:], op=ALU.add),
                                 reads=[f"t1{ui}", f"t2{ui}"], writes=[f"mx{mi}"])
                        S.dma("sp", f"mx{mi}", dmaf(mixT_s[M * 128:(M + 1) * 128, :], mx[mi][:]), reads=[f"mx{mi}"], writes=["mixT_s"])
                S.barrier()
        if last_phase <= 4:
            S.barrier()
            return nc

        S.set_phase(5)
        with ExitStack() as ph:
            sb = lambda n, s, d: ph.enter_context(nc.sbuf_tensor(_un(n), s, d))
            pp = lambda n, s, d: ph.enter_context(nc.psum_tensor(_un(n), s, d))
            mixT = sb("mixT", [128, KC, NL], BF16)
            gate1b = sb("gate1b", [128, D], F32)
            wO = [sb(f"wO{i}", [128, KC, 256], BF16) for i in range(2)]
            xin = [sb(f"xin{i}", [128, 256], F32) for i in range(4)]
            o5 = [sb(f"o5{i}", [128, 256], F32) for i in range(4)]
            po = [pp(f"po{i}", [128, 512], F32) for i in range(4)]
            for i in range(8):
                S.dma("sp", f"mixT{i}", dmaf(mixT[:, 4 * i:4 * i + 4, :], mixT_s[4 * i * 128:(4 * i + 4) * 128, :].rearrange("(c p) t -> p c t", p=128)),
                      reads=["mixT_s"], writes=[f"mixT{i}"])
            S.dma("sp", "c1", dmaf(gate1b[:], adab[0]), reads=["adab"], writes=["gate1b"])
            allmix = [f"mixT{i}" for i in range(8)]

            def loadO(cb):
                i = cb % 2
                S.dma("pool", f"wO{i}", dmaf(wO[i][:], w_out[:, cb * 256:(cb + 1) * 256].rearrange("(k p) n -> p k n", p=128)), writes=[f"wO{i}"])

            loadO(0)
            u = 0
            for cb in range(16):
                if cb + 1 < 16:
                    loadO(cb + 1)
                i = cb % 2
                cs_ = slice(cb * 256, (cb + 1) * 256)
                for t in range(NT):
                    ui = u % 4
                    u += 1
                    S.dma("sp", f"xin{ui}", dmaf(xin[ui][:], xh[128 + t * 128:128 + (t + 1) * 128, cs_]), writes=[f"xin{ui}"])
                    for kc in range(KC):
                        S.op("pe", mm(po[ui][:, 0:256], mixT[:, kc, t * 128:(t + 1) * 128], wO[i][:, kc, :], kc == 0, kc == KC - 1),
                             reads=[f"wO{i}"] + (allmix if (cb == 0 and t == 0) else []), writes=[f"po{ui}"], inc=(kc == KC - 1))
                    S.op("dve", lambda e, ui=ui, cs_=cs_: e.tensor_tensor(out=o5[ui][:], in0=po[ui][:, 0:256], in1=gate1b[:, cs_], op=ALU.mult),
                         reads=[f"po{ui}", "gate1b"], writes=[f"o5{ui}"])
                    S.op("dve", lambda e, ui=ui: e.tensor_tensor(out=o5[ui][:], in0=o5[ui][:], in1=xin[ui][:], op=ALU.add),
                         reads=[f"o5{ui}", f"xin{ui}"], writes=[f"o5{ui}"])
                    S.dma("sp", f"o5{ui}", dmaf(x1_s[t * 128:(t + 1) * 128, cs_], o5[ui][:]), reads=[f"o5{ui}"], writes=["x1_s"])
            S.barrier()
        if last_phase <= 5:
            S.barrier()
            return nc

        S.set_phase(6)
        with ExitStack() as ph:
            sb = lambda n, s, d: ph.enter_context(nc.sbuf_tensor(_un(n), s, d))
            pp = lambda n, s, d: ph.enter_context(nc.psum_tensor(_un(n), s, d))
            g2b = sb("g2b", [128, D], F32)
            sh2b = sb("sh2b", [128, D], F32)
            x1t = [sb(f"x1t{i}", [128, D], F32) for i in range(2)]
            h2 = sb("h2", [128, D], F32)
            h2b = [sb(f"h2b{i}", [128, D], BF16) for i in range(2)]
            h2T = sb("h2T", [128, KC, 128], F32)
            wr = sb("wrt", [128, KC, 72], F32)
            rbb = sb("rbb", [128, 72], F32)
            lg = sb("lg", [128, 72], F32)
            sc = sb("sc", [128, 256], F32)
            posb = sb("posb", [128, 64], F32)
            ovt = sb("ovt", [128, 64], F32)
            tmp64 = sb("tmp64", [128, 64], F32)
            eb = sb("eb", [128, 64], BF16)
            eacc = sb("eacc", [128, 64], F32)
            eaccb = sb("eaccb", [128, 64], BF16)
            st6 = sb("st6", [128, NT, 4], F32)
            ptr = [pp(f"ptr{i}", [128, 512], F32) for i in range(2)]
            pl = pp("pl", [128, 128], F32)
            ppos = pp("ppos", [128, 64], F32)
            S.dma("sp", "c0", dmaf(g2b[:], adab[2]), reads=["adab"], writes=["g2b"])
            S.dma("sp", "c0", dmaf(sh2b[:], adab[1]), reads=["adab"], writes=["sh2b"])
            S.dma("sp", "c1", dmaf(wr[:], wr_d.rearrange("(k p) n -> p k n", p=128)), writes=["wr"])
            S.dma("sp", "c1", dmaf(rbb[:], rb_d.partition_broadcast(128)), writes=["rbb"])
            zt = sb("zt", [128, D], BF16)
            S.op("pool", lambda e: e.memset(zt[:], 0.0), writes=["zt"])
            for r in range(NE):
                S.dma("sp", "zf", dmaf(xs_buf[r * CAP:(r + 1) * CAP, :], zt[:]), reads=["zt"], writes=["xs_buf"])
            S.dma("sp", "zf", dmaf(xs_buf[ZROW:ZROW + 1, :], zt[0:1, :]), reads=["zt"], writes=["xs_buf"])
            S.op("dve", lambda e: e.memset(eacc[:], 0.0), writes=["eacc"])
            S.op("dve", lambda e: e.memset(eaccb[:], 0.0), writes=["eaccb"])
            gmax, negg, gsum, pgrp = sc[:, 0:1], sc[:, 1:2], sc[:, 2:3], sc[:, 3:4]
            m1, m2, dlt, e2, w1, w2, d1f, d2f = [sc[:, k:k + 1] for k in range(4, 12)]
            ohg, ein, oh1, msk, oh2, eg = [sc[:, 16 + 8 * k:24 + 8 * k] for k in range(6)]
            t88 = sc[:, 64:128]
            E1 = sc[:, 128:192]
            E2 = sc[:, 192:256]
            v88 = lambda ap: ap.rearrange("p (g j) -> p g j", g=8)

            def dv(fn, extra_r=(), extra_w=()):
                S.op("dve", fn, reads=["sc"] + list(extra_r), writes=["sc"] + list(extra_w))

            S.dma("sp", "x1t0", dmaf(x1t[0][:], x1_s[0:128, :]), reads=["x1_s"], writes=["x1t0"])
            for t in range(NT):
                i = t % 2
                if t + 1 < NT:
                    S.dma("sp", f"x1t{1 - i}", dmaf(x1t[1 - i][:], x1_s[(t + 1) * 128:(t + 2) * 128, :]), reads=["x1_s"], writes=[f"x1t{1 - i}"])
                S.op("act", lambda e, i=i, t=t: e.activation(out=h2b[i][:], in_=x1t[i][:], func=AF.Square, accum_out=st6[:, t, 0:1]),
                     reads=[f"x1t{i}"], writes=[f"h2b{i}", "st6"])
                S.op("act", lambda e, t=t: e.activation(out=st6[:, t, 1:2], in_=st6[:, t, 0:1], func=AF.Sqrt, scale=1.0 / D, bias=epst[:, 0:1]),
                     reads=["st6", "epst"], writes=["st6"])
                S.op("dve", lambda e, t=t: e.reciprocal(out=st6[:, t, 2:3], in_=st6[:, t, 1:2]), reads=["st6"], writes=["st6"])
                S.op("dve", lambda e, i=i, t=t: e.scalar_tensor_tensor(out=h2[:], in0=x1t[i][:], scalar=st6[:, t, 2:3], in1=g2b[:], op0=ALU.mult, op1=ALU.mult),
                     reads=[f"x1t{i}", "st6", "g2b"], writes=["h2"])
                S.op("pool", lambda e: e.tensor_tensor(out=h2[:], in0=h2[:], in1=sh2b[:], op=ALU.add), reads=["h2", "sh2b"], writes=["h2"])
                S.op("act", lambda e, i=i: e.copy(out=h2b[i][:], in_=h2[:]), reads=["h2"], writes=[f"h2b{i}"])
                for g4 in range(8):
                    pi = g4 % 2
                    for j in range(4):
                        kc = g4 * 4 + j
                        S.op("pe", tr(ptr[pi][:, j * 128:(j + 1) * 128], h2[:, kc * 128:(kc + 1) * 128], identf), reads=["h2", "cst"], writes=[f"ptr{pi}"], inc=(j == 3))
                    if g4 % 2 == 0:
                        S.op("act", lambda e, pi=pi, g4=g4: e.copy(out=h2T[:, g4 * 4:(g4 + 1) * 4, :].rearrange("p a b -> p (a b)"), in_=ptr[pi][:]), reads=[f"ptr{pi}"], writes=["h2T"])
                    else:
                        S.op("dve", lambda e, pi=pi, g4=g4: e.tensor_copy(out=h2T[:, g4 * 4:(g4 + 1) * 4, :].rearrange("p a b -> p (a b)"), in_=ptr[pi][:]), reads=[f"ptr{pi}"], writes=["h2T"])
                for kc in range(KC):
                    S.op("pe", mm(pl[:, 0:72], h2T[:, kc, :], wr[:, kc, :], kc == 0, kc == KC - 1), reads=["h2T", "wr"], writes=["pl"], inc=(kc == KC - 1))
                S.op("dve", lambda e: e.tensor_tensor(out=lg[:], in0=pl[:, 0:72], in1=rbb[:], op=ALU.add), reads=["pl", "rbb"], writes=["lg"])
                dv(lambda e: e.reduce_max(out=gmax, in_=lg[:, 0:8], axis=AX.X), ["lg"])
                dv(lambda e: e.tensor_scalar(out=ohg, in0=lg[:, 0:8], scalar1=gmax, scalar2=None, op0=ALU.is_equal), ["lg"])
                dv(lambda e: e.tensor_scalar(out=negg, in0=gmax, scalar1=-1.0, scalar2=None, op0=ALU.mult))
                S.op("act", lambda e: e.activation(out=eg, in_=lg[:, 0:8], func=AF.Exp, bias=negg, accum_out=gsum), reads=["sc", "lg"], writes=["sc"])
                dv(lambda e: e.reciprocal(out=pgrp, in_=gsum))
                dv(lambda e: e.tensor_tensor(out=v88(t88), in0=v88(lg[:, 8:72]), in1=ohg.unsqueeze(2).to_broadcast([128, 8, 8]), op=ALU.mult), ["lg"])
                dv(lambda e: e.tensor_reduce(out=ein, in_=t88.rearrange("p (g j) -> p j g", g=8), axis=AX.X, op=ALU.add))
                dv(lambda e: e.reduce_max(out=m1, in_=ein, axis=AX.X))
                dv(lambda e: e.tensor_scalar(out=oh1, in0=ein, scalar1=m1, scalar2=None, op0=ALU.is_equal))
                dv(lambda e: e.scalar_tensor_tensor(out=msk, in0=oh1, scalar=-1e30, in1=ein, op0=ALU.mult, op1=ALU.add))
                dv(lambda e: e.reduce_max(out=m2, in_=msk, axis=AX.X))
                dv(lambda e: e.tensor_scalar(out=oh2, in0=msk, scalar1=m2, scalar2=None, op0=ALU.is_equal))
                dv(lambda e: e.tensor_tensor(out=dlt, in0=m2, in1=m1, op=ALU.subtract))
                S.op("act", lambda e: e.activation(out=e2, in_=dlt, func=AF.Exp), reads=["sc"], writes=["sc"])
                dv(lambda e: e.tensor_scalar(out=w1, in0=e2, scalar1=1.0, scalar2=None, op0=ALU.add))
                dv(lambda e: e.reciprocal(out=w1, in_=w1))
                dv(lambda e: e.tensor_tensor(out=w2, in0=e2, in1=w1, op=ALU.mult))
                dv(lambda e, t=t: e.tensor_scalar(out=cw[:, t, 0:1], in0=w1, scalar1=pgrp, scalar2=None, op0=ALU.mult), [], ["cw"])
                dv(lambda e, t=t: e.tensor_scalar(out=cw[:, t, 1:2], in0=w2, scalar1=pgrp, scalar2=None, op0=ALU.mult), [], ["cw"])
                dv(lambda e: e.tensor_tensor(out=v88(E1), in0=ohg.unsqueeze(2).to_broadcast([128, 8, 8]), in1=oh1.unsqueeze(1).to_broadcast([128, 8, 8]), op=ALU.mult))
                dv(lambda e: e.tensor_tensor(out=v88(E2), in0=ohg.unsqueeze(2).to_broadcast([128, 8, 8]), in1=oh2.unsqueeze(1).to_broadcast([128, 8, 8]), op=ALU.mult))
                dv(lambda e: e.tensor_tensor(out=eb[:], in0=E1, in1=E2, op=ALU.add), [], ["eb"])
                S.op("pe", mm(ppos[:], tri[:], eb[:], True, False), reads=["tri", "eb"], writes=["ppos"], inc=False)
                S.op("pe", mm(ppos[:], ones[:], eaccb[:], False, True), reads=["ones", "eaccb"], writes=["ppos"], inc=True)
                S.op("dve", lambda e: e.tensor_tensor(out=posb[:], in0=ppos[:], in1=ebase, op=ALU.add), reads=["ppos", "cst"], writes=["posb"])
                S.op("dve", lambda e: e.tensor_scalar(out=ovt[:], in0=ppos[:], scalar1=float(CAP), scalar2=None, op0=ALU.is_ge), reads=["ppos"], writes=["ovt"])
                S.op("dve", lambda e: e.tensor_scalar(out=tmp64[:], in0=posb[:], scalar1=-1.0, scalar2=float(ZROW), op0=ALU.mult, op1=ALU.add), reads=["posb"], writes=["tmp64"])
                S.op("dve", lambda e: e.tensor_tensor(out=tmp64[:], in0=tmp64[:], in1=ovt[:], op=ALU.mult), reads=["tmp64", "ovt"], writes=["tmp64"])
                S.op("dve", lambda e: e.tensor_tensor(out=posb[:], in0=posb[:], in1=tmp64[:], op=ALU.add), reads=["posb", "tmp64"], writes=["posb"])
                dv(lambda e: e.tensor_tensor(out=t88, in0=E1, in1=posb[:], op=ALU.mult), ["posb"])
                dv(lambda e: e.reduce_sum(out=d1f, in_=t88, axis=AX.X))
                dv(lambda e: e.tensor_tensor(out=t88, in0=E2, in1=posb[:], op=ALU.mult), ["posb"])
                dv(lambda e: e.reduce_sum(out=d2f, in_=t88, axis=AX.X))
                dv(lambda e, t=t: e.tensor_copy(out=di[:, t, 0:1], in_=d1f), [], ["di"])
                dv(lambda e, t=t: e.tensor_copy(out=di[:, t, 1:2], in_=d2f), [], ["di"])
                S.op("dve", lambda e: e.tensor_tensor(out=eacc[:], in0=eacc[:], in1=eb[:], op=ALU.add), reads=["eacc", "eb"], writes=["eacc"])
                S.op("dve", lambda e: e.tensor_copy(out=eaccb[:], in_=eacc[:]), reads=["eacc"], writes=["eaccb"])
                for k2 in range(2):
                    S.dma("pool", f"scat{k2}", lambda e, t=t, k2=k2, i=i: e.indirect_dma_start(
                        out=xs_buf, out_offset=bass.IndirectOffsetOnAxis(ap=di[:, t, k2:k2 + 1], axis=0),
                        in_=h2b[i][:], in_offset=None, bounds_check=bcreg, oob_is_err=False),
                        reads=[f"h2b{i}", "di"], writes=["xs_buf"])
            if "dbg_s" in debug:
                S.dma("sp", "c0", dmaf(dbg_s[:, 0:32], cw[:].rearrange("p a b -> p (a b)")), reads=["cw"], writes=["dbg"])
            S.barrier()
        if last_phase <= 6:
            S.barrier()
            return nc

        S.set_phase(7)
        with ExitStack() as ph:
            sb = lambda n, s, d: ph.enter_context(nc.sbuf_tensor(_un(n), s, d))
            pp = lambda n, s, d: ph.enter_context(nc.psum_tensor(_un(n), s, d))
            NR = 14
            ring = [sb(f"ring{i}", [128, D], BF16) for i in range(NR)]
            xs = [sb(f"xs{i}", [128, D], BF16) for i in range(2)]
            xsT = [sb(f"xsT{i}", [128, KC, 128], BF16) for i in range(2)]
            sg = sb("sg", [128, 512], F32)
            act = sb("actb", [128, 512], BF16)
            actT = sb("actT", [128, 4, 128], BF16)
            yst = [sb(f"yst{i}", [128, D], F32) for i in range(2)]
            zrow = sb("zrow", [1, D], F32)
            pxT = [pp(f"pxT{i}", [128, 8, 128], BF16) for i in range(2)]
            pg = pp("pg", [128, 512], F32)
            pu = pp("pu", [128, 512], F32)
            paT = pp("paT", [128, 4, 128], BF16)
            py = [pp(f"py{i}", [128, 512], F32) for i in range(2)]
            S.op("dve", lambda e: e.memset(zrow[:], 0.0), writes=["zrow"])
            S.dma("sp", "c0", dmaf(y_s[ZROW:ZROW + 1, :], zrow[:]), reads=["zrow"], writes=["y_s"])
            units = []
            for e_ in range(NE):
                for kg in range(4):
                    units.append(("g", e_, kg))
                    units.append(("u", e_, kg))
                for db in range(4):
                    units.append(("d", e_, db))
            nload = [0]

            def ensure_loaded(upto):
                while nload[0] < min(len(units), upto):
                    j = nload[0]
                    kind, e_, k_ = units[j]
                    s_ = j % NR
                    if kind == "g":
                        src = w_gate[e_][k_ * 1024:(k_ + 1) * 1024, :].rearrange("(c p) n -> p c n", p=128)
                        dstv = ring[s_][:].rearrange("p (c n) -> p c n", c=8)
                    elif kind == "u":
                        src = w_up[e_][k_ * 1024:(k_ + 1) * 1024, :].rearrange("(c p) n -> p c n", p=128)
                        dstv = ring[s_][:].rearrange("p (c n) -> p c n", c=8)
                    else:
                        src = w_down[e_][:, k_ * 1024:(k_ + 1) * 1024].rearrange("(c p) n -> p c n", p=128)
                        dstv = ring[s_][:].rearrange("p (c n) -> p c n", c=4)
                    S.dma("pool", f"ring{s_}", dmaf(dstv, src), writes=[f"ring{s_}"])
                    nload[0] += 1

            ui = 0
            cpy = 0
            for e_ in range(NE):
                i = e_ % 2
                S.dma("sp", f"xs{i}", dmaf(xs[i][:], xs_buf[e_ * CAP:(e_ + 1) * CAP, :]), reads=["xs_buf"], writes=[f"xs{i}"])
                for g8 in range(4):
                    pi = g8 % 2
                    for j in range(8):
                        kc = g8 * 8 + j
                        S.op("pe", tr(pxT[pi][:, j, :], xs[i][:, kc * 128:(kc + 1) * 128], ident[:]), reads=[f"xs{i}", "ident"], writes=[f"pxT{pi}"], inc=(j == 7))
                    if g8 % 2 == 0:
                        S.op("act", lambda e, pi=pi, g8=g8, i=i: e.copy(out=xsT[i][:, g8 * 8:(g8 + 1) * 8, :], in_=pxT[pi][:]), reads=[f"pxT{pi}"], writes=[f"xsT{i}"])
                    else:
                        S.op("dve", lambda e, pi=pi, g8=g8, i=i: e.tensor_copy(out=xsT[i][:, g8 * 8:(g8 + 1) * 8, :], in_=pxT[pi][:]), reads=[f"pxT{pi}"], writes=[f"xsT{i}"])
                for kg in range(4):
                    for kind, pacc, pk in (("g", pg, "pg"), ("u", pu, "pu")):
                        ensure_loaded(ui + NR)
                        s_ = ui % NR
                        assert units[ui] == (kind, e_, kg)
                        wv = ring[s_][:].rearrange("p (c n) -> p c n", c=8)
                        for j in range(8):
                            kc = kg * 8 + j
                            S.op("pe", mm(pacc[:], xsT[i][:, kc, :], wv[:, j, :], kc == 0, kc == KC - 1),
                                 reads=[f"ring{s_}", f"xsT{i}"], writes=[pk], inc=(j == 7))
                        ui += 1
                S.op("act", lambda e: e.activation(out=sg[:], in_=pg[:], func=AF.Silu), reads=["pg"], writes=["sg"])
                S.op("dve", lambda e: e.tensor_tensor(out=act[:], in0=sg[:], in1=pu[:], op=ALU.mult), reads=["sg", "pu"], writes=["act"])
                for j in range(4):
                    S.op("pe", tr(paT[:, j, :], act[:, j * 128:(j + 1) * 128], ident[:]), reads=["act", "ident"], writes=["paT"], inc=(j == 3))
                S.op("act", lambda e: e.copy(out=actT[:], in_=paT[:]), reads=["paT"], writes=["actT"])
                for db in range(4):
                    ensure_loaded(ui + NR)
                    s_ = ui % NR
                    assert units[ui] == ("d", e_, db)
                    wv = ring[s_][:].rearrange("p (c n) -> p c n", c=4)
                    for dj in range(2):
                        yi = cpy % 2
                        cpy += 1
                        for fc in range(4):
                            S.op("pe", mm(py[yi][:], actT[:, fc, :], wv[:, fc, dj * 512:(dj + 1) * 512], fc == 0, fc == 3),
                                 reads=[f"ring{s_}", "actT"], writes=[f"py{yi}"], inc=(fc == 3))
                        c0 = db * 1024 + dj * 512
                        if yi == 0:
                            S.op("act", lambda e, yi=yi, i=i, c0=c0: e.copy(out=yst[i][:, c0:c0 + 512], in_=py[yi][:]), reads=[f"py{yi}"], writes=[f"yst{i}"])
                        else:
                            S.op("dve", lambda e, yi=yi, i=i, c0=c0: e.tensor_copy(out=yst[i][:, c0:c0 + 512], in_=py[yi][:]), reads=[f"py{yi}"], writes=[f"yst{i}"])
                    ui += 1
                S.dma("sp", f"yst{i}", dmaf(y_s[e_ * CAP:(e_ + 1) * CAP, :], yst[i][:]), reads=[f"yst{i}"], writes=["y_s"])
            S.barrier()
        if last_phase <= 7:
            S.barrier()
            return nc

        S.set_phase(8)
        with ExitStack() as ph:
            sb = lambda n, s, d: ph.enter_context(nc.sbuf_tensor(_un(n), s, d))
            gate2b = sb("gate2b", [128, D], F32)
            Y1 = [sb(f"Y1{i}", [128, D], F32) for i in range(2)]
            Y2 = [sb(f"Y2{i}", [128, D], F32) for i in range(2)]
            x1t = [sb(f"x1u{i}", [128, D], F32) for i in range(2)]
            acc = sb("acc8", [128, D], F32)
            o8 = [sb(f"o8{i}", [128, D], F32) for i in range(2)]
            S.dma("sp", "c0", dmaf(gate2b[:], adab[3]), reads=["adab"], writes=["gate2b"])
            for t in range(NT):
                i = t % 2
                S.dma("pool", f"Y1{i}", lambda e, t=t, i=i: e.indirect_dma_start(
                    out=Y1[i][:], out_offset=None, in_=y_s, in_offset=bass.IndirectOffsetOnAxis(ap=di[:, t, 0:1], axis=0),
                    bounds_check=bcreg, oob_is_err=False), reads=["y_s", "di"], writes=[f"Y1{i}"])
                S.dma("pool", f"Y2{i}", lambda e, t=t, i=i: e.indirect_dma_start(
                    out=Y2[i][:], out_offset=None, in_=y_s, in_offset=bass.IndirectOffsetOnAxis(ap=di[:, t, 1:2], axis=0),
                    bounds_check=bcreg, oob_is_err=False), reads=["y_s", "di"], writes=[f"Y2{i}"])
                S.dma("sp", f"x1u{i}", dmaf(x1t[i][:], x1_s[t * 128:(t + 1) * 128, :]), reads=["x1_s"], writes=[f"x1u{i}"])
                S.op("act", lambda e, t=t, i=i: e.activation(out=acc[:], in_=Y1[i][:], func=AF.Copy, scale=cw[:, t, 0:1]), reads=[f"Y1{i}", "cw"], writes=["acc"])
                S.op("dve", lambda e, t=t, i=i: e.scalar_tensor_tensor(out=acc[:], in0=Y2[i][:], scalar=cw[:, t, 1:2], in1=acc[:], op0=ALU.mult, op1=ALU.add),
                     reads=[f"Y2{i}", "cw", "acc"], writes=["acc"])
                S.op("dve", lambda e: e.tensor_tensor(out=acc[:], in0=acc[:], in1=gate2b[:], op=ALU.mult), reads=["acc", "gate2b"], writes=["acc"])
                S.op("dve", lambda e, i=i: e.tensor_tensor(out=o8[i][:], in0=acc[:], in1=x1t[i][:], op=ALU.add), reads=["acc", f"x1u{i}"], writes=[f"o8{i}"])
                S.dma("sp", f"o8{i}", dmaf(out_d[t * 128:(t + 1) * 128, :], o8[i][:]), reads=[f"o8{i}"], writes=["out"])
            S.barrier()
    nc._sched = S
    return nc


def _prep(inputs):
    x = np.asarray(inputs["x"], np.float32)
    c = np.asarray(inputs["c"], np.float32)
    g = lambda k: np.asarray(inputs[k], np.float32)[0]
    shared = {}
    shared["w_ada"] = g("w_ada")
    b_ada = g("b_ada")
    shared["b_ada"] = b_ada
    shared["bada_pp"] = np.ascontiguousarray(b_ada[:8192].reshape(64, 128).T)
    shared["n1w_pp"] = np.ascontiguousarray(g("norm1_w").reshape(32, 128).T)
    shared["n2w"] = g("norm2_w")
    shared["w_in"] = g("w_in")
    qw = np.tile(g("q_norm_w"), 4)
    kw = np.tile(g("k_norm_w"), 4)
    shared["qkw_rep"] = np.ascontiguousarray(np.broadcast_to(np.concatenate([qw, kw])[None, :], (128, 512)))
    shared["sinks"] = g("sinks")
    slopes = (2.0 ** (-8.0 * np.arange(1, 33, dtype=np.float32) / 32)).astype(np.float32)
    qi = np.arange(128)[:, None]
    kj = np.arange(256)[None, :]
    dist = qi + 128 - kj
    valid = (dist >= 0) & (dist < 128)
    ab = np.where(valid[None], -slopes[:, None, None] * dist[None].astype(np.float32), np.float32(NEG)).astype(np.float32)
    shared["abias"] = np.ascontiguousarray(ab.transpose(1, 0, 2).reshape(128, 32 * 256))
    shared["w_pool"] = g("w_pool")
    shared["pscale_pp"] = np.ascontiguousarray(g("pool_scale").reshape(16, 128).T)
    shared["w_aup"] = g("w_attn_up")
    shared["w_bup"] = g("w_pool_up")
    shared["w_out"] = g("w_out")
    shared["wr"] = np.ascontiguousarray(np.concatenate([g("w_router_group"), g("w_router_expert")], axis=1))
    shared["rb"] = np.concatenate([g("b_router_group"), g("b_router_expert")])
    shared["w_gate"] = g("w_gate")
    shared["w_up"] = g("w_up")
    shared["w_down"] = g("w_down")
    cst = np.zeros((128, 448), np.float32)
    cst[:, 0:128] = np.eye(128, dtype=np.float32)
    cst[:, 128:256] = (np.arange(128)[:, None] < np.arange(128)[None, :]).astype(np.float32)
    cst[:, 256:384] = 1.0
    cst[:, 384:448] = (np.arange(64) * CAP)[None, :].astype(np.float32)
    shared["consts"] = cst
    in_maps = []
    for i in range(8):
        b = i // 4
        s0 = (i % 4) * NL
        m = dict(shared)
        if s0 == 0:
            halo = np.zeros((HALO, D), np.float32)
        else:
            halo = x[b, s0 - HALO:s0]
        m["xh"] = np.ascontiguousarray(np.concatenate([halo, x[b, s0:s0 + NL]], axis=0))
        m["cpp"] = np.ascontiguousarray(c[b].reshape(32, 128).T)
        fl = np.zeros((128, 2), np.float32)
        fl[:, 0] = 0.0 if s0 == 0 else 1.0
        fl[:, 1] = NEG if s0 == 0 else 0.0
        m["flag"] = fl
        invc = np.zeros((128, 64), np.float32)
        for gi, w in enumerate((2, 4, 8, 16)):
            tpos = s0 + np.arange(16) + 1
            invc[:, gi * 16:(gi + 1) * 16] = (1.0 / np.minimum(tpos, w)).astype(np.float32)[None, :]
        m["invc"] = invc
        in_maps.append(m)
    return in_maps


_NC_CACHE = {}


def kernel(**inputs):
    in_maps = _prep(inputs)
    if "nc" not in _NC_CACHE:
        _NC_CACHE["nc"] = build_nc()
    nc = _NC_CACHE["nc"]
    res = run_bass_kernel_spmd(nc, in_maps, core_ids=list(range(8)))
    out = np.empty((2, 4 * NL, D), np.float32)
    for i in range(8):
        b = i // 4
        s0 = (i % 4) * NL
        out[b, s0:s0 + NL] = res.results[i]["out"]
    return out
```

```python
import numpy as np
from contextlib import ExitStack
import concourse.bass as bass
import concourse.mybir as mybir
from concourse.bass_utils import run_bass_kernel_spmd

F32 = mybir.dt.float32
BF16 = mybir.dt.bfloat16
I32 = mybir.dt.int32
ALU = mybir.AluOpType
AF = mybir.ActivationFunctionType
AX = mybir.AxisListType

D = 4096
KC = 32
NL = 2048
NT = 16
HALO = 128
NTOK = NL + HALO
TT = 17
CAP = 128
NE = 64
NSLOT = NE * CAP
ZROW = NSLOT
EPS = 1e-6
NEG = -30000.0
INW = 12800


class Sched:
    def __init__(self, nc, stack):
        self.nc = nc
        self.stack = stack
        self.esem = {}
        self.cnt = {e: 0 for e in ["pe", "act", "dve", "pool", "sp"]}
        for e in ["pe", "act", "dve", "pool"]:
            self.esem[e] = stack.enter_context(nc.semaphore("c_" + e))
        self.dpool = []
        self.dkey = {}
        self.dfree = []
        self.res = {}
        self.waited = {e: {} for e in ["pe", "act", "dve", "pool", "sp"]}
        self.pending = {e: [] for e in ["pe", "act", "dve", "pool", "sp"]}
        self.sems = {}
        self.trace = {}
        self.mute = False
        self.only = None

    def set_phase(self, k):
        self.mute = (self.only is not None) and (k not in self.only)

    def _dma_sem(self, key):
        if key not in self.dkey:
            if self.dfree:
                idx = self.dfree.pop()
            else:
                idx = len(self.dpool)
                sem = self.stack.enter_context(self.nc.semaphore(f"d{idx}"))
                self.dpool.append([sem, 0])
                self.sems[f"d#{idx}"] = sem
            self.dkey[key] = idx
        return self.dkey[key]

    def _deps(self, eng, reads, writes):
        toks = {}

        def add(t):
            if t is None:
                return
            n, v = t
            if toks.get(n, 0) < v:
                toks[n] = v

        for k in reads:
            r = self.res.get(k)
            if r:
                add(r[0])
        for k in writes:
            r = self.res.get(k)
            if r:
                add(r[0])
                for t in r[1]:
                    add(t)
        out = []
        for n, v in toks.items():
            if eng == "pe" and n == "c_pe":
                continue
            if self.waited[eng].get(n, 0) >= v:
                continue
            self.waited[eng][n] = v
            out.append((n, v))
        return out

    def _record(self, tok, reads, writes):
        for k in reads:
            r = self.res.setdefault(k, [None, []])
            r[1].append(tok)
            if len(r[1]) > 32:
                mm = {}
                for n, v in r[1]:
                    mm[n] = max(mm.get(n, 0), v)
                r[1] = list(mm.items())
        for k in writes:
            self.res[k] = [tok, []]

    def op(self, eng, fn, reads=(), writes=(), inc=True):
        if self.mute:
            return
        reads = list(reads)
        writes = list(writes)
        deps = self._deps(eng, reads, writes)
        if inc:
            self.cnt[eng] += 1
            tok = ("c_" + eng, self.cnt[eng])
            pend = self.pending[eng]
            self.pending[eng] = []
            for (pr, pw) in pend:
                self._record(tok, pr, pw)
            self._record(tok, reads, writes)
        else:
            self.pending[eng].append((reads, writes))
        self._emit(eng, deps, fn, inc)

    def dma(self, eng, key, fn, reads=(), writes=()):
        if self.mute:
            return
        reads = list(reads)
        writes = list(writes)
        deps = self._deps(eng, reads, writes)
        idx = self._dma_sem(key)
        self.dpool[idx][1] += 16
        tok = (f"d#{idx}", self.dpool[idx][1])
        self._record(tok, reads, writes)
        self._emit(eng, deps, fn, ("dma", idx))

    def barrier(self):
        for e in ["pe", "act", "dve", "pool"]:
            assert not self.pending[e], f"pending accesses without inc on {e}"
        toks = [("c_" + e, self.cnt[e]) for e in ["pe", "act", "dve", "pool"] if self.cnt[e] > 0]
        toks += [(f"d#{i}", v[1]) for i, v in enumerate(self.dpool) if v[1] > 0]
        for e in ["pe", "act", "dve", "pool", "sp"]:
            deps = []
            for n, v in toks:
                if self.waited[e].get(n, 0) >= v:
                    continue
                self.waited[e][n] = v
                deps.append((n, v))
            if deps:
                self._emit(e, deps, None, False)
        self.dkey = {}
        self.dfree = list(range(len(self.dpool)))

    def _engobj(self, e):
        nc = self.nc
        return {"pe": nc.tensor, "act": nc.scalar, "dve": nc.vector, "pool": nc.gpsimd, "sp": nc.sync}[e]

    def _semh(self, n):
        if n.startswith("c_"):
            return self.esem[n[2:]]
        return self.sems[n]

    def _emit(self, engname, deps, fn, inc):
        eng = self._engobj(engname)
        tr_ = self.trace.setdefault(engname, [])
        for n, v in deps:
            eng.wait_ge(self._semh(n), v)
            tr_.append(("w", n, v))
        if fn is None:
            return
        ins = fn(eng)
        if inc is True:
            ins.then_inc(self.esem[engname], 1)
            tr_.append(("i", "c_" + engname, 1))
        elif isinstance(inc, tuple):
            ins.then_inc(self.dpool[inc[1]][0], 16)
            tr_.append(("i", f"d#{inc[1]}", 16))
        else:
            tr_.append(("n",))

    def simulate(self):
        val = {}
        pc = {e: 0 for e in self.trace}
        progress = True
        while progress:
            progress = False
            for e, t in self.trace.items():
                while pc[e] < len(t):
                    op = t[pc[e]]
                    if op[0] == "w":
                        if val.get(op[1], 0) < op[2]:
                            break
                    elif op[0] == "i":
                        val[op[1]] = val.get(op[1], 0) + op[2]
                    pc[e] += 1
                    progress = True
        stuck = {e: (pc[e], len(t), t[pc[e]]) for e, t in self.trace.items() if pc[e] < len(t)}
        return stuck, val


def mm(out, lhsT, rhs, st, sp):
    return lambda e: e.matmul(out, lhsT=lhsT, rhs=rhs, start=st, stop=sp)


def tr(out, in_, ident):
    return lambda e: e.transpose(out=out, in_=in_, identity=ident)


def dmaf(out, in_):
    return lambda e: e.dma_start(out=out, in_=in_)


def build_nc(debug=(), last_phase=99, only=None):
    nc = bass.Bass("TRN2", target_bir_lowering=False)

    def din(name, shape, dt=F32):
        return nc.dram_tensor(name, list(shape), dt, kind="ExternalInput").ap()

    def dscr(name, shape, dt):
        if name in debug:
            return nc.dram_tensor(name, list(shape), dt, kind="ExternalOutput").ap()
        return nc.dram_tensor(name, list(shape), dt).ap()

    xh = din("xh", [NTOK, D])
    cpp = din("cpp", [128, 32])
    flag_d = din("flag", [128, 2])
    invc_d = din("invc", [128, 64])
    w_ada = din("w_ada", [D, 6 * D])
    b_ada = din("b_ada", [6 * D])
    bada_pp = din("bada_pp", [128, 64])
    n1w_pp = din("n1w_pp", [128, 32])
    n2w = din("n2w", [D])
    w_in = din("w_in", [D, INW])
    qkw_rep = din("qkw_rep", [128, 512])
    sinks_d = din("sinks", [32])
    abias_d = din("abias", [128, 32 * 256])
    w_pool = din("w_pool", [4, 512, 512])
    pscale_pp = din("pscale_pp", [128, 16])
    w_aup = din("w_aup", [2048, D])
    w_bup = din("w_bup", [2048, D])
    w_out = din("w_out", [D, D])
    wr_d = din("wr", [D, 72])
    rb_d = din("rb", [72])
    w_gate = din("w_gate", [NE, D, 512])
    w_up = din("w_up", [NE, D, 512])
    w_down = din("w_down", [NE, 512, D])
    consts_d = din("consts", [128, 128 * 3 + 64])
    out_d = nc.dram_tensor("out", [NL, D], F32, kind="ExternalOutput").ap()

    adab = dscr("adab", [4, 128, D], F32)
    qkT_s = dscr("qkT_s", [2048 + 256, NTOK], BF16)
    v_s = dscr("v_s", [NTOK, 256], BF16)
    dT_s = dscr("dT_s", [2048, NL], BF16)
    sgT_s = dscr("sgT_s", [2 * D, NL], BF16)
    mixT_s = dscr("mixT_s", [D, NL], BF16)
    x1_s = dscr("x1_s", [NL, D], F32)
    xs_buf = dscr("xs_buf", [NSLOT + 1, D], BF16)
    y_s = dscr("y_s", [NSLOT + 1, D], F32)
    dbg_s = dscr("dbg_s", [128, NT * 8], F32)

    _uid = [0]

    def _un(n):
        _uid[0] += 1
        return f"{n}_u{_uid[0]}"

    with ExitStack() as top:
        S = Sched(nc, top)
        S.only = only
        sbt = lambda n, s, d: top.enter_context(nc.sbuf_tensor(_un(n), s, d))
        cst = sbt("cst", [128, 128 * 3 + 64], F32)
        identf = cst[:, 0:128]
        ident = sbt("ident", [128, 128], BF16)
        tri = sbt("tri", [128, 128], BF16)
        ones = sbt("ones", [128, 128], BF16)
        ebase = cst[:, 384:448]
        flag = sbt("flagt", [128, 2], F32)
        g1 = sbt("g1", [128, 32], F32)
        sh1 = sbt("sh1", [128, 32], F32)
        epst = sbt("epst", [128, 1], F32)
        cw = sbt("cw", [128, NT, 2], F32)
        di = sbt("di", [128, NT, 2], I32)

        bcreg = nc.gpsimd.alloc_register("bcreg")
        nc.gpsimd.reg_mov(bcreg, NSLOT)
        S.dma("sp", "c0", dmaf(cst[:], consts_d), writes=["cst"])
        S.dma("sp", "c0", dmaf(flag[:], flag_d), writes=["flag"])
        S.op("dve", lambda e: e.tensor_copy(out=ident[:], in_=cst[:, 0:128]), reads=["cst"], writes=["ident"])
        S.op("dve", lambda e: e.tensor_copy(out=tri[:], in_=cst[:, 128:256]), reads=["cst"], writes=["tri"])
        S.op("dve", lambda e: e.tensor_copy(out=ones[:], in_=cst[:, 256:384]), reads=["cst"], writes=["ones"])
        S.op("dve", lambda e: e.memset(epst[:], EPS), writes=["epst"])

        S.set_phase(0)
        with ExitStack() as ph:
            sb = lambda n, s, d: ph.enter_context(nc.sbuf_tensor(_un(n), s, d))
            pp = lambda n, s, d: ph.enter_context(nc.psum_tensor(_un(n), s, d))
            ct = sb("ct", [128, 32], F32)
            cs = sb("cs", [128, 32], BF16)
            wA = [sb(f"wA{i}", [128, 32, 512], BF16) for i in range(2)]
            bpp = sb("bpp", [128, 64], F32)
            n1pp = sb("n1pp", [128, 32], F32)
            adaT = sb("adaT", [128, 64], F32)
            pTa = pp("pTa", [128, 64], F32)

            S.dma("sp", "c0", dmaf(ct[:], cpp), writes=["ct"])
            S.dma("sp", "c0", dmaf(bpp[:], bada_pp), writes=["bpp"])
            S.dma("sp", "c0", dmaf(n1pp[:], n1w_pp), writes=["n1pp"])
            S.op("act", lambda e: e.activation(out=cs[:], in_=ct[:], func=AF.Silu), reads=["ct"], writes=["cs"])

            def loadA(blk):
                i = blk % 2
                S.dma("pool", f"wA{i}", dmaf(wA[i][:], w_ada[:, blk * 512:(blk + 1) * 512].rearrange("(k p) n -> p k n", p=128)),
                      writes=[f"wA{i}"])

            loadA(0)
            for blk in range(16):
                if blk + 1 < 16:
                    loadA(blk + 1)
                i = blk % 2
                for fc in range(4):
                    col = blk * 4 + fc
                    for kc in range(KC):
                        S.op("pe", mm(pTa[:, col:col + 1], wA[i][:, kc, fc * 128:(fc + 1) * 128], cs[:, kc:kc + 1], kc == 0, kc == KC - 1),
                             reads=[f"wA{i}", "cs"], writes=["pTa"], inc=(kc == KC - 1 and fc == 3))
            S.op("dve", lambda e: e.tensor_tensor(out=adaT[:], in0=pTa[:], in1=bpp[:], op=ALU.add),
                 reads=["pTa", "bpp"], writes=["adaT"])
            S.op("dve", lambda e: e.scalar_tensor_tensor(out=g1[:], in0=adaT[:, 32:64], scalar=1.0, in1=n1pp[:], op0=ALU.add, op1=ALU.mult),
                 reads=["adaT", "n1pp"], writes=["g1"])
            S.op("dve", lambda e: e.tensor_copy(out=sh1[:], in_=adaT[:, 0:32]), reads=["adaT"], writes=["sh1"])
            S.barrier()
        if last_phase <= 0:
            S.barrier()
            return nc

        S.set_phase(1)
        with ExitStack() as ph12:
            hT = ph12.enter_context(nc.sbuf_tensor(_un("hT"), [128, KC, NTOK], BF16))
            with ExitStack() as ph:
                sb = lambda n, s, d: ph.enter_context(nc.sbuf_tensor(_un(n), s, d))
                pp = lambda n, s, d: ph.enter_context(nc.psum_tensor(_un(n), s, d))
                xt = [sb(f"xt{i}", [128, D], F32) for i in range(2)]
                xb = sb("xb", [128, D], BF16)
                st = sb("st1", [128, TT, 4], F32)
                pT = [pp(f"pT{i}", [128, 8, 128], BF16) for i in range(2)]
                S.dma("sp", "xt0", dmaf(xt[0][:], xh[0:128, :]), writes=["xt0"])
                for t in range(TT):
                    i = t % 2
                    if t + 1 < TT:
                        S.dma("sp", f"xt{1 - i}", dmaf(xt[1 - i][:], xh[(t + 1) * 128:(t + 2) * 128, :]), writes=[f"xt{1 - i}"])
                    S.op("act", lambda e, i=i, t=t: e.activation(out=xb[:], in_=xt[i][:], func=AF.Square, accum_out=st[:, t, 0:1]),
                         reads=[f"xt{i}"], writes=["xb", f"st{t}"])
                    S.op("act", lambda e, t=t: e.activation(out=st[:, t, 1:2], in_=st[:, t, 0:1], func=AF.Sqrt, scale=1.0 / D, bias=epst[:, 0:1]),
                         reads=[f"st{t}", "epst"], writes=[f"st{t}"])
                    S.op("dve", lambda e, t=t: e.reciprocal(out=st[:, t, 2:3], in_=st[:, t, 1:2]), reads=[f"st{t}"], writes=[f"st{t}"])
                    S.op("act", lambda e, i=i, t=t: e.activation(out=xb[:], in_=xt[i][:], func=AF.Copy, scale=st[:, t, 2:3]),
                         reads=[f"xt{i}", f"st{t}"], writes=["xb"])
                    for g in range(4):
                        pi = (t * 4 + g) % 2
                        for j in range(8):
                            kc = g * 8 + j
                            S.op("pe", tr(pT[pi][:, j, :], xb[:, kc * 128:(kc + 1) * 128], ident[:]),
                                 reads=["xb", "ident"], writes=[f"pT{pi}"], inc=(j == 7))
                        for j in range(8):
                            kc = g * 8 + j
                            eng = "dve" if j % 2 == 0 else "act"
                            if eng == "dve":
                                S.op("dve", lambda e, pi=pi, j=j, kc=kc, t=t: e.tensor_scalar(out=hT[:, kc, t * 128:(t + 1) * 128], in0=pT[pi][:, j, :], scalar1=g1[:, kc:kc + 1], scalar2=sh1[:, kc:kc + 1], op0=ALU.mult, op1=ALU.add),
                                     reads=[f"pT{pi}", "g1", "sh1"], writes=[f"hT{t}"])
                            else:
                                S.op("act", lambda e, pi=pi, j=j, kc=kc, t=t: e.activation(out=hT[:, kc, t * 128:(t + 1) * 128], in_=pT[pi][:, j, :], func=AF.Identity, scale=g1[:, kc:kc + 1], bias=sh1[:, kc:kc + 1]),
                                     reads=[f"pT{pi}", "g1", "sh1"], writes=[f"hT{t}"])
                S.barrier()

            S.set_phase(2)
            with ExitStack() as ph:
                sb = lambda n, s, d: ph.enter_context(nc.sbuf_tensor(_un(n), s, d))
                pp = lambda n, s, d: ph.enter_context(nc.psum_tensor(_un(n), s, d))
                wB = [sb(f"wB{i}", [128, KC, 256], BF16) for i in range(2)]
                qkw = sb("qkw", [128, 512], F32)
                invc = sb("invct", [128, 64], F32)
                NS = 3
                st2 = sb("st2", [128, NS, 8], F32)
                U1 = sb("U1", [128, 2 * NTOK], BF16)
                stT = U1[:].rearrange("p (a b) -> p a b", a=2)
                vst = U1[:].rearrange("p (a b) -> p a b", a=TT)
                pb2 = U1[:].bitcast(F32)
                U2 = sb("U2", [128, NTOK], F32)
                pb1 = U2[:]
                U2K = [f"U2_{s_}" for s_ in range(NS)]
                sqs = [U2[:, s_ * 640:s_ * 640 + 256] for s_ in range(NS)]
                qns = [U2[:, s_ * 640 + 256:s_ * 640 + 512] for s_ in range(NS)]
                qbs = [U2[:, s_ * 640 + 512:s_ * 640 + 640].bitcast(BF16) for s_ in range(NS)]
                pt = sb("pt", [128, NTOK], F32)
                dst0 = sb("dst0", [128, NL], BF16)
                dst = [dst0, dst0]
                bank = [pp(f"bk{i}", [128, 512], F32) for i in range(7)]
                pTqs = [bank[3 + s_][:].bitcast(BF16)[:, 0:256].rearrange("p (a b) -> p a b", a=2) for s_ in range(NS)]
                S.dma("sp", "c0", dmaf(qkw[:], qkw_rep), writes=["qkw"])
                S.dma("sp", "c0", dmaf(invc[:], invc_d), writes=["invc"])
                allhT = [f"hT{t}" for t in range(TT)]

                def loadB(b):
                    i = b % 2
                    S.dma("pool", f"wB{i}", dmaf(wB[i][:], w_in[:, b * 256:(b + 1) * 256].rearrange("(k p) n -> p k n", p=128)),
                          writes=[f"wB{i}"])

                NBLK = INW // 256
                loadB(0)
                ecnt = 0
                pend_tr = [None]
                for b in range(NBLK):
                    if b + 1 < NBLK:
                        loadB(b + 1)
                    i = b % 2
                    wk = f"wB{i}"
                    if b < 10:
                        for t in range(TT):
                            bi = t % 3
                            pq = bank[bi]
                            for kc in range(KC):
                                S.op("pe", mm(pq[:, 0:256], hT[:, kc, t * 128:(t + 1) * 128], wB[i][:, kc, :], kc == 0, kc == KC - 1),
                                     reads=[wk, f"hT{t}"], writes=[f"bk{bi}"], inc=(kc == KC - 1))
                            if b == 9:
                                S.op("act", lambda e, pq=pq, t=t: e.copy(out=vst[:, t, :], in_=pq[:, 0:256]), reads=[f"bk{bi}"], writes=["U1"])
                                continue
                            wsl = qkw[:, 0:256] if b < 8 else qkw[:, 256:512]
                            si = ecnt % NS
                            ecnt += 1
                            sq, qn, qbt, uk, sk = sqs[si], qns[si], qbs[si], U2K[si], f"st2_{si}"
                            S.op("act", lambda e, pq=pq, sq=sq: e.activation(out=sq, in_=pq[:, 0:256], func=AF.Square), reads=[f"bk{bi}"], writes=[uk])
                            S.op("dve", lambda e, sq=sq, si=si: e.tensor_reduce(out=st2[:, si, 0:4], in_=sq.rearrange("p (a b) -> p a b", a=4), axis=AX.X, op=ALU.add),
                                 reads=[uk], writes=[sk])
                            S.op("act", lambda e, si=si: e.activation(out=st2[:, si, 4:8], in_=st2[:, si, 0:4], func=AF.Sqrt, scale=1.0 / 64, bias=epst[:, 0:1]),
                                 reads=[sk, "epst"], writes=[sk])
                            S.op("dve", lambda e, si=si: e.reciprocal(out=st2[:, si, 0:4], in_=st2[:, si, 4:8]), reads=[sk], writes=[sk])
                            S.op("dve", lambda e, pq=pq, qn=qn, si=si: e.tensor_tensor(out=qn.rearrange("p (a b) -> p a b", a=4), in0=pq[:, 0:256].rearrange("p (a b) -> p a b", a=4),
                                                                    in1=st2[:, si, 0:4].unsqueeze(2).to_broadcast([128, 4, 64]), op=ALU.mult),
                                 reads=[f"bk{bi}", sk], writes=[uk])
                            qscale = 0.125 if b < 8 else 1.0
                            S.op("dve", lambda e, qbt=qbt, qn=qn, wsl=wsl, qscale=qscale: e.scalar_tensor_tensor(out=qbt, in0=qn, scalar=qscale, in1=wsl, op0=ALU.mult, op1=ALU.mult),
                                 reads=[uk, "qkw"], writes=[uk])
                            def _trq(t=t, si=si, qbt=qbt, uk=uk):
                                for j in range(2):
                                    S.op("pe", tr(pTqs[si][:, j, :], qbt[:, j * 128:(j + 1) * 128], ident[:]), reads=[uk, "ident"], writes=[f"bk{3 + si}"], inc=(j == 1))
                                S.op("act", lambda e: e.copy(out=stT[:, :, t * 128:(t + 1) * 128], in_=pTqs[si]), reads=[f"bk{3 + si}"], writes=["U1"])
                            if pend_tr[0] is not None:
                                pend_tr[0]()
                            pend_tr[0] = _trq
                        if pend_tr[0] is not None:
                            pend_tr[0]()
                            pend_tr[0] = None
                        if b == 9:
                            S.dma("sp", "vst", dmaf(v_s.rearrange("(t p) c -> p t c", p=128), vst[:]), reads=["U1"], writes=["v_s"])
                        else:
                            for j in range(2):
                                S.dma("sp", "stT", dmaf(qkT_s[b * 256 + j * 128: b * 256 + (j + 1) * 128, :], stT[:, j, :]), reads=["U1"], writes=["qkT_s"])
                    else:
                        for m in range(2):
                            is_p = b < 18
                            col0 = b * 256 + m * 128 - 2560
                            for tb in range(4):
                                for kc in range(KC):
                                    S.op("pe", mm(bank[tb][:], wB[i][:, kc, m * 128:(m + 1) * 128], hT[:, kc, 128 + tb * 512:128 + (tb + 1) * 512], kc == 0, kc == KC - 1),
                                         reads=[wk] + allhT[1 + tb * 4:1 + (tb + 1) * 4], writes=[f"bk{tb}"], inc=(kc == KC - 1))
                            di_ = ecnt % 2
                            ecnt += 1
                            if is_p:
                                for kc in range(KC):
                                    S.op("pe", mm(bank[4][:, 0:128], wB[i][:, kc, m * 128:(m + 1) * 128], hT[:, kc, 0:128], kc == 0, kc == KC - 1),
                                         reads=[wk, "hT0"], writes=["bk4"], inc=(kc == KC - 1))
                                g = col0 // 512
                                w = (2, 4, 8, 16)[g]
                                S.op("act", lambda e: e.activation(out=pt[:, 0:128], in_=bank[4][:, 0:128], func=AF.Copy, scale=flag[:, 0:1]),
                                     reads=["bk4", "flag"], writes=["pt"])
                                for tb in range(4):
                                    S.op("act", lambda e, tb=tb: e.copy(out=pt[:, 128 + tb * 512:128 + (tb + 1) * 512], in_=bank[tb][:]),
                                         reads=[f"bk{tb}"], writes=["pt"])
                                src = pt
                                srck = ["pt"]
                                sh = 1
                                bufs = [(pb1, U2K), (pb2, ["U1"])]
                                bi2 = 0
                                while sh < w:
                                    dstb, dk = bufs[bi2 % 2]
                                    lo = 2 * sh - 1
                                    S.op("dve", lambda e, dstb=dstb, src=src, sh=sh, lo=lo: e.tensor_tensor(out=dstb[:, lo:NTOK], in0=src[:, lo:NTOK], in1=src[:, lo - sh:NTOK - sh], op=ALU.add),
                                         reads=srck, writes=dk)
                                    src, srck = dstb, dk
                                    sh *= 2
                                    bi2 += 1
                                S.op("dve", lambda e, src=src, w=w, di_=di_: e.scalar_tensor_tensor(out=dst[di_][:], in0=src[:, 128:NTOK], scalar=1.0 / w, in1=pt[:, 128:NTOK], op0=ALU.mult, op1=ALU.subtract),
                                     reads=srck + ["pt"], writes=["dst0"])
                                fixb, fixk = (pb1, U2K) if src is not pb1 else (pb2, ["U1"])
                                S.op("dve", lambda e, src=src, g=g, fixb=fixb: e.tensor_tensor(out=fixb[:, 0:16], in0=src[:, 128:144], in1=invc[:, g * 16:(g + 1) * 16], op=ALU.mult),
                                     reads=srck + ["invc"], writes=fixk)
                                S.op("dve", lambda e, fixb=fixb, di_=di_: e.tensor_tensor(out=dst[di_][:, 0:16], in0=fixb[:, 0:16], in1=pt[:, 128:144], op=ALU.subtract),
                                     reads=fixk + ["pt"], writes=["dst0"])
                                S.dma("sp", "dst0", dmaf(dT_s[col0:col0 + 128, :], dst[di_][:]), reads=["dst0"], writes=["dT_s"])
                            else:
                                for tb in range(4):
                                    S.op("act", lambda e, tb=tb, di_=di_: e.activation(out=dst[di_][:, tb * 512:(tb + 1) * 512], in_=bank[tb][:], func=AF.Sigmoid),
                                         reads=[f"bk{tb}"], writes=["dst0"])
                                r0 = col0 - 2048
                                S.dma("sp", "dst0", dmaf(sgT_s[r0:r0 + 128, :], dst[di_][:]), reads=["dst0"], writes=["sgT_s"])
                S.barrier()
        if last_phase <= 2:
            S.barrier()
            return nc

        S.set_phase(3)
        with ExitStack() as ph34:
            AT = ph34.enter_context(nc.sbuf_tensor(_un("AT"), [128, 16, NL], BF16))
            with ExitStack() as ph:
                sb = lambda n, s, d: ph.enter_context(nc.sbuf_tensor(_un(n), s, d))
                pp = lambda n, s, d: ph.enter_context(nc.psum_tensor(_un(n), s, d))
                abias = sb("abias", [128, 32, 256], F32)
                esink = sb("esink", [128, 32], F32)
                vall = sb("vall", [128, TT, 256], BF16)
                _q0 = sb("qTg0", [128, 4, NTOK], BF16)
                qTg = [_q0, _q0]
                _ka = sb("kTa0", [128, NTOK], BF16)
                _kb = sb("kTb0", [128, NTOK], BF16)
                kTa = [_ka, _ka]
                kTb = [_kb, _kb]
                ct2 = sb("ct2", [128, 32], F32)
                cs2 = sb("cs2", [128, 32], BF16)
                cmat = sb("cmat", [128, 32, 128], BF16)
                wA2 = [sb(f"wA2{i}", [128, KC, 256], BF16) for i in range(2)]
                bb2 = [sb(f"bb2{i}", [128, 256], F32) for i in range(2)]
                n2s = [sb(f"n2s{i}", [128, 256], F32) for i in range(2)]
                stg2 = [sb(f"stg2{i}", [128, 256], F32) for i in range(2)]
                _pA2 = pp("pA2", [128, 256], F32)
                pA2 = [_pA2, _pA2]
                S.dma("sp", "c1", dmaf(ct2[:], cpp), writes=["ct2"])
                S.op("act", lambda e: e.activation(out=cs2[:], in_=ct2[:], func=AF.Silu), reads=["ct2"], writes=["cs2"])
                S.op("dve", lambda e: e.tensor_copy(out=cmat[:], in_=cs2[:].unsqueeze(2).to_broadcast([128, 32, 128])),
                     reads=["cs2"], writes=["cmat"])

                def ada_load(j):
                    i = j % 2
                    col0 = 2 * D + j * 256
                    S.dma("pool", f"wA2{i}", dmaf(wA2[i][:], w_ada[:, col0:col0 + 256].rearrange("(k p) n -> p k n", p=128)), writes=[f"wA2{i}"])
                    S.dma("sp", f"bb2{i}", dmaf(bb2[i][:], b_ada[col0:col0 + 256].partition_broadcast(128)), writes=[f"bb2{i}"])
                    if j // 16 == 2:
                        c4 = (j % 16) * 256
                        S.dma("sp", f"bb2{i}", dmaf(n2s[i][:], n2w[c4:c4 + 256].partition_broadcast(128)), writes=[f"n2s{i}"])

                def ada_compute(j):
                    i = j % 2
                    which = j // 16
                    c4 = (j % 16) * 256
                    for kc in range(KC):
                        S.op("pe", mm(pA2[i][:], cmat[:, kc, :], wA2[i][:, kc, :], kc == 0, kc == KC - 1),
                             reads=[f"wA2{i}", "cmat"], writes=["pA2"], inc=(kc == KC - 1))
                    S.op("dve", lambda e, i=i: e.tensor_tensor(out=stg2[i][:], in0=pA2[i][:], in1=bb2[i][:], op=ALU.add),
                         reads=["pA2", f"bb2{i}"], writes=[f"stg2{i}"])
                    if which == 2:
                        S.op("dve", lambda e, i=i: e.scalar_tensor_tensor(out=stg2[i][:], in0=stg2[i][:], scalar=1.0, in1=n2s[i][:], op0=ALU.add, op1=ALU.mult),
                             reads=[f"stg2{i}", f"n2s{i}"], writes=[f"stg2{i}"])
                    S.dma("sp", f"stg2{i}", dmaf(adab[which][:, c4:c4 + 256], stg2[i][:]), reads=[f"stg2{i}"], writes=["adab"])

                ssb = [sb(f"ssb{i}", [128, 512], F32) for i in range(2)]
                Pb = [sb(f"Pb{i}", [128, 512], BF16) for i in range(2)]
                PT = [sb(f"PT{i}", [128, 4, 128], BF16) for i in range(2)]
                rsb = sb("rsb", [128, 4, 8], F32)
                Atok = [sb(f"Atok{i}", [128, 512], BF16) for i in range(2)]
                pS = [pp(f"pS{i}", [128, 512], F32) for i in range(2)]
                pPT = [pp(f"pPT{i}", [128, 4, 128], BF16) for i in range(2)]
                pO = [pp(f"pO{i}", [128, 128], F32) for i in range(2)]
                pAT = pp("pAT", [128, 4, 128], BF16)
                S.dma("sp", "c0", dmaf(abias[:].rearrange("p a b -> p (a b)"), abias_d), writes=["abias"])
                S.dma("sp", "c0", dmaf(esink[:], sinks_d.partition_broadcast(128)), writes=["esink"])
                S.dma("sp", "c0", dmaf(vall[:], v_s.rearrange("(t p) c -> p t c", p=128)), reads=["v_s"], writes=["vall"])
                S.op("act", lambda e: e.activation(out=esink[:], in_=esink[:], func=AF.Exp), reads=["esink"], writes=["esink"])

                def loadqk(kvh):
                    i = 0
                    S.dma("sp", f"qk{i}", dmaf(qTg[i][:], qkT_s[kvh * 512:(kvh + 1) * 512, :].rearrange("(c p) t -> p c t", p=128)),
                          reads=["qkT_s"], writes=[f"qTg{i}"])
                    S.dma("sp", f"qk{i}", dmaf(kTa[i][0:64, :], qkT_s[2048 + kvh * 64:2048 + (kvh + 1) * 64, :]), reads=["qkT_s"], writes=[f"kTg{i}"])
                    S.dma("sp", f"qk{i}", dmaf(kTb[i][64:128, :], qkT_s[2048 + kvh * 64:2048 + (kvh + 1) * 64, :]), reads=["qkT_s"], writes=[f"kTg{i}"])

                S.op("pool", lambda e: e.memset(kTa[0][64:128, :], 0.0), writes=["kTg0"])
                S.op("pool", lambda e: e.memset(kTb[0][0:64, :], 0.0), writes=["kTg0"])
                ada_load(0)
                units_ = [(kvh, n, hp) for kvh in range(4) for n in range(NT) for hp in range(4)]
                NU = len(units_)

                def st_S(i):
                    kvh, n, hp = units_[i]
                    if n == 0 and hp == 0:
                        loadqk(kvh)
                    if hp == 0:
                        jb = kvh * NT + n
                        if jb + 1 < 64:
                            ada_load(jb + 1)
                        ada_compute(jb)
                    ui = i % 2
                    ri = i % 4
                    h0 = kvh * 8 + 2 * hp
                    qc0 = 128 + n * 128
                    kc0 = n * 128
                    S.op("pe", mm(pS[ui][:, 0:256], qTg[0][:, hp, qc0:qc0 + 128], kTa[0][:, kc0:kc0 + 256], True, True),
                         reads=["qTg0", "kTg0"], writes=[f"pS{ui}"], inc=False)
                    S.op("pe", mm(pS[ui][:, 256:512], qTg[0][:, hp, qc0:qc0 + 128], kTb[0][:, kc0:kc0 + 256], True, True),
                         reads=["qTg0", "kTg0"], writes=[f"pS{ui}"], inc=True)
                    S.op("dve", lambda e: e.tensor_tensor(out=ssb[ui][:].rearrange("p (a b) -> p a b", a=2), in0=pS[ui][:].rearrange("p (a b) -> p a b", a=2), in1=abias[:, h0:h0 + 2, :], op=ALU.add),
                         reads=[f"pS{ui}", "abias"], writes=[f"ssb{ui}"])
                    if n == 0:
                        S.op("dve", lambda e: e.tensor_scalar(out=ssb[ui][:].rearrange("p (a b) -> p a b", a=2)[:, :, 0:128], in0=ssb[ui][:].rearrange("p (a b) -> p a b", a=2)[:, :, 0:128], scalar1=flag[:, 1:2], scalar2=None, op0=ALU.add),
                             reads=[f"ssb{ui}", "flag"], writes=[f"ssb{ui}"])
                    for a_ in range(2):
                        S.op("act", lambda e, a_=a_: e.activation(out=Pb[ui][:, a_ * 256:(a_ + 1) * 256], in_=ssb[ui][:, a_ * 256:(a_ + 1) * 256], func=AF.Exp, accum_out=rsb[:, ri, a_:a_ + 1]),
                             reads=[f"ssb{ui}"], writes=[f"Pb{ui}", f"rsb{ri}"])
                    S.op("dve", lambda e: e.tensor_tensor(out=rsb[:, ri, 2:4], in0=rsb[:, ri, 0:2], in1=esink[:, h0:h0 + 2], op=ALU.add),
                         reads=[f"rsb{ri}", "esink"], writes=[f"rsb{ri}"])
                    S.op("dve", lambda e: e.reciprocal(out=rsb[:, ri, 4:6], in_=rsb[:, ri, 2:4]), reads=[f"rsb{ri}"], writes=[f"rsb{ri}"])

                def st_T(i):
                    ui = i % 2
                    for j in range(4):
                        S.op("pe", tr(pPT[ui][:, j, :], Pb[ui][:, j * 128:(j + 1) * 128], ident[:]), reads=[f"Pb{ui}", "ident"], writes=[f"pPT{ui}"], inc=(j == 3))
                    if i % 2 == 0:
                        S.op("act", lambda e: e.copy(out=PT[ui][:], in_=pPT[ui][:]), reads=[f"pPT{ui}"], writes=[f"PT{ui}"])
                    else:
                        S.op("dve", lambda e: e.tensor_copy(out=PT[ui][:], in_=pPT[ui][:]), reads=[f"pPT{ui}"], writes=[f"PT{ui}"])

                def st_O(i):
                    kvh, n, hp = units_[i]
                    ui = i % 2
                    ri = i % 4
                    ai = (kvh * NT + n) % 2
                    vs = slice(kvh * 64, (kvh + 1) * 64)
                    S.op("pe", mm(pO[ui][:, 0:64], PT[ui][:, 0, :], vall[:, n, vs], True, False), reads=[f"PT{ui}", "vall"], writes=[f"pO{ui}"], inc=False)
                    S.op("pe", mm(pO[ui][:, 0:64], PT[ui][:, 1, :], vall[:, n + 1, vs], False, True), reads=[f"PT{ui}", "vall"], writes=[f"pO{ui}"], inc=False)
                    S.op("pe", mm(pO[ui][:, 64:128], PT[ui][:, 2, :], vall[:, n, vs], True, False), reads=[f"PT{ui}", "vall"], writes=[f"pO{ui}"], inc=False)
                    S.op("pe", mm(pO[ui][:, 64:128], PT[ui][:, 3, :], vall[:, n + 1, vs], False, True), reads=[f"PT{ui}", "vall"], writes=[f"pO{ui}"], inc=True)
                    for a_ in range(2):
                        S.op("dve", lambda e, a_=a_: e.tensor_scalar(out=Atok[ai][:, hp * 128 + a_ * 64:hp * 128 + (a_ + 1) * 64], in0=pO[ui][:, a_ * 64:(a_ + 1) * 64], scalar1=rsb[:, ri, 4 + a_:5 + a_], scalar2=None, op0=ALU.mult),
                             reads=[f"pO{ui}", f"rsb{ri}"], writes=[f"Atok{ai}"])

                def st_A(i):
                    kvh, n, hp = units_[i]
                    ai = (kvh * NT + n) % 2
                    for j in range(4):
                        S.op("pe", tr(pAT[:, j, :], Atok[ai][:, j * 128:(j + 1) * 128], ident[:]), reads=[f"Atok{ai}", "ident"], writes=["pAT"], inc=(j == 3))
                    S.op("act", lambda e: e.copy(out=AT[:, kvh * 4:(kvh + 1) * 4, n * 128:(n + 1) * 128], in_=pAT[:]), reads=["pAT"], writes=[f"AT{n}"])

                for i in range(NU + 4):
                    if i < NU:
                        st_S(i)
                    if 0 <= i - 1 < NU:
                        st_T(i - 1)
                    if 0 <= i - 2 < NU:
                        st_O(i - 2)
                    if 0 <= i - 3 < NU and units_[i - 3][2] == 3:
                        st_A(i - 3)
                S.barrier()

            S.set_phase(35)
            BT = ph34.enter_context(nc.sbuf_tensor(_un("BT"), [128, 16, NL], BF16))
            with ExitStack() as ph:
                sb = lambda n, s, d: ph.enter_context(nc.sbuf_tensor(_un(n), s, d))
                pp = lambda n, s, d: ph.enter_context(nc.psum_tensor(_un(n), s, d))
                wp = [sb(f"wp{i}", [128, 4, 512], BF16) for i in range(2)]
                dTg = [sb(f"dTg{i}", [128, 4, NL], BF16) for i in range(2)]
                psc = sb("psc", [128, 16], F32)
                bank = [pp(f"bq{i}", [128, 512], F32) for i in range(4)]
                S.dma("sp", "c0", dmaf(psc[:], pscale_pp), writes=["psc"])
                for g in range(4):
                    i = g % 2
                    S.dma("pool", f"wp{i}", dmaf(wp[i][:], w_pool[g].rearrange("(c p) n -> p c n", p=128)), writes=[f"wp{i}"])
                    S.dma("sp", f"dTg{i}", dmaf(dTg[i][:], dT_s[g * 512:(g + 1) * 512, :].rearrange("(c p) t -> p c t", p=128)), reads=["dT_s"], writes=[f"dTg{i}"])
                    for m in range(4):
                        for tb in range(4):
                            bi = (m * 4 + tb) % 4
                            for cc in range(4):
                                S.op("pe", mm(bank[bi][:], wp[i][:, cc, m * 128:(m + 1) * 128], dTg[i][:, cc, tb * 512:(tb + 1) * 512], cc == 0, cc == 3),
                                     reads=[f"wp{i}", f"dTg{i}"], writes=[f"bq{bi}"], inc=(cc == 3))
                            S.op("act", lambda e, bi=bi, g=g, m=m, tb=tb: e.activation(out=BT[:, g * 4 + m, tb * 512:(tb + 1) * 512], in_=bank[bi][:], func=AF.Copy, scale=psc[:, g * 4 + m:g * 4 + m + 1]),
                                 reads=[f"bq{bi}", "psc"], writes=["BT"])
                S.barrier()

            S.set_phase(4)
            with ExitStack() as ph:
                sb = lambda n, s, d: ph.enter_context(nc.sbuf_tensor(_un(n), s, d))
                pp = lambda n, s, d: ph.enter_context(nc.psum_tensor(_un(n), s, d))
                wUa = [sb(f"wUa{i}", [128, 16, 256], BF16) for i in range(2)]
                wUb = [sb(f"wUb{i}", [128, 16, 256], BF16) for i in range(2)]
                sga = [sb(f"sga{i}", [128, NL], BF16) for i in range(2)]
                sgb = [sb(f"sgb{i}", [128, NL], BF16) for i in range(2)]
                t1 = [sb(f"t1{i}", [128, 512], F32) for i in range(2)]
                t2 = [sb(f"t2{i}", [128, 512], F32) for i in range(2)]
                mx = [sb(f"mx{i}", [128, NL], BF16) for i in range(2)]
                pa = [pp(f"pa{i}", [128, 512], F32) for i in range(2)]
                pb = [pp(f"pb{i}", [128, 512], F32) for i in range(2)]
                allAT = [f"AT{n}" for n in range(NT)]

                def loadU(cb):
                    i = cb % 2
                    S.dma("pool", f"wUa{i}", dmaf(wUa[i][:], w_aup[:, cb * 256:(cb + 1) * 256].rearrange("(k p) n -> p k n", p=128)), writes=[f"wUa{i}"])
                    S.dma("pool", f"wUb{i}", dmaf(wUb[i][:], w_bup[:, cb * 256:(cb + 1) * 256].rearrange("(k p) n -> p k n", p=128)), writes=[f"wUb{i}"])

                loadU(0)
                u = 0
                for cb in range(16):
                    if cb + 1 < 16:
                        loadU(cb + 1)
                    i = cb % 2
                    for m2 in range(2):
                        M = cb * 2 + m2
                        mi = M % 2
                        S.dma("sp", f"sga{mi}", dmaf(sga[mi][:], sgT_s[M * 128:(M + 1) * 128, :]), reads=["sgT_s"], writes=[f"sga{mi}"])
                        S.dma("sp", f"sgb{mi}", dmaf(sgb[mi][:], sgT_s[D + M * 128:D + (M + 1) * 128, :]), reads=["sgT_s"], writes=[f"sgb{mi}"])
                        for tb in range(4):
                            ui = u % 2
                            u += 1
                            ts_ = slice(tb * 512, (tb + 1) * 512)
                            for kc in range(16):
                                S.op("pe", mm(pa[ui][:], wUa[i][:, kc, m2 * 128:(m2 + 1) * 128], AT[:, kc, ts_], kc == 0, kc == 15),
                                     reads=[f"wUa{i}"] + allAT[tb * 4:(tb + 1) * 4], writes=[f"pa{ui}"], inc=(kc == 15))
                            for kc in range(16):
                                S.op("pe", mm(pb[ui][:], wUb[i][:, kc, m2 * 128:(m2 + 1) * 128], BT[:, kc, ts_], kc == 0, kc == 15),
                                     reads=[f"wUb{i}", "BT"], writes=[f"pb{ui}"], inc=(kc == 15))
                            S.op("dve", lambda e, ui=ui, mi=mi, ts_=ts_: e.tensor_tensor(out=t1[ui][:], in0=pa[ui][:], in1=sga[mi][:, ts_], op=ALU.mult),
                                 reads=[f"pa{ui}", f"sga{mi}"], writes=[f"t1{ui}"])
                            S.op("dve", lambda e, ui=ui, mi=mi, ts_=ts_: e.tensor_tensor(out=t2[ui][:], in0=pb[ui][:], in1=sgb[mi][:, ts_], op=ALU.mult),
                                 reads=[f"pb{ui}", f"sgb{mi}"], writes=[f"t2{ui}"])
                            S.op("dve", lambda e, ui=ui, mi=mi, ts_=ts_: e.tensor_tensor(out=mx[mi][:, ts_], in0=t1[ui][:], in1=t2[ui][:], op=ALU.add),
                                 reads=[f"t1{ui}", f"t2{ui}"], writes=[f"mx{mi}"])
                        S.dma("sp", f"mx{mi}", dmaf(mixT_s[M * 128:(M + 1) * 128, :], mx[mi][:]), reads=[f"mx{mi}"], writes=["mixT_s"])
                S.barrier()
        if last_phase <= 4:
            S.barrier()
            return nc

        S.set_phase(5)
        with ExitStack() as ph:
            sb = lambda n, s, d: ph.enter_context(nc.sbuf_tensor(_un(n), s, d))
            pp = lambda n, s, d: ph.enter_context(nc.psum_tensor(_un(n), s, d))
            mixT = sb("mixT", [128, KC, NL], BF16)
            gate1b = sb("gate1b", [128, D], F32)
            wO = [sb(f"wO{i}", [128, KC, 256], BF16) for i in range(2)]
            xin = [sb(f"xin{i}", [128, 256], F32) for i in range(4)]
            o5 = [sb(f"o5{i}", [128, 256], F32) for i in range(4)]
            po = [pp(f"po{i}", [128, 512], F32) for i in range(4)]
            for i in range(8):
                S.dma("sp", f"mixT{i}", dmaf(mixT[:, 4 * i:4 * i + 4, :], mixT_s[4 * i * 128:(4 * i + 4) * 128, :].rearrange("(c p) t -> p c t", p=128)),
                      reads=["mixT_s"], writes=[f"mixT{i}"])
            S.dma("sp", "c1", dmaf(gate1b[:], adab[0]), reads=["adab"], writes=["gate1b"])
            allmix = [f"mixT{i}" for i in range(8)]

            def loadO(cb):
                i = cb % 2
                S.dma("pool", f"wO{i}", dmaf(wO[i][:], w_out[:, cb * 256:(cb + 1) * 256].rearrange("(k p) n -> p k n", p=128)), writes=[f"wO{i}"])

            loadO(0)
            u = 0
            for cb in range(16):
                if cb + 1 < 16:
                    loadO(cb + 1)
                i = cb % 2
                cs_ = slice(cb * 256, (cb + 1) * 256)
                for t in range(NT):
                    ui = u % 4
                    u += 1
                    S.dma("sp", f"xin{ui}", dmaf(xin[ui][:], xh[128 + t * 128:128 + (t + 1) * 128, cs_]), writes=[f"xin{ui}"])
                    for kc in range(KC):
                        S.op("pe", mm(po[ui][:, 0:256], mixT[:, kc, t * 128:(t + 1) * 128], wO[i][:, kc, :], kc == 0, kc == KC - 1),
                             reads=[f"wO{i}"] + (allmix if (cb == 0 and t == 0) else []), writes=[f"po{ui}"], inc=(kc == KC - 1))
                    S.op("dve", lambda e, ui=ui, cs_=cs_: e.tensor_tensor(out=o5[ui][:], in0=po[ui][:, 0:256], in1=gate1b[:, cs_], op=ALU.mult),
                         reads=[f"po{ui}", "gate1b"], writes=[f"o5{ui}"])
                    S.op("dve", lambda e, ui=ui: e.tensor_tensor(out=o5[ui][:], in0=o5[ui][:], in1=xin[ui][:], op=ALU.add),
                         reads=[f"o5{ui}", f"xin{ui}"], writes=[f"o5{ui}"])
                    S.dma("sp", f"o5{ui}", dmaf(x1_s[t * 128:(t + 1) * 128, cs_], o5[ui][:]), reads=[f"o5{ui}"], writes=["x1_s"])
            S.barrier()
        if last_phase <= 5:
            S.barrier()
            return nc

        S.set_phase(6)
        with ExitStack() as ph:
            sb = lambda n, s, d: ph.enter_context(nc.sbuf_tensor(_un(n), s, d))
            pp = lambda n, s, d: ph.enter_context(nc.psum_tensor(_un(n), s, d))
            g2b = sb("g2b", [128, D], F32)
            sh2b = sb("sh2b", [128, D], F32)
            x1t = [sb(f"x1t{i}", [128, D], F32) for i in range(2)]
            h2 = sb("h2", [128, D], F32)
            h2b = [sb(f"h2b{i}", [128, D], BF16) for i in range(2)]
            h2T = sb("h2T", [128, KC, 128], F32)
            wr = sb("wrt", [128, KC, 72], F32)
            rbb = sb("rbb", [128, 72], F32)
            lg = sb("lg", [128, 72], F32)
            sc = sb("sc", [128, 256], F32)
            posb = sb("posb", [128, 64], F32)
            ovt = sb("ovt", [128, 64], F32)
            tmp64 = sb("tmp64", [128, 64], F32)
            eb = sb("eb", [128, 64], BF16)
            eacc = sb("eacc", [128, 64], F32)
            eaccb = sb("eaccb", [128, 64], BF16)
            st6 = sb("st6", [128, NT, 4], F32)
            ptr = [pp(f"ptr{i}", [128, 512], F32) for i in range(2)]
            pl = pp("pl", [128, 128], F32)
            ppos = pp("ppos", [128, 64], F32)
            S.dma("sp", "c0", dmaf(g2b[:], adab[2]), reads=["adab"], writes=["g2b"])
            S.dma("sp", "c0", dmaf(sh2b[:], adab[1]), reads=["adab"], writes=["sh2b"])
            S.dma("sp", "c1", dmaf(wr[:], wr_d.rearrange("(k p) n -> p k n", p=128)), writes=["wr"])
            S.dma("sp", "c1", dmaf(rbb[:], rb_d.partition_broadcast(128)), writes=["rbb"])
            zt = sb("zt", [128, D], BF16)
            S.op("pool", lambda e: e.memset(zt[:], 0.0), writes=["zt"])
            for r in range(NE):
                S.dma("sp", "zf", dmaf(xs_buf[r * CAP:(r + 1) * CAP, :], zt[:]), reads=["zt"], writes=["xs_buf"])
            S.dma("sp", "zf", dmaf(xs_buf[ZROW:ZROW + 1, :], zt[0:1, :]), reads=["zt"], writes=["xs_buf"])
            S.op("dve", lambda e: e.memset(eacc[:], 0.0), writes=["eacc"])
            S.op("dve", lambda e: e.memset(eaccb[:], 0.0), writes=["eaccb"])
            gmax, negg, gsum, pgrp = sc[:, 0:1], sc[:, 1:2], sc[:, 2:3], sc[:, 3:4]
            m1, m2, dlt, e2, w1, w2, d1f, d2f = [sc[:, k:k + 1] for k in range(4, 12)]
            ohg, ein, oh1, msk, oh2, eg = [sc[:, 16 + 8 * k:24 + 8 * k] for k in range(6)]
            t88 = sc[:, 64:128]
            E1 = sc[:, 128:192]
            E2 = sc[:, 192:256]
            v88 = lambda ap: ap.rearrange("p (g j) -> p g j", g=8)

            def dv(fn, extra_r=(), extra_w=()):
                S.op("dve", fn, reads=["sc"] + list(extra_r), writes=["sc"] + list(extra_w))

            S.dma("sp", "x1t0", dmaf(x1t[0][:], x1_s[0:128, :]), reads=["x1_s"], writes=["x1t0"])
            for t in range(NT):
                i = t % 2
                if t + 1 < NT:
                    S.dma("sp", f"x1t{1 - i}", dmaf(x1t[1 - i][:], x1_s[(t + 1) * 128:(t + 2) * 128, :]), reads=["x1_s"], writes=[f"x1t{1 - i}"])
                S.op("act", lambda e, i=i, t=t: e.activation(out=h2b[i][:], in_=x1t[i][:], func=AF.Square, accum_out=st6[:, t, 0:1]),
                     reads=[f"x1t{i}"], writes=[f"h2b{i}", "st6"])
                S.op("act", lambda e, t=t: e.activation(out=st6[:, t, 1:2], in_=st6[:, t, 0:1], func=AF.Sqrt, scale=1.0 / D, bias=epst[:, 0:1]),
                     reads=["st6", "epst"], writes=["st6"])
                S.op("dve", lambda e, t=t: e.reciprocal(out=st6[:, t, 2:3], in_=st6[:, t, 1:2]), reads=["st6"], writes=["st6"])
                S.op("dve", lambda e, i=i, t=t: e.scalar_tensor_tensor(out=h2[:], in0=x1t[i][:], scalar=st6[:, t, 2:3], in1=g2b[:], op0=ALU.mult, op1=ALU.mult),
                     reads=[f"x1t{i}", "st6", "g2b"], writes=["h2"])
                S.op("pool", lambda e: e.tensor_tensor(out=h2[:], in0=h2[:], in1=sh2b[:], op=ALU.add), reads=["h2", "sh2b"], writes=["h2"])
                S.op("act", lambda e, i=i: e.copy(out=h2b[i][:], in_=h2[:]), reads=["h2"], writes=[f"h2b{i}"])
                for g4 in range(8):
                    pi = g4 % 2
                    for j in range(4):
                        kc = g4 * 4 + j
                        S.op("pe", tr(ptr[pi][:, j * 128:(j + 1) * 128], h2[:, kc * 128:(kc + 1) * 128], identf), reads=["h2", "cst"], writes=[f"ptr{pi}"], inc=(j == 3))
                    if g4 % 2 == 0:
                        S.op("act", lambda e, pi=pi, g4=g4: e.copy(out=h2T[:, g4 * 4:(g4 + 1) * 4, :].rearrange("p a b -> p (a b)"), in_=ptr[pi][:]), reads=[f"ptr{pi}"], writes=["h2T"])
                    else:
                        S.op("dve", lambda e, pi=pi, g4=g4: e.tensor_copy(out=h2T[:, g4 * 4:(g4 + 1) * 4, :].rearrange("p a b -> p (a b)"), in_=ptr[pi][:]), reads=[f"ptr{pi}"], writes=["h2T"])
                for kc in range(KC):
                    S.op("pe", mm(pl[:, 0:72], h2T[:, kc, :], wr[:, kc, :], kc == 0, kc == KC - 1), reads=["h2T", "wr"], writes=["pl"], inc=(kc == KC - 1))
                S.op("dve", lambda e: e.tensor_tensor(out=lg[:], in0=pl[:, 0:72], in1=rbb[:], op=ALU.add), reads=["pl", "rbb"], writes=["lg"])
                dv(lambda e: e.reduce_max(out=gmax, in_=lg[:, 0:8], axis=AX.X), ["lg"])
                dv(lambda e: e.tensor_scalar(out=ohg, in0=lg[:, 0:8], scalar1=gmax, scalar2=None, op0=ALU.is_equal), ["lg"])
                dv(lambda e: e.tensor_scalar(out=negg, in0=gmax, scalar1=-1.0, scalar2=None, op0=ALU.mult))
                S.op("act", lambda e: e.activation(out=eg, in_=lg[:, 0:8], func=AF.Exp, bias=negg, accum_out=gsum), reads=["sc", "lg"], writes=["sc"])
                dv(lambda e: e.reciprocal(out=pgrp, in_=gsum))
                dv(lambda e: e.tensor_tensor(out=v88(t88), in0=v88(lg[:, 8:72]), in1=ohg.unsqueeze(2).to_broadcast([128, 8, 8]), op=ALU.mult), ["lg"])
                dv(lambda e: e.tensor_reduce(out=ein, in_=t88.rearrange("p (g j) -> p j g", g=8), axis=AX.X, op=ALU.add))
                dv(lambda e: e.reduce_max(out=m1, in_=ein, axis=AX.X))
                dv(lambda e: e.tensor_scalar(out=oh1, in0=ein, scalar1=m1, scalar2=None, op0=ALU.is_equal))
                dv(lambda e: e.scalar_tensor_tensor(out=msk, in0=oh1, scalar=-1e30, in1=ein, op0=ALU.mult, op1=ALU.add))
                dv(lambda e: e.reduce_max(out=m2, in_=msk, axis=AX.X))
                dv(lambda e: e.tensor_scalar(out=oh2, in0=msk, scalar1=m2, scalar2=None, op0=ALU.is_equal))
                dv(lambda e: e.tensor_tensor(out=dlt, in0=m2, in1=m1, op=ALU.subtract))
                S.op("act", lambda e: e.activation(out=e2, in_=dlt, func=AF.Exp), reads=["sc"], writes=["sc"])
                dv(lambda e: e.tensor_scalar(out=w1, in0=e2, scalar1=1.0, scalar2=None, op0=ALU.add))
                dv(lambda e: e.reciprocal(out=w1, in_=w1))
                dv(lambda e: e.tensor_tensor(out=w2, in0=e2, in1=w1, op=ALU.mult))
                dv(lambda e, t=t: e.tensor_scalar(out=cw[:, t, 0:1], in0=w1, scalar1=pgrp, scalar2=None, op0=ALU.mult), [], ["cw"])
                dv(lambda e, t=t: e.tensor_scalar(out=cw[:, t, 1:2], in0=w2, scalar1=pgrp, scalar2=None, op0=ALU.mult), [], ["cw"])
                dv(lambda e: e.tensor_tensor(out=v88(E1), in0=ohg.unsqueeze(2).to_broadcast([128, 8, 8]), in1=oh1.unsqueeze(1).to_broadcast([128, 8, 8]), op=ALU.mult))
                dv(lambda e: e.tensor_tensor(out=v88(E2), in0=ohg.unsqueeze(2).to_broadcast([128, 8, 8]), in1=oh2.unsqueeze(1).to_broadcast([128, 8, 8]), op=ALU.mult))
                dv(lambda e: e.tensor_tensor(out=eb[:], in0=E1, in1=E2, op=ALU.add), [], ["eb"])
                S.op("pe", mm(ppos[:], tri[:], eb[:], True, False), reads=["tri", "eb"], writes=["ppos"], inc=False)
                S.op("pe", mm(ppos[:], ones[:], eaccb[:], False, True), reads=["ones", "eaccb"], writes=["ppos"], inc=True)
                S.op("dve", lambda e: e.tensor_tensor(out=posb[:], in0=ppos[:], in1=ebase, op=ALU.add), reads=["ppos", "cst"], writes=["posb"])
                S.op("dve", lambda e: e.tensor_scalar(out=ovt[:], in0=ppos[:], scalar1=float(CAP), scalar2=None, op0=ALU.is_ge), reads=["ppos"], writes=["ovt"])
                S.op("dve", lambda e: e.tensor_scalar(out=tmp64[:], in0=posb[:], scalar1=-1.0, scalar2=float(ZROW), op0=ALU.mult, op1=ALU.add), reads=["posb"], writes=["tmp64"])
                S.op("dve", lambda e: e.tensor_tensor(out=tmp64[:], in0=tmp64[:], in1=ovt[:], op=ALU.mult), reads=["tmp64", "ovt"], writes=["tmp64"])
                S.op("dve", lambda e: e.tensor_tensor(out=posb[:], in0=posb[:], in1=tmp64[:], op=ALU.add), reads=["posb", "tmp64"], writes=["posb"])
                dv(lambda e: e.tensor_tensor(out=t88, in0=E1, in1=posb[:], op=ALU.mult), ["posb"])
                dv(lambda e: e.reduce_sum(out=d1f, in_=t88, axis=AX.X))
                dv(lambda e: e.tensor_tensor(out=t88, in0=E2, in1=posb[:], op=ALU.mult), ["posb"])
                dv(lambda e: e.reduce_sum(out=d2f, in_=t88, axis=AX.X))
                dv(lambda e, t=t: e.tensor_copy(out=di[:, t, 0:1], in_=d1f), [], ["di"])
                dv(lambda e, t=t: e.tensor_copy(out=di[:, t, 1:2], in_=d2f), [], ["di"])
                S.op("dve", lambda e: e.tensor_tensor(out=eacc[:], in0=eacc[:], in1=eb[:], op=ALU.add), reads=["eacc", "eb"], writes=["eacc"])
                S.op("dve", lambda e: e.tensor_copy(out=eaccb[:], in_=eacc[:]), reads=["eacc"], writes=["eaccb"])
                for k2 in range(2):
                    S.dma("pool", f"scat{k2}", lambda e, t=t, k2=k2, i=i: e.indirect_dma_start(
                        out=xs_buf, out_offset=bass.IndirectOffsetOnAxis(ap=di[:, t, k2:k2 + 1], axis=0),
                        in_=h2b[i][:], in_offset=None, bounds_check=bcreg, oob_is_err=False),
                        reads=[f"h2b{i}", "di"], writes=["xs_buf"])
            if "dbg_s" in debug:
                S.dma("sp", "c0", dmaf(dbg_s[:, 0:32], cw[:].rearrange("p a b -> p (a b)")), reads=["cw"], writes=["dbg"])
            S.barrier()
        if last_phase <= 6:
            S.barrier()
            return nc

        S.set_phase(7)
        with ExitStack() as ph:
            sb = lambda n, s, d: ph.enter_context(nc.sbuf_tensor(_un(n), s, d))
            pp = lambda n, s, d: ph.enter_context(nc.psum_tensor(_un(n), s, d))
            NR = 14
            ring = [sb(f"ring{i}", [128, D], BF16) for i in range(NR)]
            xs = [sb(f"xs{i}", [128, D], BF16) for i in range(2)]
            xsT = [sb(f"xsT{i}", [128, KC, 128], BF16) for i in range(2)]
            sg = sb("sg", [128, 512], F32)
            act = sb("actb", [128, 512], BF16)
            actT = sb("actT", [128, 4, 128], BF16)
            yst = [sb(f"yst{i}", [128, D], F32) for i in range(2)]
            zrow = sb("zrow", [1, D], F32)
            pxT = [pp(f"pxT{i}", [128, 8, 128], BF16) for i in range(2)]
            pg = pp("pg", [128, 512], F32)
            pu = pp("pu", [128, 512], F32)
            paT = pp("paT", [128, 4, 128], BF16)
            py = [pp(f"py{i}", [128, 512], F32) for i in range(2)]
            S.op("dve", lambda e: e.memset(zrow[:], 0.0), writes=["zrow"])
            S.dma("sp", "c0", dmaf(y_s[ZROW:ZROW + 1, :], zrow[:]), reads=["zrow"], writes=["y_s"])
            units = []
            for e_ in range(NE):
                for kg in range(4):
                    units.append(("g", e_, kg))
                    units.append(("u", e_, kg))
                for db in range(4):
                    units.append(("d", e_, db))
            nload = [0]

            def ensure_loaded(upto):
                while nload[0] < min(len(units), upto):
                    j = nload[0]
                    kind, e_, k_ = units[j]
                    s_ = j % NR
                    if kind == "g":
                        src = w_gate[e_][k_ * 1024:(k_ + 1) * 1024, :].rearrange("(c p) n -> p c n", p=128)
                        dstv = ring[s_][:].rearrange("p (c n) -> p c n", c=8)
                    elif kind == "u":
                        src = w_up[e_][k_ * 1024:(k_ + 1) * 1024, :].rearrange("(c p) n -> p c n", p=128)
                        dstv = ring[s_][:].rearrange("p (c n) -> p c n", c=8)
                    else:
                        src = w_down[e_][:, k_ * 1024:(k_ + 1) * 1024].rearrange("(c p) n -> p c n", p=128)
                        dstv = ring[s_][:].rearrange("p (c n) -> p c n", c=4)
                    S.dma("pool", f"ring{s_}", dmaf(dstv, src), writes=[f"ring{s_}"])
                    nload[0] += 1

            ui = 0
            cpy = 0
            for e_ in range(NE):
                i = e_ % 2
                S.dma("sp", f"xs{i}", dmaf(xs[i][:], xs_buf[e_ * CAP:(e_ + 1) * CAP, :]), reads=["xs_buf"], writes=[f"xs{i}"])
                for g8 in range(4):
                    pi = g8 % 2
                    for j in range(8):
                        kc = g8 * 8 + j
                        S.op("pe", tr(pxT[pi][:, j, :], xs[i][:, kc * 128:(kc + 1) * 128], ident[:]), reads=[f"xs{i}", "ident"], writes=[f"pxT{pi}"], inc=(j == 7))
                    if g8 % 2 == 0:
                        S.op("act", lambda e, pi=pi, g8=g8, i=i: e.copy(out=xsT[i][:, g8 * 8:(g8 + 1) * 8, :], in_=pxT[pi][:]), reads=[f"pxT{pi}"], writes=[f"xsT{i}"])
                    else:
                        S.op("dve", lambda e, pi=pi, g8=g8, i=i: e.tensor_copy(out=xsT[i][:, g8 * 8:(g8 + 1) * 8, :], in_=pxT[pi][:]), reads=[f"pxT{pi}"], writes=[f"xsT{i}"])
                for kg in range(4):
                    for kind, pacc, pk in (("g", pg, "pg"), ("u", pu, "pu")):
                        ensure_loaded(ui + NR)
                        s_ = ui % NR
                        assert units[ui] == (kind, e_, kg)
                        wv = ring[s_][:].rearrange("p (c n) -> p c n", c=8)
                        for j in range(8):
                            kc = kg * 8 + j
                            S.op("pe", mm(pacc[:], xsT[i][:, kc, :], wv[:, j, :], kc == 0, kc == KC - 1),
                                 reads=[f"ring{s_}", f"xsT{i}"], writes=[pk], inc=(j == 7))
                        ui += 1
                S.op("act", lambda e: e.activation(out=sg[:], in_=pg[:], func=AF.Silu), reads=["pg"], writes=["sg"])
                S.op("dve", lambda e: e.tensor_tensor(out=act[:], in0=sg[:], in1=pu[:], op=ALU.mult), reads=["sg", "pu"], writes=["act"])
                for j in range(4):
                    S.op("pe", tr(paT[:, j, :], act[:, j * 128:(j + 1) * 128], ident[:]), reads=["act", "ident"], writes=["paT"], inc=(j == 3))
                S.op("act", lambda e: e.copy(out=actT[:], in_=paT[:]), reads=["paT"], writes=["actT"])
                for db in range(4):
                    ensure_loaded(ui + NR)
                    s_ = ui % NR
                    assert units[ui] == ("d", e_, db)
                    wv = ring[s_][:].rearrange("p (c n) -> p c n", c=4)
                    for dj in range(2):
                        yi = cpy % 2
                        cpy += 1
                        for fc in range(4):
                            S.op("pe", mm(py[yi][:], actT[:, fc, :], wv[:, fc, dj * 512:(dj + 1) * 512], fc == 0, fc == 3),
                                 reads=[f"ring{s_}", "actT"], writes=[f"py{yi}"], inc=(fc == 3))
                        c0 = db * 1024 + dj * 512
                        if yi == 0:
                            S.op("act", lambda e, yi=yi, i=i, c0=c0: e.copy(out=yst[i][:, c0:c0 + 512], in_=py[yi][:]), reads=[f"py{yi}"], writes=[f"yst{i}"])
                        else:
                            S.op("dve", lambda e, yi=yi, i=i, c0=c0: e.tensor_copy(out=yst[i][:, c0:c0 + 512], in_=py[yi][:]), reads=[f"py{yi}"], writes=[f"yst{i}"])
                    ui += 1
                S.dma("sp", f"yst{i}", dmaf(y_s[e_ * CAP:(e_ + 1) * CAP, :], yst[i][:]), reads=[f"yst{i}"], writes=["y_s"])
            S.barrier()
        if last_phase <= 7:
            S.barrier()
            return nc

        S.set_phase(8)
        with ExitStack() as ph:
            sb = lambda n, s, d: ph.enter_context(nc.sbuf_tensor(_un(n), s, d))
            gate2b = sb("gate2b", [128, D], F32)
            Y1 = [sb(f"Y1{i}", [128, D], F32) for i in range(2)]
            Y2 = [sb(f"Y2{i}", [128, D], F32) for i in range(2)]
            x1t = [sb(f"x1u{i}", [128, D], F32) for i in range(2)]
            acc = sb("acc8", [128, D], F32)
            o8 = [sb(f"o8{i}", [128, D], F32) for i in range(2)]
            S.dma("sp", "c0", dmaf(gate2b[:], adab[3]), reads=["adab"], writes=["gate2b"])
            for t in range(NT):
                i = t % 2
                S.dma("pool", f"Y1{i}", lambda e, t=t, i=i: e.indirect_dma_start(
                    out=Y1[i][:], out_offset=None, in_=y_s, in_offset=bass.IndirectOffsetOnAxis(ap=di[:, t, 0:1], axis=0),
                    bounds_check=bcreg, oob_is_err=False), reads=["y_s", "di"], writes=[f"Y1{i}"])
                S.dma("pool", f"Y2{i}", lambda e, t=t, i=i: e.indirect_dma_start(
                    out=Y2[i][:], out_offset=None, in_=y_s, in_offset=bass.IndirectOffsetOnAxis(ap=di[:, t, 1:2], axis=0),
                    bounds_check=bcreg, oob_is_err=False), reads=["y_s", "di"], writes=[f"Y2{i}"])
                S.dma("sp", f"x1u{i}", dmaf(x1t[i][:], x1_s[t * 128:(t + 1) * 128, :]), reads=["x1_s"], writes=[f"x1u{i}"])
                S.op("act", lambda e, t=t, i=i: e.activation(out=acc[:], in_=Y1[i][:], func=AF.Copy, scale=cw[:, t, 0:1]), reads=[f"Y1{i}", "cw"], writes=["acc"])
                S.op("dve", lambda e, t=t, i=i: e.scalar_tensor_tensor(out=acc[:], in0=Y2[i][:], scalar=cw[:, t, 1:2], in1=acc[:], op0=ALU.mult, op1=ALU.add),
                     reads=[f"Y2{i}", "cw", "acc"], writes=["acc"])
                S.op("dve", lambda e: e.tensor_tensor(out=acc[:], in0=acc[:], in1=gate2b[:], op=ALU.mult), reads=["acc", "gate2b"], writes=["acc"])
                S.op("dve", lambda e, i=i: e.tensor_tensor(out=o8[i][:], in0=acc[:], in1=x1t[i][:], op=ALU.add), reads=["acc", f"x1u{i}"], writes=[f"o8{i}"])
                S.dma("sp", f"o8{i}", dmaf(out_d[t * 128:(t + 1) * 128, :], o8[i][:]), reads=[f"o8{i}"], writes=["out"])
            S.barrier()
    nc._sched = S
    return nc


def _prep(inputs):
    x = np.asarray(inputs["x"], np.float32)
    c = np.asarray(inputs["c"], np.float32)
    g = lambda k: np.asarray(inputs[k], np.float32)[0]
    shared = {}
    shared["w_ada"] = g("w_ada")
    b_ada = g("b_ada")
    shared["b_ada"] = b_ada
    shared["bada_pp"] = np.ascontiguousarray(b_ada[:8192].reshape(64, 128).T)
    shared["n1w_pp"] = np.ascontiguousarray(g("norm1_w").reshape(32, 128).T)
    shared["n2w"] = g("norm2_w")
    shared["w_in"] = g("w_in")
    qw = np.tile(g("q_norm_w"), 4)
    kw = np.tile(g("k_norm_w"), 4)
    shared["qkw_rep"] = np.ascontiguousarray(np.broadcast_to(np.concatenate([qw, kw])[None, :], (128, 512)))
    shared["sinks"] = g("sinks")
    slopes = (2.0 ** (-8.0 * np.arange(1, 33, dtype=np.float32) / 32)).astype(np.float32)
    qi = np.arange(128)[:, None]
    kj = np.arange(256)[None, :]
    dist = qi + 128 - kj
    valid = (dist >= 0) & (dist < 128)
    ab = np.where(valid[None], -slopes[:, None, None] * dist[None].astype(np.float32), np.float32(NEG)).astype(np.float32)
    shared["abias"] = np.ascontiguousarray(ab.transpose(1, 0, 2).reshape(128, 32 * 256))
    shared["w_pool"] = g("w_pool")
    shared["pscale_pp"] = np.ascontiguousarray(g("pool_scale").reshape(16, 128).T)
    shared["w_aup"] = g("w_attn_up")
    shared["w_bup"] = g("w_pool_up")
    shared["w_out"] = g("w_out")
    shared["wr"] = np.ascontiguousarray(np.concatenate([g("w_router_group"), g("w_router_expert")], axis=1))
    shared["rb"] = np.concatenate([g("b_router_group"), g("b_router_expert")])
    shared["w_gate"] = g("w_gate")
    shared["w_up"] = g("w_up")
    shared["w_down"] = g("w_down")
    cst = np.zeros((128, 448), np.float32)
    cst[:, 0:128] = np.eye(128, dtype=np.float32)
    cst[:, 128:256] = (np.arange(128)[:, None] < np.arange(128)[None, :]).astype(np.float32)
    cst[:, 256:384] = 1.0
    cst[:, 384:448] = (np.arange(64) * CAP)[None, :].astype(np.float32)
    shared["consts"] = cst
    in_maps = []
    for i in range(8):
        b = i // 4
        s0 = (i % 4) * NL
        m = dict(shared)
        if s0 == 0:
            halo = np.zeros((HALO, D), np.float32)
        else:
            halo = x[b, s0 - HALO:s0]
        m["xh"] = np.ascontiguousarray(np.concatenate([halo, x[b, s0:s0 + NL]], axis=0))
        m["cpp"] = np.ascontiguousarray(c[b].reshape(32, 128).T)
        fl = np.zeros((128, 2), np.float32)
        fl[:, 0] = 0.0 if s0 == 0 else 1.0
        fl[:, 1] = NEG if s0 == 0 else 0.0
        m["flag"] = fl
        invc = np.zeros((128, 64), np.float32)
        for gi, w in enumerate((2, 4, 8, 16)):
            tpos = s0 + np.arange(16) + 1
            invc[:, gi * 16:(gi + 1) * 16] = (1.0 / np.minimum(tpos, w)).astype(np.float32)[None, :]
        m["invc"] = invc
        in_maps.append(m)
    return in_maps


_NC_CACHE = {}


def kernel(**inputs):
    in_maps = _prep(inputs)
    if "nc" not in _NC_CACHE:
        _NC_CACHE["nc"] = build_nc()
    nc = _NC_CACHE["nc"]
    res = run_bass_kernel_spmd(nc, in_maps, core_ids=list(range(8)))
    out = np.empty((2, 4 * NL, D), np.float32)
    for i in range(8):
        b = i // 4
        s0 = (i % 4) * NL
        out[b, s0:s0 + NL] = res.results[i]["out"]
    return out
```
